# Optimizing a Trainium2 kernel written in Bass

```python
import math
import jax
import jax.numpy as jnp
from jax import lax
import numpy as np

D_MODEL = 1024
BATCH = 8
SEQ = 4096
DEPTH = 2

HEAD_DIM = 64
N_HEADS_DIFF = D_MODEL // (2 * HEAD_DIM)
DIFF_QK_DIM = HEAD_DIM // 2
N_HEADS_DIL = D_MODEL // (2 * HEAD_DIM)
N_ATTN_HEADS = N_HEADS_DIFF + N_HEADS_DIL
DIL_PAIRS = ((128, 1), (512, 4), (2048, 16))
ATTN_BLOCK = 128
SSM_GROUP_WIDTH = 16
SSM_GROUPS = D_MODEL // SSM_GROUP_WIDTH
SSM_STATE = 64
SSM_CHUNK = 128
D_FF = 2816
N_EXPERTS = 8
TOP_K = 2
D_FF_EXPERT = 1408
DN_ALPHA = (2.0 * DEPTH) ** 0.25
DN_BETA = (8.0 * DEPTH) ** -0.25
EPS = 1e-5
F32 = jnp.float32

kernel_name = 'hybrid_diff_dilated_s5_moe_block'


def _layernorm(x, g, b):
    xf = x.astype(F32)
    mu = jnp.mean(xf, axis=-1, keepdims=True)
    var = jnp.mean(jnp.square(xf - mu), axis=-1, keepdims=True)
    y = (xf - mu) * lax.rsqrt(var + EPS)
    return (y * g.astype(F32) + b.astype(F32)).astype(x.dtype)


def _rmsnorm(x, w):
    xf = x.astype(F32)
    y = xf * lax.rsqrt(jnp.mean(jnp.square(xf), axis=-1, keepdims=True) + EPS)
    return (y * w.astype(F32)).astype(x.dtype)


def _ada(c, w, b):
    mod = jax.nn.silu(c) @ w + b
    return jnp.split(mod[:, None, :], 6, axis=-1)


def _swiglu(h, w_gate, w_up, w_down):
    return (jax.nn.silu(h @ w_gate) * (h @ w_up)) @ w_down


def _alibi_slopes():
    i = jnp.arange(N_ATTN_HEADS, dtype=F32) + 1.0
    return jnp.exp2(-8.0 * i / N_ATTN_HEADS)


def _diff_attention(q, k, v, lam, slopes):
    bsz, n_h, s_len, _, dq = q.shape
    nb = s_len // ATTN_BLOCK
    qb = q.reshape(bsz, n_h, nb, ATTN_BLOCK, 2, dq).transpose(2, 0, 1, 3, 4, 5)
    scale = dq ** -0.5
    spos = jnp.arange(s_len)
    lam = lam.astype(F32)

    def block(args):
        i, qi = args
        s = jnp.einsum('bhqmd,bhkmd->bhmqk', qi, k).astype(F32) * scale
        dist = (i * ATTN_BLOCK + jnp.arange(ATTN_BLOCK))[:, None] - spos[None, :]
        bias = -slopes[:, None, None, None] * dist.astype(F32)
        s = jnp.where(dist >= 0, s + bias, -jnp.inf)
        p = jax.nn.softmax(s, axis=-1).astype(v.dtype)
        o = jnp.einsum('bhmqk,bhkd->bhmqd', p, v)
        return o[:, :, 0] - lam.astype(o.dtype) * o[:, :, 1]

    o = lax.map(block, (jnp.arange(nb), qb))
    return o.transpose(1, 2, 0, 3, 4).reshape(bsz, n_h, s_len, -1)


def _dilated_branch(q, k, v, slopes, window, dilation):
    bsz, n_h, s_len, dh = q.shape
    blk = ATTN_BLOCK
    n_back = window // dilation
    sub_len = -(-s_len // dilation)
    sub_len = -(-sub_len // blk) * blk
    s_pad = sub_len * dilation
    nb = sub_len // blk
    pad = ((0, 0), (0, 0), (0, s_pad - s_len), (0, 0))

    def strided(a):
        a = jnp.pad(a, pad).reshape(bsz, n_h, sub_len, dilation, dh).transpose(0, 1, 3, 2, 4)
        return a.reshape(bsz, n_h, dilation, nb, blk, dh)

    def with_prev(a):
        prev = jnp.pad(a, ((0, 0),) * 3 + ((1, 0), (0, 0), (0, 0)))[:, :, :, :-1]
        return jnp.concatenate([prev, a], axis=4)

    qs = strided(q)
    kc = with_prev(strided(k))
    vc = with_prev(strided(v))
    s = jnp.einsum('bhrnqd,bhrnkd->bhrnqk', qs, kc).astype(F32) * (dh ** -0.5)
    qi = np.arange(blk)[:, None]
    kj = np.arange(2 * blk)[None, :]
    dist = qi + blk - kj
    valid = (dist >= 0) & (dist <= n_back)
    mask = valid[None] & ((np.arange(nb)[:, None, None] > 0) | (kj[None] >= blk))
    bias = -slopes[:, None, None, None, None] * jnp.asarray(dist * dilation, F32)
    s = jnp.where(mask, s + bias, -jnp.inf)
    lse = jax.nn.logsumexp(s, axis=-1)
    p = jnp.exp(s - lse[..., None]).astype(v.dtype)
    o = jnp.einsum('bhrnqk,bhrnkd->bhrnqd', p, vc)
    o = o.reshape(bsz, n_h, dilation, sub_len, dh).transpose(0, 1, 3, 2, 4)
    o = o.reshape(bsz, n_h, s_pad, dh)[:, :, :s_len]
    lse = lse.reshape(bsz, n_h, dilation, sub_len).transpose(0, 1, 3, 2)
    lse = lse.reshape(bsz, n_h, s_pad)[:, :, :s_len]
    return o, lse


def _dilated_mixture(q, k, v, slopes):
    outs, lses = [], []
    for window, dilation in DIL_PAIRS:
        o, l = _dilated_branch(q, k, v, slopes, window, dilation)
        outs.append(o)
        lses.append(l)
    wts = jax.nn.softmax(jnp.stack(lses), axis=0).astype(q.dtype)
    return jnp.einsum('gbhs,gbhsd->bhsd', wts, jnp.stack(outs))


def _hybrid_attention(h, layer_idx, w_in, lam_q1, lam_k1, lam_q2, lam_k2, subln_w, w_out):
    bsz, s_len, _ = h.shape
    proj = h @ w_in
    aq, ak, av, bq, bk, bv = jnp.split(proj, 6, axis=-1)
    slopes = _alibi_slopes()
    q = aq.reshape(bsz, s_len, N_HEADS_DIFF, 2, DIFF_QK_DIM).transpose(0, 2, 1, 3, 4)
    k = ak.reshape(bsz, s_len, N_HEADS_DIFF, 2, DIFF_QK_DIM).transpose(0, 2, 1, 3, 4)
    v = av.reshape(bsz, s_len, N_HEADS_DIFF, HEAD_DIM).transpose(0, 2, 1, 3)
    lam_init = 0.8 - 0.6 * math.exp(-0.3 * layer_idx)
    lam = (jnp.exp(jnp.sum(lam_q1.astype(F32) * lam_k1.astype(F32)))
           - jnp.exp(jnp.sum(lam_q2.astype(F32) * lam_k2.astype(F32))) + lam_init)
    oa = _diff_attention(q, k, v, lam, slopes[:N_HEADS_DIFF])
    oa = _rmsnorm(oa, subln_w) * (1.0 - lam_init)
    q = bq.reshape(bsz, s_len, N_HEADS_DIL, HEAD_DIM).transpose(0, 2, 1, 3)
    k = bk.reshape(bsz, s_len, N_HEADS_DIL, HEAD_DIM).transpose(0, 2, 1, 3)
    v = bv.reshape(bsz, s_len, N_HEADS_DIL, HEAD_DIM).transpose(0, 2, 1, 3)
    ob = _dilated_mixture(q, k, v, slopes[N_HEADS_DIFF:])
    o = jnp.concatenate([oa, ob], axis=1).transpose(0, 2, 1, 3).reshape(bsz, s_len, D_MODEL)
    return o @ w_out


def _cplx_combine(e1, e2):
    a1r, a1i, b1r, b1i = e1
    a2r, a2i, b2r, b2i = e2
    return (a2r * a1r - a2i * a1i,
            a2r * a1i + a2i * a1r,
            a2r * b1r - a2i * b1i + b2r,
            a2r * b1i + a2i * b1r + b2i)


def _s5(h, a_re, a_im, log_dt, b_re, b_im, c_re, c_im, d_skip):
    bsz, s_len, _ = h.shape
    u = h.astype(F32)
    ar, ai = a_re.astype(F32), a_im.astype(F32)
    dt = jnp.exp(log_dt.astype(F32))[:, None]
    mag = jnp.exp(dt * ar)
    abr, abi = mag * jnp.cos(dt * ai), mag * jnp.sin(dt * ai)
    den = ar * ar + ai * ai
    nr, ni = abr - 1.0, abi
    zr, zi = (nr * ar + ni * ai) / den, (ni * ar - nr * ai) / den
    br, bi = b_re.astype(F32), b_im.astype(F32)
    bbr = zr[..., None] * br - zi[..., None] * bi
    bbi = zr[..., None] * bi + zi[..., None] * br
    cr, ci = c_re.astype(F32), c_im.astype(F32)
    n_chunks = s_len // SSM_CHUNK
    uc = u.reshape(bsz, n_chunks, SSM_CHUNK, SSM_GROUPS, SSM_GROUP_WIDTH).transpose(1, 2, 0, 3, 4)

    def chunk(carry, uk):
        h0r, h0i = carry
        xr = jnp.einsum('lbgc,gpc->lbgp', uk, bbr)
        xi = jnp.einsum('lbgc,gpc->lbgp', uk, bbi)
        a_r = jnp.broadcast_to(abr, xr.shape)
        a_i = jnp.broadcast_to(abi, xr.shape)
        cum_r, cum_i, hr, hi = lax.associative_scan(_cplx_combine, (a_r, a_i, xr, xi), axis=0)
        hr, hi = hr + cum_r * h0r - cum_i * h0i, hi + cum_r * h0i + cum_i * h0r
        y = jnp.einsum('lbgp,gcp->lbgc', hr, cr) - jnp.einsum('lbgp,gcp->lbgc', hi, ci)
        return (hr[-1], hi[-1]), y

    init = (jnp.zeros((bsz, SSM_GROUPS, SSM_STATE), F32),) * 2
    _, y = lax.scan(chunk, init, uc)
    y = y.transpose(2, 0, 1, 3, 4).reshape(bsz, s_len, D_MODEL)
    return (y + d_skip.astype(F32) * u).astype(h.dtype)


def _moe(h, router_w, router_b, w_gate, w_up, w_down):
    logits = (h @ router_w).astype(F32) + router_b.astype(F32)
    top_v, top_i = lax.top_k(logits, TOP_K)
    gates = jax.nn.softmax(top_v, axis=-1)
    cw = jnp.sum(jax.nn.one_hot(top_i, N_EXPERTS, dtype=F32) * gates[..., None], axis=-2)
    cw = cw.astype(h.dtype)
    y = jnp.zeros_like(h)
    for e in range(N_EXPERTS):
        y = y + cw[..., e:e + 1] * _swiglu(h, w_gate[e], w_up[e], w_down[e])
    return y


def _attention_layer(x, c, layer_idx, ada_w, ada_b, w_in, lam_q1, lam_k1, lam_q2, lam_k2,
                     subln_w, w_out, ln1_g, ln1_b, ffn_w_gate, ffn_w_up, ffn_w_down, ln2_g, ln2_b):
    sh1, sc1, g1, sh2, sc2, g2 = _ada(c, ada_w, ada_b)
    y = _hybrid_attention(x * (1.0 + sc1) + sh1, layer_idx, w_in, lam_q1, lam_k1, lam_q2, lam_k2,
                          subln_w, w_out)
    x = _layernorm(DN_ALPHA * x + (1.0 + g1) * y, ln1_g, ln1_b)
    y = _swiglu(x * (1.0 + sc2) + sh2, ffn_w_gate, ffn_w_up, ffn_w_down)
    return _layernorm(DN_ALPHA * x + (1.0 + g2) * y, ln2_g, ln2_b)


def _ssm_layer(x, c, ada_w, ada_b, a_re, a_im, log_dt, b_re, b_im, c_re, c_im, d_skip, w_glu, b_glu,
               ln1_g, ln1_b, router_w, router_b, exp_w_gate, exp_w_up, exp_w_down, ln2_g, ln2_b):
    sh1, sc1, g1, sh2, sc2, g2 = _ada(c, ada_w, ada_b)
    y = _s5(x * (1.0 + sc1) + sh1, a_re, a_im, log_dt, b_re, b_im, c_re, c_im, d_skip)
    z = jax.nn.gelu(y) @ w_glu + b_glu
    y = z[..., :D_MODEL] * jax.nn.sigmoid(z[..., D_MODEL:])
    x = _layernorm(DN_ALPHA * x + (1.0 + g1) * y, ln1_g, ln1_b)
    y = _moe(x * (1.0 + sc2) + sh2, router_w, router_b, exp_w_gate, exp_w_up, exp_w_down)
    return _layernorm(DN_ALPHA * x + (1.0 + g2) * y, ln2_g, ln2_b)


def setup_inputs(seed: int = 0) -> dict:
    key = jax.random.key(seed)
    ks = iter(jax.random.split(key, 64))
    D = D_MODEL

    def nrm(shape, scale):
        return jax.random.normal(next(ks), shape, F32) * scale

    def gain(n):
        return 1.0 + nrm((n,), 0.01)

    ada_scale = 0.1 * D ** -0.5
    v_cols = jnp.concatenate([jnp.ones((D,), F32), jnp.full((D // 2,), DN_BETA, F32),
                              jnp.ones((D,), F32), jnp.full((D // 2,), DN_BETA, F32)])
    n_idx = jnp.arange(SSM_STATE, dtype=F32)[None, :]
    p = {}
    p['x'] = nrm((BATCH, SEQ, D), 1.0)
    p['c'] = nrm((BATCH, D), 1.0)
    p['l0_ada_w'] = nrm((D, 6 * D), ada_scale)
    p['l0_ada_b'] = nrm((6 * D,), 0.01)
    p['l0_w_in'] = nrm((D, 3 * D), D ** -0.5) * v_cols
    p['l0_lam_q1'] = nrm((DIFF_QK_DIM,), 0.1)
    p['l0_lam_k1'] = nrm((DIFF_QK_DIM,), 0.1)
    p['l0_lam_q2'] = nrm((DIFF_QK_DIM,), 0.1)
    p['l0_lam_k2'] = nrm((DIFF_QK_DIM,), 0.1)
    p['l0_subln_w'] = gain(HEAD_DIM)
    p['l0_w_out'] = nrm((D, D), DN_BETA * D ** -0.5)
    p['l0_ln1_g'] = gain(D)
    p['l0_ln1_b'] = nrm((D,), 0.01)
    p['l0_ffn_w_gate'] = nrm((D, D_FF), D ** -0.5)
    p['l0_ffn_w_up'] = nrm((D, D_FF), D ** -0.5)
    p['l0_ffn_w_down'] = nrm((D_FF, D), DN_BETA * D_FF ** -0.5)
    p['l0_ln2_g'] = gain(D)
    p['l0_ln2_b'] = nrm((D,), 0.01)
    p['l1_ada_w'] = nrm((D, 6 * D), ada_scale)
    p['l1_ada_b'] = nrm((6 * D,), 0.01)
    p['l1_a_re'] = -0.5 + nrm((SSM_GROUPS, SSM_STATE), 0.01)
    p['l1_a_im'] = math.pi * n_idx + nrm((SSM_GROUPS, SSM_STATE), 0.01)
    p['l1_log_dt'] = jax.random.uniform(next(ks), (SSM_GROUPS,), F32, math.log(1e-3), math.log(1e-1))
    p['l1_b_re'] = nrm((SSM_GROUPS, SSM_STATE, SSM_GROUP_WIDTH), (2.0 * SSM_GROUP_WIDTH) ** -0.5)
    p['l1_b_im'] = nrm((SSM_GROUPS, SSM_STATE, SSM_GROUP_WIDTH), (2.0 * SSM_GROUP_WIDTH) ** -0.5)
    p['l1_c_re'] = nrm((SSM_GROUPS, SSM_GROUP_WIDTH, SSM_STATE), (2.0 * SSM_STATE) ** -0.5)
    p['l1_c_im'] = nrm((SSM_GROUPS, SSM_GROUP_WIDTH, SSM_STATE), (2.0 * SSM_STATE) ** -0.5)
    p['l1_d_skip'] = nrm((D,), 0.5)
    p['l1_w_glu'] = nrm((D, 2 * D), DN_BETA * D ** -0.5)
    p['l1_b_glu'] = nrm((2 * D,), 0.01)
    p['l1_ln1_g'] = gain(D)
    p['l1_ln1_b'] = nrm((D,), 0.01)
    p['l1_router_w'] = nrm((D, N_EXPERTS), D ** -0.5)
    p['l1_router_b'] = nrm((N_EXPERTS,), 0.01)
    p['l1_exp_w_gate'] = nrm((N_EXPERTS, D, D_FF_EXPERT), D ** -0.5)
    p['l1_exp_w_up'] = nrm((N_EXPERTS, D, D_FF_EXPERT), D ** -0.5)
    p['l1_exp_w_down'] = nrm((N_EXPERTS, D_FF_EXPERT, D), DN_BETA * D_FF_EXPERT ** -0.5)
    p['l1_ln2_g'] = gain(D)
    p['l1_ln2_b'] = nrm((D,), 0.01)
    return p


def reference(x, c,
              l0_ada_w, l0_ada_b, l0_w_in, l0_lam_q1, l0_lam_k1, l0_lam_q2, l0_lam_k2, l0_subln_w,
              l0_w_out, l0_ln1_g, l0_ln1_b, l0_ffn_w_gate, l0_ffn_w_up, l0_ffn_w_down, l0_ln2_g, l0_ln2_b,
              l1_ada_w, l1_ada_b, l1_a_re, l1_a_im, l1_log_dt, l1_b_re, l1_b_im, l1_c_re, l1_c_im,
              l1_d_skip, l1_w_glu, l1_b_glu, l1_ln1_g, l1_ln1_b, l1_router_w, l1_router_b,
              l1_exp_w_gate, l1_exp_w_up, l1_exp_w_down, l1_ln2_g, l1_ln2_b):
    layer_params = (
        (l0_ada_w, l0_ada_b, l0_w_in, l0_lam_q1, l0_lam_k1, l0_lam_q2, l0_lam_k2, l0_subln_w,
         l0_w_out, l0_ln1_g, l0_ln1_b, l0_ffn_w_gate, l0_ffn_w_up, l0_ffn_w_down, l0_ln2_g, l0_ln2_b),
        (l1_ada_w, l1_ada_b, l1_a_re, l1_a_im, l1_log_dt, l1_b_re, l1_b_im, l1_c_re, l1_c_im,
         l1_d_skip, l1_w_glu, l1_b_glu, l1_ln1_g, l1_ln1_b, l1_router_w, l1_router_b,
         l1_exp_w_gate, l1_exp_w_up, l1_exp_w_down, l1_ln2_g, l1_ln2_b),
    )
    for i in range(DEPTH):
        if i % 2 == 0:
            x = _attention_layer(x, c, i, *layer_params[i])
        else:
            x = _ssm_layer(x, c, *layer_params[i])
    return x
```

```python
import math
from contextlib import ExitStack
import numpy as np
import concourse.bass as bass
import concourse.mybir as mybir
from concourse.bass_utils import run_bass_kernel_spmd

F32 = mybir.dt.float32
BF16 = mybir.dt.bfloat16
I32 = mybir.dt.int32
AF = mybir.ActivationFunctionType
ALU = mybir.AluOpType
AX = mybir.AxisListType

D = 1024
S = 4096
NT = S // 128
KC = D // 128
DFF = 2816
FC = DFF // 128
NE = 8
DFE = 1408
FCE = DFE // 128
DN_ALPHA = (2.0 * 2) ** 0.25
EPS = 1e-5
LAM_INIT0 = 0.8 - 0.6 * math.exp(-0.3 * 0)
SLOPES = [2.0 ** (-8.0 * (i + 1) / 16.0) for i in range(16)]

ENGS = ("pe", "act", "dve", "pool", "sp")


class Tok:
    __slots__ = ("name", "w", "r", "sem", "cnt")

    def __init__(self, name, sem=None):
        self.name = name
        self.w = None
        self.r = {}
        self.sem = sem
        self.cnt = 0


class Prog:
    def __init__(self, nc, stack):
        self.nc = nc
        self.stack = stack
        self.sem = {e: stack.enter_context(nc.semaphore("eng_" + e)) for e in ENGS}
        self.cnt = {e: 0 for e in ENGS}
        self.waited = {e: {} for e in ENGS}
        self.q = {e: [] for e in ENGS}
        self.ndsem = 0
        self.out_events = []

    def tok(self, name, dma=False):
        t = Tok(name)
        if dma:
            t.sem = self.stack.enter_context(self.nc.semaphore("d%d_%s" % (self.ndsem, name)))
            self.ndsem += 1
        return t

    def _wait(self, eng, deps, skip_own):
        own = self.sem[eng]
        for s, v in deps.items():
            if skip_own and s is own:
                if eng == "pe" or self.cnt[eng] - v >= 4:
                    continue
            if self.waited[eng].get(s, 0) >= v:
                continue
            self.waited[eng][s] = v
            self.q[eng].append(("wait", s, v))

    @staticmethod
    def _collect(reads, writes):
        deps = {}

        def add(ev):
            if ev is None:
                return
            s, v = ev
            if deps.get(s, 0) < v:
                deps[s] = v
        for t in reads:
            add(t.w)
            if t.name[0] == "p" and t.name != "post":
                for s, v in t.r.items():
                    add((s, v))
        for t in writes:
            add(t.w)
            for s, v in t.r.items():
                add((s, v))
        return deps

    mute = False

    def op(self, eng, fn, reads=(), writes=(), inc=True):
        if self.mute:
            return
        deps = self._collect(reads, writes)
        self._wait(eng, deps, True)
        own = self.sem[eng]
        if inc:
            self.cnt[eng] += 1
            n = self.cnt[eng]
        else:
            n = self.cnt[eng] + 1
        self.q[eng].append(("op", fn, own if inc else None))
        for t in reads:
            if t.name[0] == "p" and t.name != "post":
                t.w = (own, n)
                t.r = {}
            elif t.r.get(own, 0) < n:
                t.r[own] = n
        for t in writes:
            t.w = (own, n)
            t.r = {}

    def dma(self, eng, out, in_, reads, writes, st, **kw):
        if self.mute:
            return (st.sem, st.cnt)
        deps = self._collect(reads, writes)
        self._wait(eng, deps, False)
        st.cnt += 16
        sem, v = st.sem, st.cnt
        self.q[eng].append(("dma", out, in_, sem, kw))
        for t in reads:
            if t.r.get(sem, 0) < v:
                t.r[sem] = v
        for t in writes:
            t.w = (sem, v)
            t.r = {}
        return (sem, v)

    def flush(self, final_waits=()):
        nc = self.nc
        q = self.q
        self.q = {e: [] for e in ENGS}

        def replay(e, lst, extra=()):
            for it in lst:
                k = it[0]
                if k == "wait":
                    e.wait_ge(it[1], it[2])
                elif k == "op":
                    ins = it[1](e)
                    if it[2] is not None:
                        ins.then_inc(it[2], 1)
                else:
                    e.dma_start(out=it[1], in_=it[2], **it[4]).then_inc(it[3], 16)
            for s, v in extra:
                e.wait_ge(s, v)

        with nc.Block() as block:
            @block.tensor
            def _(e):
                replay(e, q["pe"])

            @block.scalar
            def _(e):
                replay(e, q["act"])

            @block.vector
            def _(e):
                replay(e, q["dve"])

            @block.gpsimd
            def _(e):
                replay(e, q["pool"])

            @block.sync
            def _(e):
                replay(e, q["sp"], final_waits)


class K:
    pass


def build_nc(debug=None, phases=("0", "A", "B", "C", "D0", "D1", "D1b", "D2"), x2_input=False, opts=None):
    nc = bass.Bass("TRN2", target_bir_lowering=False)
    k = K()
    k.nc = nc
    k.debug = debug
    for kk_, vv_ in (opts or {}).items():
        setattr(k, kk_, vv_)

    def din(name, shape):
        return nc.dram_tensor(name, list(shape), F32, kind="ExternalInput").ap()

    k.x = din("x", [S, D])
    k.c = din("c", [1, D])
    k.l0 = dict(
        ada_w=din("l0_ada_w", [D, 6 * D]), ada_b=din("l0_ada_b", [1, 6 * D]),
        w_in=din("l0_w_in", [D, 3 * D]),
        lam=din("l0_lam", [1, 128]),
        subln_w=din("l0_subln_w", [1, 64]),
        w_out=din("l0_w_out", [D, D]),
        ln1_g=din("l0_ln1_g", [1, D]), ln1_b=din("l0_ln1_b", [1, D]),
        w_gate=din("l0_ffn_w_gate", [D, DFF]), w_up=din("l0_ffn_w_up", [D, DFF]),
        w_down=din("l0_ffn_w_down", [DFF, D]),
        ln2_g=din("l0_ln2_g", [1, D]), ln2_b=din("l0_ln2_b", [1, D]),
    )
    k.l1 = dict(
        ada_w=din("l1_ada_w", [D, 6 * D]), ada_b=din("l1_ada_b", [1, 6 * D]),
        a_re=din("l1_a_re", [64, 64]), a_im=din("l1_a_im", [64, 64]), log_dt=din("l1_log_dt", [1, 64]),
        b_re=din("l1_b_re", [64, 64, 16]), b_im=din("l1_b_im", [64, 64, 16]),
        c_re=din("l1_c_re", [1024, 64]), c_im=din("l1_c_im", [1024, 64]),
        d_skip=din("l1_d_skip", [1, D]),
        w_glu=din("l1_w_glu", [D, 2 * D]), b_glu=din("l1_b_glu", [1, 2 * D]),
        ln1_g=din("l1_ln1_g", [1, D]), ln1_b=din("l1_ln1_b", [1, D]),
        router_w=din("l1_router_w", [D, NE]), router_b=din("l1_router_b", [1, NE]),
        w_gate=din("l1_exp_w_gate", [NE, D, DFE]), w_up=din("l1_exp_w_up", [NE, D, DFE]),
        w_down=din("l1_exp_w_down", [NE, DFE, D]),
        ln2_g=din("l1_ln2_g", [1, D]), ln2_b=din("l1_ln2_b", [1, D]),
    )
    k.out = nc.dram_tensor("out", [S, D], F32, kind="ExternalOutput").ap()

    k.qT_s = nc.dram_tensor("qT_s", [4 * 512, S], BF16).ap()
    k.v_s = nc.dram_tensor("v_s", [S, D], BF16).ap()
    k.o_s = nc.dram_tensor("o_s", [S, D], BF16).ap()
    if x2_input:
        k.x2_s = din("x2_in", [S, D])
    else:
        k.x2_s = nc.dram_tensor("x2_s", [S, D], F32).ap()
    k.mod_s = nc.dram_tensor("mod_s", [1, 6 * D], F32).ap()
    k.gpad_s = nc.dram_tensor("gpad_s", [64, 16, 63 * 16], BF16).ap()
    k.yT_s = nc.dram_tensor("yT_s", [8, 128, 32, 128], BF16).ap()
    k.x3_s = nc.dram_tensor("x3_s", [32, 128, D], F32).ap()
    k.hmT_s = nc.dram_tensor("hmT_s", [D, S], BF16).ap()
    k.cw_s = nc.dram_tensor("cw_s", [32, 128, NE], F32).ap()
    k.yacc_s = nc.dram_tensor("yacc_s", [32, 128, D], F32).ap()
    k.aug_s = nc.dram_tensor("aug_s", [3, 16, 512], BF16).ap()
    k.x1_s = nc.dram_tensor("x1_s", [S, D], F32).ap()
    k.h2T_s = nc.dram_tensor("h2T_s", [D, S], BF16).ap()

    if debug:
        k.dbg = {}
        for name, shape, dt in debug:
            k.dbg[name] = nc.dram_tensor("dbg_" + name, list(shape), dt, kind="ExternalOutput").ap()

    with ExitStack() as gs:
        P = Prog(nc, gs)
        k.P = P
        k.gs = gs
        setup_consts(k)
        with ExitStack() as cB:
            if "B" in phases:
                attn_consts_alloc(k, cB)
            if "0" in phases:
                phase0_mod(k, 0)
            if "A" in phases:
                phaseA(k)
            if "B" in phases:
                phaseB(k)
        if "C" in phases:
            phaseC(k)
        if "D0" in phases:
            phase0_mod(k, 1)
        if "D1" in phases:
            phaseD1(k)
        if "D1b" in phases:
            phaseD1b(k)
        if "D2" in phases:
            phaseD2(k)
        P.flush(final_waits=P.out_events)
    return nc


_UNIQ = [0]


def sb(k, stack, name, shape, dt):
    _UNIQ[0] += 1
    return stack.enter_context(k.nc.sbuf_tensor("%s_%d" % (name, _UNIQ[0]), list(shape), dt))


def ps(k, stack, name, shape, dt=F32):
    _UNIQ[0] += 1
    return stack.enter_context(k.nc.psum_tensor("%s_%d" % (name, _UNIQ[0]), list(shape), dt))


def setup_consts(k):
    nc, P, gs = k.nc, k.P, k.gs
    k.ident_f = sb(k, gs, "ident_f", [128, 128], F32)
    k.ident_b = sb(k, gs, "ident_b", [128, 128], BF16)
    k.t_ident = P.tok("ident")
    idf, idb = k.ident_f, k.ident_b

    def mk_ident(e):
        e.memset(idf[:], 1.0)
        return e.affine_select(out=idf[:], in_=idf[:], pattern=[[-1, 128]], compare_op=ALU.is_equal,
                               fill=0.0, base=0, channel_multiplier=1)
    P.op("pool", mk_ident, writes=[k.t_ident])
    P.op("pool", lambda e: e.tensor_copy(out=idb[:], in_=idf[:]), reads=[k.t_ident], writes=[k.t_ident])
    k.modT = [sb(k, gs, "modT%d" % l, [128, 48], F32) for l in range(2)]
    k.t_modT = [P.tok("modT%d" % l) for l in range(2)]
    k.gbc = sb(k, gs, "gbc", [128, 2, D], F32)
    k.t_gbc = P.tok("gbc")


def phase0_mod(k, l):
    nc, P = k.nc, k.P
    prm = k.l0 if l == 0 else k.l1
    with ExitStack() as ph:
        c_col = sb(k, ph, "c_col", [128, KC], F32)
        sc_bc = sb(k, ph, "sc_bc", [128, KC, 128], F32)
        ab = sb(k, ph, "ada_b_sb", [1, 6 * D], F32)
        ones1 = sb(k, ph, "ones1", [1, 128], F32)
        wst = [sb(k, ph, "ada_st%d" % i, [128, KC, 512], F32) for i in range(2)]
        modbc = sb(k, ph, "modbc", [128, 6 * D], F32)
        pmm = [ps(k, ph, "pmod%d" % i, [128, 512]) for i in range(2)]
        ptr = ps(k, ph, "ptr_mod", [128, 512])
        t_c = P.tok("c_col", dma=True)
        t_ab = P.tok("ada_b", dma=True)
        t_w = [P.tok("ada_st%d" % i, dma=True) for i in range(2)]
        t_scbc = P.tok("sc_bc")
        t_ones = P.tok("ones1")
        t_pmm = [P.tok("pmod%d" % i) for i in range(2)]
        t_ptr = P.tok("ptr")
        t_modbc = P.tok("modbc")

        P.dma("sp", c_col[:], k.c.rearrange("o (kc p) -> p (o kc)", p=128), [], [t_c], t_c,
              allow_slow_non_contiguous=True)
        P.dma("sp", ab[:], prm["ada_b"], [], [t_ab], t_ab)
        P.op("act", lambda e: e.activation(out=c_col[:], in_=c_col[:], func=AF.Silu), reads=[t_c], writes=[t_c])
        P.op("dve", lambda e: e.memset(ones1[:], 1.0), writes=[t_ones])
        for kc in range(KC):
            P.op("dve", lambda e, kc=kc: e.tensor_copy(out=sc_bc[:, kc, :],
                                                      in_=c_col[:, kc:kc + 1].broadcast_to([128, 128])),
                 reads=[t_c], writes=[t_scbc])
        wv = prm["ada_w"].rearrange("(kc p) n -> p kc n", p=128)
        for n in range(12):
            b = n % 2
            P.dma("sp", wst[b][:], wv[:, :, n * 512:(n + 1) * 512], [], [t_w[b]], t_w[b])
            for kc in range(KC):
                P.op("pe", lambda e, b=b, kc=kc: e.matmul(pmm[b][:], lhsT=sc_bc[:, kc, :], rhs=wst[b][:, kc, :],
                                                         start=(kc == 0), stop=False),
                     reads=[t_scbc, t_w[b]], writes=[t_pmm[b]], inc=False)
            P.op("pe", lambda e, b=b, n=n: e.matmul(pmm[b][:], lhsT=ones1[0:1, :], rhs=ab[0:1, n * 512:(n + 1) * 512],
                                                   start=False, stop=True),
                 reads=[t_ones, t_ab], writes=[t_pmm[b]])
            P.op("dve", lambda e, b=b, n=n: e.tensor_copy(out=modbc[:, n * 512:(n + 1) * 512], in_=pmm[b][:]),
                 reads=[t_pmm[b]], writes=[t_modbc])
        gbc = k.gbc
        P.op("dve", lambda e: e.tensor_scalar_add(out=gbc[:, 0, :], in0=modbc[:, 2 * D:3 * D], scalar1=1.0),
             reads=[t_modbc], writes=[k.t_gbc])
        P.op("dve", lambda e: e.tensor_scalar_add(out=gbc[:, 1, :], in0=modbc[:, 5 * D:6 * D], scalar1=1.0),
             reads=[t_modbc], writes=[k.t_gbc])
        if l == 1:
            t_ms = P.tok("modrow", dma=True)
            P.dma("sp", k.mod_s, modbc[0:1, :], [t_modbc], [], t_ms)
            dma_barrier(P, "sp", [t_ms])
        modT = k.modT[l]
        idf = k.ident_f
        for g4 in range(12):
            for i in range(4):
                j = g4 * 4 + i
                P.op("pe", lambda e, i=i, j=j: e.transpose(ptr[:, i * 128:(i + 1) * 128],
                                                          modbc[:, j * 128:(j + 1) * 128], idf[:]),
                     reads=[t_modbc, k.t_ident], writes=[t_ptr], inc=(i == 3))
            P.op("dve", lambda e, g4=g4: e.tensor_copy(
                out=modT[:, g4 * 4:(g4 + 1) * 4],
                in_=ptr[:].rearrange("p (i c) -> p i c", c=128)[:, :, 0]),
                reads=[t_ptr], writes=[k.t_modT[l]])
        P.op("dve", lambda e: e.tensor_scalar_add(out=modT[:, 8:16], in0=modT[:, 8:16], scalar1=1.0),
             reads=[k.t_modT[l]], writes=[k.t_modT[l]])
        P.op("dve", lambda e: e.tensor_scalar_add(out=modT[:, 32:40], in0=modT[:, 32:40], scalar1=1.0),
             reads=[k.t_modT[l]], writes=[k.t_modT[l]])
        if k.debug and ("modT%d" % l) in k.dbg:
            t_d = P.tok("dbgmod", dma=True)
            ev = P.dma("sp", k.dbg["modT%d" % l], modT[:], [k.t_modT[l]], [], t_d)
            P.out_events.append(ev)
        if l == 0 and getattr(k, "BC", None) is not None:
            with ExitStack() as tmpc:
                attn_consts_gen(k, tmpc)
                P.flush()
        else:
            P.flush()


def dma_barrier(P, eng, toks):
    for t in toks:
        if t.cnt > 0 and P.waited[eng].get(t.sem, 0) < t.cnt:
            P.waited[eng][t.sem] = t.cnt
            P.q[eng].append(("wait", t.sem, t.cnt))


def load_cast_weight(k, dst, src_view, tok, nchunks, eng="pool"):
    P = k.P
    A = dst.shape[1]
    step = (A + nchunks - 1) // nchunks
    for a0 in range(0, A, step):
        a1 = min(A, a0 + step)
        P.dma(eng, dst[:, a0:a1, :], src_view[:, a0:a1, :], [], [tok], tok)


def phaseA(k):
    nc, P = k.nc, k.P
    modT = k.modT[0]
    with ExitStack() as ph:
        w_in = sb(k, ph, "w_in_sb", [128, KC, 3 * D], BF16)
        xs = [sb(k, ph, "xA%d" % i, [128, 4, D], F32) for i in range(2)]
        hT = [sb(k, ph, "hT%d" % i, [128, KC, 512], BF16) for i in range(2)]
        qt = [sb(k, ph, "qtA%d" % i, [128, 512], BF16) for i in range(4)]
        vsb = [sb(k, ph, "vA%d" % i, [128, D], BF16) for i in range(2)]
        ptr = [ps(k, ph, "ptrA%d" % i, [128, 512]) for i in range(2)]
        pm = [ps(k, ph, "pmA%d" % i, [128, 512]) for i in range(3)]
        t_win = P.tok("w_in", dma=True)
        t_x = [P.tok("xA%d" % i, dma=True) for i in range(2)]
        t_hT = [P.tok("hT%d" % i) for i in range(2)]
        t_qt = [P.tok("qt%d" % i, dma=True) for i in range(4)]
        t_v = [P.tok("vsb%d" % i, dma=True) for i in range(2)]
        t_ptr = [P.tok("ptrA%d" % i) for i in range(2)]
        t_pm = [P.tok("pmA%d" % i) for i in range(3)]
        t_scr = P.tok("scrA")

        load_cast_weight(k, w_in, k.l0["w_in"].rearrange("(kc p) n -> p kc n", p=128), t_win, 8)
        xv = k.x.rearrange("(g t p) d -> g p t d", p=128, t=4)
        idf = k.ident_f
        sec_col = [0, 512, 1536, 2048]
        sec_scale = [32 ** -0.5, 1.0, 64 ** -0.5, 1.0]
        v_col = [1024, 2560]
        ipm = 0
        iqt = 0
        itr = 0
        P.dma("sp", xs[0][:], xv[0], [], [t_x[0]], t_x[0])
        for g in range(8):
            b = g % 2
            if g + 1 < 8:
                P.dma("sp", xs[1 - b][:], xv[g + 1], [], [t_x[1 - b]], t_x[1 - b])
            for kc in range(KC):
                tb = itr % 2
                itr += 1
                for tt in range(4):
                    P.op("pe", lambda e, tb=tb, tt=tt, kc=kc, b=b: e.transpose(
                        ptr[tb][:, tt * 128:(tt + 1) * 128], xs[b][:, tt, kc * 128:(kc + 1) * 128], idf[:]),
                        reads=[t_x[b], k.t_ident], writes=[t_ptr[tb]], inc=(tt == 3))
                P.op("act", lambda e, tb=tb, kc=kc, b=b: e.activation(
                    out=hT[b][:, kc, :], in_=ptr[tb][:], func=AF.Identity,
                    scale=modT[:, 8 + kc:9 + kc], bias=modT[:, kc:kc + 1]),
                    reads=[t_ptr[tb], k.t_modT[0]], writes=[t_hT[b]])
            for sec in range(4):
                for f in range(4):
                    pb = ipm % 3
                    ipm += 1
                    c0 = sec_col[sec] + f * 128
                    for kc in range(KC):
                        P.op("pe", lambda e, pb=pb, kc=kc, c0=c0, b=b: e.matmul(
                            pm[pb][:], lhsT=w_in[:, kc, c0:c0 + 128], rhs=hT[b][:, kc, :],
                            start=(kc == 0), stop=(kc == KC - 1)),
                            reads=[t_win, t_hT[b]], writes=[t_pm[pb]], inc=(kc == KC - 1))
                    qb = iqt % 4
                    iqt += 1
                    sc = sec_scale[sec]
                    if (sec * 4 + f) % 2 == 0:
                        P.op("act", lambda e, qb=qb, pb=pb, sc=sc: e.activation(
                            out=qt[qb][:], in_=pm[pb][:], func=AF.Copy, scale=sc),
                            reads=[t_pm[pb]], writes=[t_qt[qb]])
                    else:
                        P.op("dve", lambda e, qb=qb, pb=pb, sc=sc: e.tensor_scalar_mul(
                            out=qt[qb][:], in0=pm[pb][:], scalar1=sc),
                            reads=[t_pm[pb]], writes=[t_qt[qb]])
                    r0 = sec * 512 + f * 128
                    P.dma("sp", k.qT_s[r0:r0 + 128, g * 512:(g + 1) * 512], qt[qb][:], [t_qt[qb]], [t_scr], t_qt[qb])
            for tt in range(4):
                vb = (g * 4 + tt) % 2
                for vs in range(2):
                    pb = ipm % 3
                    ipm += 1
                    c0 = v_col[vs]
                    for kc in range(KC):
                        P.op("pe", lambda e, pb=pb, kc=kc, c0=c0, b=b, tt=tt: e.matmul(
                            pm[pb][:], lhsT=hT[b][:, kc, tt * 128:(tt + 1) * 128], rhs=w_in[:, kc, c0:c0 + 512],
                            start=(kc == 0), stop=(kc == KC - 1)),
                            reads=[t_win, t_hT[b]], writes=[t_pm[pb]], inc=(kc == KC - 1))
                    if vs == 0:
                        P.op("act", lambda e, vb=vb, pb=pb: e.activation(
                            out=vsb[vb][:, 0:512], in_=pm[pb][:], func=AF.Copy),
                            reads=[t_pm[pb]], writes=[t_v[vb]])
                    else:
                        P.op("dve", lambda e, vb=vb, pb=pb: e.tensor_copy(
                            out=vsb[vb][:, 512:1024], in_=pm[pb][:]),
                            reads=[t_pm[pb]], writes=[t_v[vb]])
                t0 = g * 512 + tt * 128
                P.dma("sp", k.v_s[t0:t0 + 128, :], vsb[vb][:], [t_v[vb]], [t_scr], t_v[vb])
        dma_barrier(P, "sp", t_qt + t_v)
        if k.debug and "qT" in k.dbg:
            big = sb(k, ph, "dbgbig", [128, 16, 1024], BF16)
            t_big = P.tok("dbgbig", dma=True)
            for c4 in range(4):
                P.dma("sp", big[:], k.qT_s.rearrange("(a p) t -> p a t", p=128)[:, :, c4 * 1024:(c4 + 1) * 1024], [t_scr], [t_big], t_big)
                ev = P.dma("sp", k.dbg["qT"].rearrange("(a p) t -> p a t", p=128)[:, :, c4 * 1024:(c4 + 1) * 1024], big[:], [t_big], [], t_big)
            for c2 in range(2):
                P.dma("sp", big[:], k.v_s.rearrange("(a p) t -> p a t", p=128)[:, c2 * 16:(c2 + 1) * 16, :], [t_scr], [t_big], t_big)
                ev = P.dma("sp", k.dbg["v"].rearrange("(a p) t -> p a t", p=128)[:, c2 * 16:(c2 + 1) * 16, :], big[:], [t_big], [], t_big)
            P.out_events.append(ev)
        P.flush()


def attn_consts_alloc(k, ph):
    nc, P = k.nc, k.P
    aug = sb(k, ph, "aug", [128, 16, 512], BF16)
    t_aug = P.tok("aug", dma=True)
    bt = sb(k, ph, "bt", [128, 16, 32], F32)
    t_bt = P.tok("bt")
    negmask = sb(k, ph, "negmask", [128, 128], F32)
    t_negmask = P.tok("negmask")
    tm = sb(k, ph, "tm", [128, 2944], BF16)
    t_tm = P.tok("tm")
    zer = sb(k, ph, "zer", [1, 512], BF16)
    t_zer = P.tok("zer")
    neglam = sb(k, ph, "neglam", [128, 1], F32)
    t_neglam = P.tok("neglam")
    wsub = sb(k, ph, "wsub", [128, 64], F32)
    t_wsub = P.tok("wsub", dma=True)
    epsb = sb(k, ph, "epsb", [128, 1], F32)
    t_eps = P.tok("epsb")
    k.BC = dict(aug=aug, t_aug=t_aug, bt=bt, t_bt=t_bt, negmask=negmask, t_negmask=t_negmask, tm=tm, t_tm=t_tm, zer=zer, t_zer=t_zer, neglam=neglam, t_neglam=t_neglam, wsub=wsub, t_wsub=t_wsub, epsb=epsb, t_eps=t_eps)


def attn_consts_gen(k, tmp):
    nc, P = k.nc, k.P
    aug = k.BC['aug']
    t_aug = k.BC['t_aug']
    bt = k.BC['bt']
    t_bt = k.BC['t_bt']
    negmask = k.BC['negmask']
    t_negmask = k.BC['t_negmask']
    tm = k.BC['tm']
    t_tm = k.BC['t_tm']
    zer = k.BC['zer']
    t_zer = k.BC['t_zer']
    neglam = k.BC['neglam']
    t_neglam = k.BC['t_neglam']
    wsub = k.BC['wsub']
    t_wsub = k.BC['t_wsub']
    epsb = k.BC['epsb']
    t_eps = k.BC['t_eps']
    io = sb(k, tmp, "io", [16, 512], F32)
    slc = sb(k, tmp, "slc", [16, 1], F32)
    val = sb(k, tmp, "val", [16, 512], F32)
    hi = sb(k, tmp, "hi", [16, 512], BF16)
    mid = sb(k, tmp, "mid", [16, 512], BF16)
    lo = sb(k, tmp, "lo", [16, 512], BF16)
    hif = sb(k, tmp, "hif", [16, 512], F32)
    t_t = P.tok("augtmp")
    t_hi = P.tok("hi"); t_mid = P.tok("mid"); t_lo = P.tok("lo")
    P.op("pool", lambda e: e.iota(io[:], [[1, 512]], base=0, channel_multiplier=0,
                                  allow_small_or_imprecise_dtypes=True), writes=[t_t])
    P.op("pool", lambda e: e.iota(slc[:], [[1, 1]], base=1, channel_multiplier=1,
                                  allow_small_or_imprecise_dtypes=True), writes=[t_t])
    P.op("act", lambda e: e.activation(out=slc[:], in_=slc[:], func=AF.Exp, scale=-0.5 * math.log(2.0)),
         reads=[t_t], writes=[t_t])
    P.op("dve", lambda e: e.tensor_scalar(out=val[:], in0=io[:], scalar1=slc[:, 0:1], scalar2=-1.0,
                                          op0=ALU.mult, op1=ALU.mult), reads=[t_t], writes=[t_t])
    P.op("dve", lambda e: e.tensor_copy(out=hi[:], in_=val[:]), reads=[t_t], writes=[t_hi])
    P.op("dve", lambda e: e.tensor_copy(out=hif[:], in_=hi[:]), reads=[t_hi], writes=[t_t])
    P.op("dve", lambda e: e.tensor_sub(out=val[:], in0=val[:], in1=hif[:]), reads=[t_t], writes=[t_t])
    P.op("dve", lambda e: e.tensor_copy(out=mid[:], in_=val[:]), reads=[t_t], writes=[t_mid])
    P.op("dve", lambda e: e.tensor_copy(out=hif[:], in_=mid[:]), reads=[t_mid], writes=[t_t])
    P.op("dve", lambda e: e.tensor_sub(out=val[:], in0=val[:], in1=hif[:]), reads=[t_t], writes=[t_t])
    P.op("dve", lambda e: e.tensor_copy(out=lo[:], in_=val[:]), reads=[t_t], writes=[t_lo])
    t_augs = P.tok("aug_s")
    t_hi.sem = t_mid.sem = t_lo.sem = None
    t_hml = P.tok("hml", dma=True)
    P.dma("sp", k.aug_s[0], hi[:], [t_hi], [t_augs], t_hml)
    P.dma("sp", k.aug_s[1], mid[:], [t_mid], [t_augs], t_hml)
    P.dma("sp", k.aug_s[2], lo[:], [t_lo], [t_augs], t_hml)
    dma_barrier(P, "sp", [t_hml])
    for base in (32, 96, 64):
        for r in range(3):
            P.dma("sp", aug[base + r:base + r + 1], k.aug_s[r:r + 1], [t_augs], [t_aug], t_aug)
    bti = sb(k, tmp, "bti", [128, 32], F32)
    P.op("pool", lambda e: e.iota(bti[:], [[128, 32]], base=-28 * 128, channel_multiplier=1,
                                  allow_small_or_imprecise_dtypes=True), writes=[t_bt])
    for h in range(16):
        P.op("dve", lambda e, h=h: e.tensor_scalar_mul(out=bt[:, h, :], in0=bti[:], scalar1=SLOPES[h]),
             reads=[t_bt], writes=[t_bt])
    P.op("pool", lambda e: e.memset(negmask[:], 0.0), writes=[t_negmask])
    P.op("pool", lambda e: e.affine_select(out=negmask[:], in_=negmask[:], pattern=[[1, 128]],
                                           compare_op=ALU.is_ge, fill=-30000.0, base=0,
                                           channel_multiplier=-1),
         reads=[t_negmask], writes=[t_negmask])
    dl = sb(k, tmp, "dl", [128, 2944], I32)
    dlf = sb(k, tmp, "dlf", [128, 2944], F32)
    a_i = sb(k, tmp, "a_i", [128, 2944], I32)
    m1 = sb(k, tmp, "m1", [128, 2944], F32)
    m2 = sb(k, tmp, "m2", [128, 2944], F32)
    acc = sb(k, tmp, "acc", [128, 2944], F32)
    t_dl = P.tok("dl")
    P.op("pool", lambda e: e.iota(dl[:], [[1, 2944]], base=-384, channel_multiplier=-1), writes=[t_dl])
    P.op("dve", lambda e: e.tensor_copy(out=dlf[:], in_=dl[:]), reads=[t_dl], writes=[t_dl])
    P.op("dve", lambda e: e.tensor_scalar(out=m1[:], in0=dlf[:], scalar1=0.0, scalar2=None, op0=ALU.is_ge),
         reads=[t_dl], writes=[t_dl])
    P.op("dve", lambda e: e.tensor_scalar(out=m2[:], in0=dlf[:], scalar1=128.0, scalar2=None, op0=ALU.is_le),
         reads=[t_dl], writes=[t_dl])
    P.op("dve", lambda e: e.tensor_mul(out=acc[:], in0=m1[:], in1=m2[:]), reads=[t_dl], writes=[t_dl])
    for dd, win in ((4, 512.0), (16, 2048.0)):
        P.op("dve", lambda e, dd=dd: e.tensor_single_scalar(out=a_i[:], in_=dl[:], scalar=dd - 1,
                                                            op=ALU.bitwise_and),
             reads=[t_dl], writes=[t_dl])
        P.op("dve", lambda e: e.tensor_copy(out=m2[:], in_=a_i[:]), reads=[t_dl], writes=[t_dl])
        P.op("dve", lambda e: e.tensor_scalar(out=m2[:], in0=m2[:], scalar1=0.0, scalar2=None,
                                              op0=ALU.is_equal), reads=[t_dl], writes=[t_dl])
        P.op("dve", lambda e: e.tensor_mul(out=m2[:], in0=m2[:], in1=m1[:]), reads=[t_dl], writes=[t_dl])
        P.op("dve", lambda e, win=win: e.scalar_tensor_tensor(out=m2[:], in0=dlf[:], scalar=win, in1=m2[:],
                                                              op0=ALU.is_le, op1=ALU.mult),
             reads=[t_dl], writes=[t_dl])
        P.op("dve", lambda e: e.tensor_add(out=acc[:], in0=acc[:], in1=m2[:]), reads=[t_dl], writes=[t_dl])
    P.op("dve", lambda e: e.tensor_copy(out=tm[:], in_=acc[:]), reads=[t_dl], writes=[t_tm])
    P.op("dve", lambda e: e.memset(zer[:], 0.0), writes=[t_zer])
    P.op("dve", lambda e: e.memset(epsb[:], EPS), writes=[t_eps])
    lam4 = sb(k, tmp, "lam4", [128, 128], F32)
    t_lam = P.tok("lam4", dma=True)
    P.dma("sp", lam4[:], k.l0["lam"].partition_broadcast(128), [], [t_lam], t_lam)
    pr = sb(k, tmp, "lampr", [128, 2, 32], F32)
    s2 = sb(k, tmp, "lams2", [128, 2], F32)
    P.op("dve", lambda e: e.tensor_mul(out=pr[:, 0, :], in0=lam4[:, 0:32], in1=lam4[:, 32:64]),
         reads=[t_lam], writes=[t_neglam])
    P.op("dve", lambda e: e.tensor_mul(out=pr[:, 1, :], in0=lam4[:, 64:96], in1=lam4[:, 96:128]),
         reads=[t_lam], writes=[t_neglam])
    P.op("dve", lambda e: e.reduce_sum(out=s2[:], in_=pr[:], axis=AX.X), reads=[t_neglam], writes=[t_neglam])
    P.op("act", lambda e: e.activation(out=s2[:], in_=s2[:], func=AF.Exp), reads=[t_neglam], writes=[t_neglam])
    P.op("dve", lambda e: e.tensor_sub(out=neglam[:], in0=s2[:, 1:2], in1=s2[:, 0:1]),
         reads=[t_neglam], writes=[t_neglam])
    P.op("dve", lambda e: e.tensor_scalar_add(out=neglam[:], in0=neglam[:], scalar1=-LAM_INIT0),
         reads=[t_neglam], writes=[t_neglam])
    P.dma("sp", wsub[:], k.l0["subln_w"].partition_broadcast(128), [], [t_wsub], t_wsub)
    P.op("dve", lambda e: e.tensor_scalar_mul(out=wsub[:], in0=wsub[:], scalar1=1.0 - LAM_INIT0),
         reads=[t_wsub], writes=[t_wsub])


def phaseB(k):
    nc, P = k.nc, k.P
    aug = k.BC['aug']
    t_aug = k.BC['t_aug']
    bt = k.BC['bt']
    t_bt = k.BC['t_bt']
    negmask = k.BC['negmask']
    t_negmask = k.BC['t_negmask']
    tm = k.BC['tm']
    t_tm = k.BC['t_tm']
    zer = k.BC['zer']
    t_zer = k.BC['t_zer']
    neglam = k.BC['neglam']
    t_neglam = k.BC['t_neglam']
    wsub = k.BC['wsub']
    t_wsub = k.BC['t_wsub']
    epsb = k.BC['epsb']
    t_eps = k.BC['t_eps']
    with ExitStack() as ph:
        QT = [[sb(k, ph, "QT%d_%d" % (i, m), [128, S], BF16) for m in range(2)] for i in range(2)]
        KT = [sb(k, ph, "KT%d" % i, [128, S], BF16) for i in range(2)]
        VT = [sb(k, ph, "VT%d" % i, [128, NT, 65], BF16) for i in range(2)]
        PT = [sb(k, ph, "PT%d" % i, [128, 512], BF16) for i in range(6)]
        pst = [ps(k, ph, "pst%d" % i, [128, 512]) for i in range(3)]
        t_post = P.tok("post")
        pO = [[ps(k, ph, "pO%d_%d" % (i, m), [128, 512]) for m in range(2)] for i in range(2)]
        pTr = [ps(k, ph, "pTr%d" % m, [128, 512]) for m in range(1)] * 2
        t_pTr = [P.tok("pTr%d" % m) for m in range(1)] * 2
        mhalfB = sb(k, ph, "mhalfB", [128, 4], F32)
        P.op("pool", lambda e: e.memset(mhalfB[:], -0.5), writes=[t_post])
        OTs = [sb(k, ph, "OTs%d" % m, [65, 512], F32) for m in range(2)]
        t_OTs = [P.tok("OTs%d" % m) for m in range(2)]
        idf65 = k.ident_f[0:65, 0:65]
        odf = sb(k, ph, "odf", [128, 4, 64], F32)
        otmp = sb(k, ph, "otmp", [128, 4, 64], F32)
        rr = sb(k, ph, "rr", [128, 8], F32)
        ss = sb(k, ph, "ss", [128, 4], F32)
        ob = [sb(k, ph, "ob%d" % i, [128, 4, 64], BF16) for i in range(2)]
        t_Q = [P.tok("QT%d" % i, dma=True) for i in range(2)]
        t_K = [P.tok("KT%d" % i, dma=True) for i in range(2)]
        t_V = [P.tok("VT%d" % i, dma=True) for i in range(2)]
        t_PT = [P.tok("PT%d" % i) for i in range(6)]
        t_pst = [P.tok("pst%d" % i) for i in range(3)]
        t_pO = [[P.tok("pO%d_%d" % (i, m)) for m in range(2)] for i in range(2)]
        t_ob = [P.tok("ob%d" % i, dma=True) for i in range(2)]
        t_os = P.tok("o_s")
        for i in range(2):
            P.op("pool", lambda e, i=i: e.memset(VT[i][:, :, 64:65], 1.0), writes=[t_V[i]])
            P.op("pool", lambda e, i=i: e.memset(KT[i][:], 1.0), writes=[t_K[i]])
            for m in range(2):
                P.op("pool", lambda e, i=i, m=m: e.memset(QT[i][m][:], 0.0), writes=[t_Q[i]])

        NPT = len(PT)
        LA = NPT - 2

        def emit_loads(hh):
            diff = hh < 8
            h = hh if diff else hh - 8
            b = hh % 2
            if diff:
                for m in range(2):
                    r0 = h * 64 + m * 32
                    P.dma("sp", QT[b][m][64 * m:64 * m + 32, :], k.qT_s[r0:r0 + 32, :], [], [t_Q[b]], t_Q[b])
                    P.dma("sp", KT[b][64 * m:64 * m + 32, :], k.qT_s[512 + r0:512 + r0 + 32, :], [], [t_K[b]], t_K[b])
                for base in (32, 96):
                    P.op("pool", lambda e, b=b, hh=hh, base=base: e.tensor_copy(
                        out=QT[b][base // 64][base:base + 3, :].rearrange("p (c j) -> p c j", j=512),
                        in_=aug[base:base + 3, hh, :].unsqueeze(1).broadcast_to([3, 8, 512])),
                        reads=[t_aug], writes=[t_Q[b]])
                    P.op("pool", lambda e, b=b, base=base: e.memset(KT[b][base:base + 3, :], 1.0), writes=[t_K[b]])
                vcol = h * 64
            else:
                r0 = 1024 + h * 64
                P.dma("sp", QT[b][0][0:64, :], k.qT_s[r0:r0 + 64, :], [], [t_Q[b]], t_Q[b])
                P.dma("sp", KT[b][0:64, :], k.qT_s[512 + r0:512 + r0 + 64, :], [], [t_K[b]], t_K[b])
                P.op("pool", lambda e, b=b, hh=hh: e.tensor_copy(
                    out=QT[b][0][64:67, :].rearrange("p (c j) -> p c j", j=512),
                    in_=aug[64:67, hh, :].unsqueeze(1).broadcast_to([3, 8, 512])),
                    reads=[t_aug], writes=[t_Q[b]])
                P.op("pool", lambda e, b=b: e.memset(KT[b][64:67, :], 1.0), writes=[t_K[b]])
                vcol = 512 + h * 64
            P.dma("sp", VT[b][:, :, 0:64], k.v_s[:, vcol:vcol + 64].rearrange("(n p) d -> p n d", p=128),
                  [], [t_V[b]], t_V[b])

        tiles = []
        for hh in range(16):
            diff = hh < 8
            nm = 2 if diff else 1
            for qc in range(8):
                kb_lo = 0 if diff else max(0, 4 * qc - 16)
                lst = [(kb, m) for kb in range(kb_lo, 4 * qc + 4) for m in range(nm)]
                for idx, (kb, m) in enumerate(lst):
                    tiles.append(dict(hh=hh, qc=qc, kb=kb, m=m, first=(idx == 0), last=(idx == len(lst) - 1),
                                      head_first=(qc == 0 and idx == 0)))

        st = dict(ist=0, ipt=0, iob=0, cpar=0)

        def stage1(T):
            hh, qc, kb, m = T["hh"], T["qc"], T["kb"], T["m"]
            diff = hh < 8
            b = hh % 2
            kr = 35 if diff else 67
            j = kb - 4 * qc
            n0 = max(0, j)
            pb = 64 * m
            si = st["ist"] % len(pst)
            st["ist"] += 1
            P.op("pe", lambda e, si=si, b=b, m=m, kb=kb, qc=qc, n0=n0: e.matmul(
                pst[si][:, n0 * 128:512], lhsT=KT[b][:, kb * 128:(kb + 1) * 128],
                rhs=QT[b][m][:, qc * 512 + n0 * 128:(qc + 1) * 512], start=True, stop=True),
                reads=[t_K[b], t_Q[b]], writes=[t_pst[si]])
            if j >= 0 and diff:
                P.op("dve", lambda e, si=si, j=j: e.tensor_tensor(
                    out=pst[si][:, j * 128:(j + 1) * 128], in0=pst[si][:, j * 128:(j + 1) * 128],
                    in1=negmask[:], op=ALU.add),
                    reads=[t_negmask], writes=[t_pst[si]])
            pi = st["ipt"] % NPT
            st["ipt"] += 1
            T["pi"] = pi
            P.op("act", lambda e, si=si, pi=pi, n0=n0, hh=hh, j=j: e.activation(
                out=PT[pi][:, n0 * 128:512], in_=pst[si][:, n0 * 128:512], func=AF.Exp,
                bias=bt[:, hh, j + 28:j + 29]),
                reads=[t_pst[si], t_bt], writes=[t_PT[pi]])
            if not diff:
                x0 = (4 * qc - kb + 3) * 128 + n0 * 128
                eng = "pool" if (st["ipt"] % 2 == 0) else "dve"
                P.op(eng, lambda e, pi=pi, n0=n0, x0=x0: e.tensor_tensor(
                    out=PT[pi][:, n0 * 128:512], in0=PT[pi][:, n0 * 128:512],
                    in1=tm[:, x0:x0 + 512 - n0 * 128], op=ALU.mult),
                    reads=[t_tm], writes=[t_PT[pi]])

        def stage2(T):
            hh, qc, kb, m, pi = T["hh"], T["qc"], T["kb"], T["m"], T["pi"]
            diff = hh < 8
            b = hh % 2
            if T["first"]:
                st["cpar"] ^= 1
            ob_i = st["cpar"]
            n0 = max(0, kb - 4 * qc)
            kb_lo = 0 if diff else max(0, 4 * qc - 16)
            P.op("pe", lambda e, pi=pi, n0=n0, ob_i=ob_i, m=m, b=b, kb=kb, first=(kb == kb_lo): e.matmul(
                pO[ob_i][m][0:65, n0 * 128:512], lhsT=VT[b][:, kb, :], rhs=PT[pi][:, n0 * 128:512],
                start=first, stop=True, skip_group_check=True),
                reads=[t_PT[pi], t_V[b]], writes=[t_pO[ob_i][m]])
            if T["last"]:
                post(hh, qc, ob_i)

        def post_a(hh, qc, ob_i):
            nm = 2 if hh < 8 else 1
            for m in range(nm):
                P.op("dve", lambda e, ob_i=ob_i, m=m: e.tensor_copy(out=OTs[m][:], in_=pO[ob_i][m][0:65, :]),
                     reads=[t_pO[ob_i][m]], writes=[t_OTs[m]])

        def tr_map(m):
            for sub in range(4):
                P.op("pe", lambda e, m=m, sub=sub: e.transpose(
                    pTr[0][:, sub * 65:(sub + 1) * 65], OTs[m][:, sub * 128:(sub + 1) * 128], idf65),
                    reads=[t_OTs[m], k.t_ident], writes=[t_pTr[0]], inc=(sub == 3))

        def post_b(hh, qc):
            diff = hh < 8
            O0 = pTr[0][:, 0:260].rearrange("p (s c) -> p s c", c=65)
            obi = st["iob"] % 2
            st["iob"] += 1
            if diff:
                rd = [t_pTr[0], t_neglam, t_wsub, t_eps]
                tr_map(1)
                P.op("dve", lambda e: e.reciprocal(out=rr[:, 4:8], in_=O0[:, :, 64]), reads=rd, writes=[t_post])
                P.op("dve", lambda e: e.tensor_scalar_mul(out=rr[:, 4:8], in0=rr[:, 4:8], scalar1=neglam[:, 0:1]),
                     reads=rd, writes=[t_post])
                for sub in range(4):
                    P.op("dve", lambda e, sub=sub: e.tensor_scalar_mul(
                        out=otmp[:, sub, :], in0=O0[:, sub, 0:64], scalar1=rr[:, 4 + sub:5 + sub]),
                        reads=rd, writes=[t_post])
                tr_map(0)
                P.op("dve", lambda e: e.reciprocal(out=rr[:, 0:4], in_=O0[:, :, 64]), reads=rd, writes=[t_post])
                for sub in range(4):
                    P.op("dve", lambda e, sub=sub: e.scalar_tensor_tensor(
                        out=odf[:, sub, :], in0=O0[:, sub, 0:64], scalar=rr[:, sub:sub + 1], in1=otmp[:, sub, :],
                        op0=ALU.mult, op1=ALU.add), reads=rd, writes=[t_post])
                P.op("dve", lambda e: e.tensor_mul(out=otmp[:], in0=odf[:], in1=odf[:]), reads=rd, writes=[t_post])
                P.op("dve", lambda e: e.reduce_sum(out=ss[:], in_=otmp[:], axis=AX.X), reads=rd, writes=[t_post])
                P.op("dve", lambda e: e.tensor_scalar(out=ss[:], in0=ss[:], scalar1=1.0 / 64.0, scalar2=EPS,
                                                      op0=ALU.mult, op1=ALU.add), reads=rd, writes=[t_post])
                P.op("pool", lambda e: e.tensor_tensor(out=ss[:], in0=ss[:], in1=mhalfB[:], op=ALU.pow),
                     reads=[t_post], writes=[t_post])
                for sub in range(4):
                    P.op("dve", lambda e, sub=sub, obi=obi: e.scalar_tensor_tensor(
                        out=ob[obi][:, sub, :], in0=odf[:, sub, :], scalar=ss[:, sub:sub + 1], in1=wsub[:],
                        op0=ALU.mult, op1=ALU.mult), reads=[t_post, t_wsub], writes=[t_ob[obi]])
            else:
                tr_map(0)
                rd = [t_pTr[0]]
                P.op("dve", lambda e: e.reciprocal(out=rr[:, 0:4], in_=O0[:, :, 64]), reads=rd, writes=[t_post])
                for sub in range(4):
                    P.op("dve", lambda e, sub=sub, obi=obi: e.tensor_scalar_mul(
                        out=ob[obi][:, sub, :], in0=O0[:, sub, 0:64], scalar1=rr[:, sub:sub + 1]),
                        reads=rd + [t_post], writes=[t_ob[obi]])
            ocol = hh * 64
            P.dma("sp", k.o_s[qc * 512:(qc + 1) * 512, ocol:ocol + 64].rearrange("(s p) d -> p s d", p=128),
                  ob[obi][:], [t_ob[obi]], [t_os], t_ob[obi])

        pending = []

        def post(hh, qc, ob_i):
            while pending:
                _, h2, q2 = pending.pop(0)
                post_b(h2, q2)
            post_a(hh, qc, ob_i)
            pending.append([3, hh, qc])

        def tick():
            if pending:
                pending[0][0] -= 1
                if pending[0][0] <= 0:
                    _, h2, q2 = pending.pop(0)
                    post_b(h2, q2)

        emit_loads(0)
        emit_loads(1)
        for step in range(len(tiles) + LA):
            if step < len(tiles):
                stage1(tiles[step])
            if step >= LA:
                U = tiles[step - LA]
                if U["head_first"] and U["hh"] >= 1 and U["hh"] + 1 < 16:
                    emit_loads(U["hh"] + 1)
                tick()
                stage2(U)
        while pending:
            _, h2, q2 = pending.pop(0)
            post_b(h2, q2)
        dma_barrier(P, "sp", t_ob)
        if k.debug and "o" in k.dbg:
            big = sb(k, ph, "dbgbigB", [128, 16, 1024], BF16)
            t_big = P.tok("dbgbigB", dma=True)
            for c2 in range(2):
                P.dma("sp", big[:], k.o_s.rearrange("(a p) t -> p a t", p=128)[:, c2 * 16:(c2 + 1) * 16, :], [t_os], [t_big], t_big)
                ev = P.dma("sp", k.dbg["o"].rearrange("(a p) t -> p a t", p=128)[:, c2 * 16:(c2 + 1) * 16, :], big[:], [t_big], [], t_big)
            P.out_events.append(ev)
        P.flush()


class LNScratch:
    def __init__(self, k, stack, name):
        P = k.P
        self.stats = sb(k, stack, name + "_st", [128, 2, 6], F32)
        self.mv = sb(k, stack, name + "_mv", [128, 2], F32)
        self.rstd = sb(k, stack, name + "_rs", [128, 1], F32)
        self.nmr = sb(k, stack, name + "_nm", [128, 1], F32)
        self.mhalf = sb(k, stack, name + "_mh", [128, 1], F32)
        self.t = P.tok(name + "_ln")
        mh = self.mhalf
        P.op("pool", lambda e: e.memset(mh[:], -0.5), writes=[self.t])


def emit_ln(k, r, t_r, L):
    P = k.P
    for hf in range(2):
        P.op("dve", lambda e, hf=hf: e.bn_stats(out=L.stats[:, hf, :], in_=r[:, hf * 512:(hf + 1) * 512]),
             reads=[t_r], writes=[L.t])
    P.op("dve", lambda e: e.bn_aggr(out=L.mv[:], in_=L.stats[:].rearrange("p a b -> p (a b)")), reads=[L.t], writes=[L.t])
    P.op("dve", lambda e: e.tensor_scalar_add(out=L.rstd[:], in0=L.mv[:, 1:2], scalar1=EPS), reads=[L.t], writes=[L.t])
    P.op("pool", lambda e: e.tensor_tensor(out=L.rstd[:], in0=L.rstd[:], in1=L.mhalf[:], op=ALU.pow),
         reads=[L.t], writes=[L.t])
    P.op("dve", lambda e: e.scalar_tensor_tensor(out=L.nmr[:], in0=L.mv[:, 0:1], scalar=-1.0, in1=L.rstd[:],
                                                 op0=ALU.mult, op1=ALU.mult), reads=[L.t], writes=[L.t])
    P.op("act", lambda e: e.activation(out=r, in_=r, func=AF.Identity, scale=L.rstd[:, 0:1], bias=L.nmr[:, 0:1]),
         reads=[t_r, L.t], writes=[t_r])


def load_bc(k, dst, src_row, tok):
    k.P.dma("sp", dst, src_row.partition_broadcast(128), [], [tok], tok)


def phaseC(k):
    nc, P = k.nc, k.P
    prm = k.l0
    modT = k.modT[0]
    gbc = k.gbc
    with ExitStack() as wsc:
        w_gate = sb(k, wsc, "w_gate_sb", [128, KC, DFF], BF16)
        w_up = sb(k, wsc, "w_up_sb", [128, KC, DFF], BF16)
        t_wg = P.tok("w_gate", dma=True)
        t_wu = P.tok("w_up", dma=True)
        t_wd = P.tok("w_down", dma=True)
        load_cast_weight(k, w_gate, prm["w_gate"].rearrange("(kc p) n -> p kc n", p=128), t_wg, 4)
        load_cast_weight(k, w_up, prm["w_up"].rearrange("(kc p) n -> p kc n", p=128), t_wu, 4)
        with ExitStack() as ph:
            w_out = sb(k, ph, "w_out_sb", [128, KC, D], BF16)
            wst = sb(k, ph, "w_out_st", [128, 2, D], F32)
            t_wo = P.tok("w_out")
            t_wst = P.tok("w_out_st", dma=True)
            wov = prm["w_out"].rearrange("(kc p) n -> p kc n", p=128)
            for c in range(4):
                P.dma("sp", wst[:], wov[:, 2 * c:2 * c + 2, :], [], [t_wst], t_wst)
                for i in range(2):
                    P.op("dve", lambda e, c=c, i=i: e.tensor_mul(out=w_out[:, 2 * c + i, :], in0=wst[:, i, :],
                                                                in1=gbc[:, 0, :]),
                         reads=[t_wst, k.t_gbc], writes=[t_wo])
            lng = sb(k, ph, "ln1g", [128, D], F32)
            lnb = sb(k, ph, "ln1b", [128, D], F32)
            t_lng = P.tok("ln1g", dma=True)
            t_lnb = P.tok("ln1b", dma=True)
            load_bc(k, lng[:], prm["ln1_g"], t_lng)
            load_bc(k, lnb[:], prm["ln1_b"], t_lnb)
            gcol = sb(k, ph, "gcol", [128, 2, KC], F32)
            t_gcol = P.tok("gcol", dma=True)
            P.dma("sp", gcol[:, 0, :], prm["ln1_g"].rearrange("o (kc p) -> p (o kc)", p=128), [], [t_gcol], t_gcol,
                  allow_slow_non_contiguous=True)
            P.dma("sp", gcol[:, 1, :], prm["ln1_b"].rearrange("o (kc p) -> p (o kc)", p=128), [], [t_gcol], t_gcol,
                  allow_slow_non_contiguous=True)
            AB = sb(k, ph, "AB2", [128, 2, KC], F32)
            t_AB = P.tok("AB2")
            P.op("dve", lambda e: e.tensor_mul(out=AB[:, 0, :], in0=gcol[:, 0, :], in1=modT[:, 32:40]),
                 reads=[t_gcol, k.t_modT[0]], writes=[t_AB])
            P.op("dve", lambda e: e.tensor_mul(out=AB[:, 1, :], in0=gcol[:, 1, :], in1=modT[:, 32:40]),
                 reads=[t_gcol, k.t_modT[0]], writes=[t_AB])
            P.op("dve", lambda e: e.tensor_add(out=AB[:, 1, :], in0=AB[:, 1, :], in1=modT[:, 24:32]),
                 reads=[k.t_modT[0]], writes=[t_AB])
            L = LNScratch(k, ph, "c1")
            xs = None
            og = [sb(k, ph, "ogC%d" % i, [128, 4, D], BF16) for i in range(1)] * 2
            oT = sb(k, ph, "oT", [128, KC, 512], BF16)
            r1 = [sb(k, ph, "r1_%d" % i, [128, D], F32) for i in range(2)]
            x1o = [sb(k, ph, "x1o_%d" % i, [128, D], F32) for i in range(2)]
            h2T = [sb(k, ph, "h2TC%d" % i, [128, KC, 512], BF16) for i in range(1)] * 2
            ptb = [ps(k, ph, "ptbC%d" % i, [128, 512], BF16) for i in range(2)]
            ptf = [ps(k, ph, "ptfC%d" % i, [128, 512]) for i in range(2)]
            py = [ps(k, ph, "pyC%d" % i, [128, 512]) for i in range(4)]
            t_x = None
            t_og = [P.tok("ogC%d" % i, dma=True) for i in range(1)] * 2
            t_oT = P.tok("oT")
            t_r1 = [P.tok("r1_%d" % i) for i in range(2)]
            t_x1o = [P.tok("x1o_%d" % i, dma=True) for i in range(2)]
            t_h2T = [P.tok("h2TC%d" % i, dma=True) for i in range(1)] * 2
            t_ptb = [P.tok("ptbC%d" % i) for i in range(2)]
            t_ptf = [P.tok("ptfC%d" % i) for i in range(2)]
            t_py = [P.tok("pyC%d" % i) for i in range(4)]
            t_scr = P.tok("scrC1")
            xv = k.x.rearrange("(g t p) d -> g p t d", p=128, t=4)
            ov = k.o_s.rearrange("(g t p) d -> g p t d", p=128, t=4)
            idf, idb = k.ident_f, k.ident_b
            xt3 = [sb(k, ph, "xtC%d" % i, [128, D], F32) for i in range(3)]
            t_xt3 = [P.tok("xtC%d" % i, dma=True) for i in range(3)]
            r1x = sb(k, ph, "r1_2", [128, D], F32)
            r1 = r1 + [r1x]
            t_r1 = t_r1 + [P.tok("r1_2")]
            cst = dict(itb=0, itf=0, ipy=0)
            pys_of = {}

            def G1(g):
                P.dma("sp", og[0][:], ov[g], [], [t_og[0]], t_og[0])
                for kc in range(KC):
                    tb = cst["itb"] % 2
                    cst["itb"] += 1
                    for tt in range(4):
                        P.op("pe", lambda e, tb=tb, tt=tt, kc=kc: e.transpose(
                            ptb[tb][:, tt * 128:(tt + 1) * 128], og[0][:, tt, kc * 128:(kc + 1) * 128], idb[:]),
                            reads=[t_og[0], k.t_ident], writes=[t_ptb[tb]], inc=(tt == 3))
                    if kc % 2 == 0:
                        P.op("act", lambda e, tb=tb, kc=kc: e.activation(out=oT[:, kc, :], in_=ptb[tb][:], func=AF.Copy),
                             reads=[t_ptb[tb]], writes=[t_oT])
                    else:
                        P.op("dve", lambda e, tb=tb, kc=kc: e.tensor_copy(out=oT[:, kc, :], in_=ptb[tb][:]),
                             reads=[t_ptb[tb]], writes=[t_oT])

            def S1(i):
                g, tt = i // 4, i % 4
                xb = i % 3
                t0 = i * 128
                P.dma("sp", xt3[xb][:], k.x[t0:t0 + 128, :], [], [t_xt3[xb]], t_xt3[xb])
                pys = []
                for dh in range(2):
                    pb = cst["ipy"] % 4
                    cst["ipy"] += 1
                    pys.append(pb)
                    for kc in range(KC):
                        P.op("pe", lambda e, pb=pb, kc=kc, tt=tt, dh=dh: e.matmul(
                            py[pb][:], lhsT=oT[:, kc, tt * 128:(tt + 1) * 128], rhs=w_out[:, kc, dh * 512:(dh + 1) * 512],
                            start=(kc == 0), stop=(kc == KC - 1)),
                            reads=[t_oT, t_wo], writes=[t_py[pb]], inc=(kc == KC - 1))
                pys_of[i] = pys

            def S2(i):
                rb = i % 3
                xb = i % 3
                ob_ = i % 2
                pys = pys_of[i]
                for dh in range(2):
                    pb = pys[dh]
                    P.op("dve", lambda e, pb=pb, dh=dh, rb=rb, xb=xb: e.scalar_tensor_tensor(
                        out=r1[rb][:, dh * 512:(dh + 1) * 512], in0=xt3[xb][:, dh * 512:(dh + 1) * 512],
                        scalar=DN_ALPHA, in1=py[pb][:], op0=ALU.mult, op1=ALU.add),
                        reads=[t_xt3[xb], t_py[pb]], writes=[t_r1[rb]])
                emit_ln(k, r1[rb][:], t_r1[rb], L)
                P.op("pool", lambda e, rb=rb, ob_=ob_: e.tensor_tensor(out=x1o[ob_][:], in0=r1[rb][:], in1=lng[:], op=ALU.mult),
                     reads=[t_r1[rb], t_lng], writes=[t_x1o[ob_]])
                P.op("pool", lambda e, ob_=ob_: e.tensor_tensor(out=x1o[ob_][:], in0=x1o[ob_][:], in1=lnb[:], op=ALU.add),
                     reads=[t_lnb], writes=[t_x1o[ob_]])
                t0 = i * 128
                P.dma("sp", k.x1_s[t0:t0 + 128, :], x1o[ob_][:], [t_x1o[ob_]], [t_scr], t_x1o[ob_])

            def S3(i):
                g, tt = i // 4, i % 4
                rb = i % 3
                for k4 in range(2):
                    tf = cst["itf"] % 2
                    cst["itf"] += 1
                    for q in range(4):
                        kc = k4 * 4 + q
                        P.op("pe", lambda e, tf=tf, kc=kc, q=q, rb=rb: e.transpose(
                            ptf[tf][:, q * 128:(q + 1) * 128], r1[rb][:, kc * 128:(kc + 1) * 128], idf[:]),
                            reads=[t_r1[rb], k.t_ident], writes=[t_ptf[tf]], inc=(q == 3))
                    for q in range(4):
                        kc = k4 * 4 + q
                        P.op("act", lambda e, tf=tf, kc=kc, q=q, tt=tt: e.activation(
                            out=h2T[0][:, kc, tt * 128:(tt + 1) * 128], in_=ptf[tf][:, q * 128:(q + 1) * 128],
                            func=AF.Identity, scale=AB[:, 0, kc:kc + 1], bias=AB[:, 1, kc:kc + 1]),
                            reads=[t_ptf[tf], t_AB], writes=[t_h2T[0]])
                if tt == 3:
                    P.dma("sp", k.h2T_s.rearrange("(kc p) t -> p kc t", p=128)[:, :, g * 512:(g + 1) * 512], h2T[0][:],
                          [t_h2T[0]], [t_scr], t_h2T[0])

            G1(0)
            S1(0)
            for i in range(32):
                if i + 1 < 32:
                    if (i + 1) % 4 == 0:
                        G1((i + 1) // 4)
                    S1(i + 1)
                S2(i)
                if i >= 1:
                    S3(i - 1)
            S3(31)
            dma_barrier(P, "sp", t_x1o + t_h2T[:1])
            if k.debug and "x1" in k.dbg:
                big = sb(k, ph, "dbgbigC", [128, 4, 1024], F32)
                t_big = P.tok("dbgbigC", dma=True)
                for c8 in range(8):
                    P.dma("sp", big[:], k.x1_s.rearrange("(a p) t -> p a t", p=128)[:, c8 * 4:(c8 + 1) * 4, :], [t_scr], [t_big], t_big)
                    ev = P.dma("sp", k.dbg["x1"].rearrange("(a p) t -> p a t", p=128)[:, c8 * 4:(c8 + 1) * 4, :], big[:], [t_big], [], t_big)
                P.out_events.append(ev)
            P.flush()
        with ExitStack() as ph:
            w_down = sb(k, ph, "w_down_sb", [128, FC, D], BF16)
            load_cast_weight(k, w_down, prm["w_down"].rearrange("(fc p) n -> p fc n", p=128), t_wd, 11)
            lng = sb(k, ph, "ln2g", [128, D], F32)
            lnb = sb(k, ph, "ln2b", [128, D], F32)
            t_lng = P.tok("ln2g", dma=True)
            t_lnb = P.tok("ln2b", dma=True)
            load_bc(k, lng[:], prm["ln2_g"], t_lng)
            load_bc(k, lnb[:], prm["ln2_b"], t_lnb)
            L = LNScratch(k, ph, "c2")
            h2T = sb(k, ph, "h2T2", [128, KC, 512], BF16)
            actT = sb(k, ph, "actT", [128, FC, 512], BF16)
            sg = [sb(k, ph, "sg%d" % i, [128, 512], BF16) for i in range(2)]
            x1t = [sb(k, ph, "x1t%d" % i, [128, D], F32) for i in range(2)]
            r2 = [sb(k, ph, "r2_%d" % i, [128, D], F32) for i in range(2)]
            pg = [ps(k, ph, "pgC%d" % i, [128, 512]) for i in range(2)]
            pu = [ps(k, ph, "puC%d" % i, [128, 512]) for i in range(2)]
            py = [ps(k, ph, "py2C%d" % i, [128, 512]) for i in range(4)]
            t_h2T = P.tok("h2T2", dma=True)
            t_actT = P.tok("actT")
            t_sg = [P.tok("sg%d" % i) for i in range(2)]
            t_x1t = [P.tok("x1t%d" % i, dma=True) for i in range(2)]
            t_r2 = [P.tok("r2_%d" % i, dma=True) for i in range(2)]
            t_pg = [P.tok("pg%d" % i) for i in range(2)]
            t_pu = [P.tok("pu%d" % i) for i in range(2)]
            t_py = [P.tok("py2_%d" % i) for i in range(4)]
            t_scr2 = P.tok("scrC2")
            ipy = 0
            P.dma("sp", h2T[:], k.h2T_s.rearrange("(kc p) t -> p kc t", p=128)[:, :, 0:512], [], [t_h2T], t_h2T)
            for g in range(8):
                for fc in range(FC):
                    b = fc % 2
                    for kc in range(KC):
                        P.op("pe", lambda e, b=b, kc=kc, fc=fc: e.matmul(
                            pg[b][:], lhsT=w_gate[:, kc, fc * 128:(fc + 1) * 128], rhs=h2T[:, kc, :],
                            start=(kc == 0), stop=(kc == KC - 1)),
                            reads=[t_wg, t_h2T], writes=[t_pg[b]], inc=(kc == KC - 1))
                    for kc in range(KC):
                        P.op("pe", lambda e, b=b, kc=kc, fc=fc: e.matmul(
                            pu[b][:], lhsT=w_up[:, kc, fc * 128:(fc + 1) * 128], rhs=h2T[:, kc, :],
                            start=(kc == 0), stop=(kc == KC - 1)),
                            reads=[t_wu, t_h2T], writes=[t_pu[b]], inc=(kc == KC - 1))
                    P.op("act", lambda e, b=b: e.activation(out=sg[b][:], in_=pg[b][:], func=AF.Silu),
                         reads=[t_pg[b]], writes=[t_sg[b]])
                    P.op("dve", lambda e, b=b, fc=fc: e.tensor_tensor(out=actT[:, fc, :], in0=sg[b][:], in1=pu[b][:],
                                                                     op=ALU.mult),
                         reads=[t_sg[b], t_pu[b]], writes=[t_actT])
                if g + 1 < 8:
                    P.dma("sp", h2T[:], k.h2T_s.rearrange("(kc p) t -> p kc t", p=128)[:, :, (g + 1) * 512:(g + 2) * 512],
                          [], [t_h2T], t_h2T)
                for tt in range(4):
                    rb = (g * 4 + tt) % 2
                    t0 = g * 512 + tt * 128
                    P.dma("sp", x1t[rb][:], k.x1_s[t0:t0 + 128, :], [], [t_x1t[rb]], t_x1t[rb])
                    pys = []
                    for dh in range(2):
                        pb = ipy % 4
                        ipy += 1
                        pys.append(pb)
                        for fc in range(FC):
                            P.op("pe", lambda e, pb=pb, fc=fc, tt=tt, dh=dh: e.matmul(
                                py[pb][:], lhsT=actT[:, fc, tt * 128:(tt + 1) * 128], rhs=w_down[:, fc, dh * 512:(dh + 1) * 512],
                                start=(fc == 0), stop=(fc == FC - 1)),
                                reads=[t_actT, t_wd], writes=[t_py[pb]], inc=(fc == FC - 1))
                    for dh in range(2):
                        pb = pys[dh]
                        P.op("dve", lambda e, pb=pb, dh=dh, rb=rb: e.tensor_tensor(
                            out=r2[rb][:, dh * 512:(dh + 1) * 512], in0=py[pb][:], in1=gbc[:, 1, dh * 512:(dh + 1) * 512],
                            op=ALU.mult), reads=[t_py[pb], k.t_gbc], writes=[t_r2[rb]])
                        P.op("dve", lambda e, dh=dh, rb=rb: e.scalar_tensor_tensor(
                            out=r2[rb][:, dh * 512:(dh + 1) * 512], in0=x1t[rb][:, dh * 512:(dh + 1) * 512],
                            scalar=DN_ALPHA, in1=r2[rb][:, dh * 512:(dh + 1) * 512], op0=ALU.mult, op1=ALU.add),
                            reads=[t_x1t[rb]], writes=[t_r2[rb]])
                    emit_ln(k, r2[rb][:], t_r2[rb], L)
                    P.op("pool", lambda e, rb=rb: e.tensor_tensor(out=r2[rb][:], in0=r2[rb][:], in1=lng[:], op=ALU.mult),
                         reads=[t_lng], writes=[t_r2[rb]])
                    P.op("pool", lambda e, rb=rb: e.tensor_tensor(out=r2[rb][:], in0=r2[rb][:], in1=lnb[:], op=ALU.add),
                         reads=[t_lnb], writes=[t_r2[rb]])
                    P.dma("sp", k.x2_s[t0:t0 + 128, :], r2[rb][:], [t_r2[rb]], [t_scr2], t_r2[rb])
            dma_barrier(P, "sp", t_r2)
            if k.debug and "x2" in k.dbg:
                big = sb(k, ph, "dbgbigC2", [128, 2, 1024], F32)
                t_big = P.tok("dbgbigC2", dma=True)
                for c8 in range(16):
                    P.dma("sp", big[:], k.x2_s.rearrange("(a p) t -> p a t", p=128)[:, c8 * 2:(c8 + 1) * 2, :], [t_scr2], [t_big], t_big)
                    ev = P.dma("sp", k.dbg["x2"].rearrange("(a p) t -> p a t", p=128)[:, c8 * 2:(c8 + 1) * 2, :], big[:], [t_big], [], t_big)
                P.out_events.append(ev)
            P.flush()


TWO_PI = 2.0 * math.pi


def emit_sin(k, out, x, tmpf, tmpi, tok, shift=0.0):
    P = k.P
    rd = [tok]
    P.op("dve", lambda e: e.tensor_scalar(out=out, in0=x, scalar1=shift, scalar2=1.0 / TWO_PI, op0=ALU.add, op1=ALU.mult),
         reads=rd, writes=[tok])
    P.op("dve", lambda e: e.tensor_copy(out=tmpi, in_=out), reads=rd, writes=[tok])
    P.op("dve", lambda e: e.tensor_copy(out=tmpf, in_=tmpi), reads=rd, writes=[tok])
    P.op("dve", lambda e: e.scalar_tensor_tensor(out=out, in0=tmpf, scalar=-TWO_PI, in1=x, op0=ALU.mult, op1=ALU.add),
         reads=rd, writes=[tok])
    if shift != 0.0:
        P.op("dve", lambda e: e.tensor_scalar_add(out=out, in0=out, scalar1=shift), reads=rd, writes=[tok])
    P.op("dve", lambda e: e.tensor_single_scalar(out=tmpf, in_=out, scalar=math.pi, op=ALU.is_gt), reads=rd, writes=[tok])
    P.op("dve", lambda e: e.scalar_tensor_tensor(out=out, in0=tmpf, scalar=-TWO_PI, in1=out, op0=ALU.mult, op1=ALU.add),
         reads=rd, writes=[tok])
    P.op("dve", lambda e: e.tensor_single_scalar(out=tmpf, in_=out, scalar=-math.pi, op=ALU.is_lt), reads=rd, writes=[tok])
    P.op("dve", lambda e: e.scalar_tensor_tensor(out=out, in0=tmpf, scalar=TWO_PI, in1=out, op0=ALU.mult, op1=ALU.add),
         reads=rd, writes=[tok])
    P.op("act", lambda e: e.activation(out=out, in_=out, func=AF.Sin), reads=rd, writes=[tok])


def cmul(k, out_r, out_i, xr, xi, cr, ci, t1, t2, tok, eng="dve"):
    P = k.P
    rd = [tok]
    P.op(eng, lambda e: e.tensor_tensor(out=t1, in0=xr, in1=cr, op=ALU.mult), reads=rd, writes=[tok])
    P.op(eng, lambda e: e.tensor_tensor(out=t2, in0=xi, in1=ci, op=ALU.mult), reads=rd, writes=[tok])
    P.op(eng, lambda e: e.tensor_tensor(out=out_r, in0=t1, in1=t2, op=ALU.subtract), reads=rd, writes=[tok])
    P.op(eng, lambda e: e.tensor_tensor(out=t1, in0=xr, in1=ci, op=ALU.mult), reads=rd, writes=[tok])
    P.op(eng, lambda e: e.tensor_tensor(out=t2, in0=xi, in1=cr, op=ALU.mult), reads=rd, writes=[tok])
    P.op(eng, lambda e: e.tensor_tensor(out=out_i, in0=t1, in1=t2, op=ALU.add), reads=rd, writes=[tok])


def phaseD1(k):
    nc, P = k.nc, k.P
    prm = k.l1
    idf, idb = k.ident_f, k.ident_b
    with ExitStack() as ph:
        NP = 64
        APr = sb(k, ph, "APr", [NP, 64, 33], F32)
        APi = sb(k, ph, "APi", [NP, 64, 33], F32)
        RPr = sb(k, ph, "RPr", [NP, 64, 32], F32)
        RPi = sb(k, ph, "RPi", [NP, 64, 32], F32)
        Ctr = sb(k, ph, "Ctr", [NP, 1024], F32)
        Cti = sb(k, ph, "Cti", [NP, 1024], F32)
        bbr_f = sb(k, ph, "bbr", [NP, 72, 16], F32)
        bbi = sb(k, ph, "bbi", [NP, 64, 16], F32)
        nbbi_f = sb(k, ph, "nbbi", [NP, 72, 16], F32)
        bbr = bbr_f[:, 0:64, :]
        nbbi = nbbi_f[:, 0:64, :]
        P.op("pool", lambda e: e.memset(bbr_f[:, 64:72, :], 0.0), writes=[])
        P.op("pool", lambda e: e.memset(nbbi_f[:, 64:72, :], 0.0), writes=[])
        SQr = sb(k, ph, "SQr", [NP, 64, 7], F32)
        SQi = sb(k, ph, "SQi", [NP, 64, 7], F32)
        nSQi = sb(k, ph, "nSQi", [NP, 64, 7], F32)
        M10 = sb(k, ph, "M10", [128, 2, 64], F32)
        dcol = sb(k, ph, "dcol", [16, 64], F32)
        t_tab = P.tok("s5tab")
        t_M = P.tok("M10", dma=True)
        t_dcol = P.tok("dcol", dma=True)
        with ExitStack() as tmp:
            pmisc = ps(k, tmp, "pmiscD", [128, 512])
            t_pmisc = P.tok("pmiscD")
            def t64(name):
                return sb(k, tmp, name, [NP, 64], F32)
            a0 = sb(k, tmp, "a0", [64, 2, 64], F32)
            t_a0 = P.tok("a0", dma=True)
            P.dma("sp", a0[:, 0, :], prm["a_re"], [], [t_a0], t_a0)
            P.dma("sp", a0[:, 1, :], prm["a_im"], [], [t_a0], t_a0)
            arT, aiT, dtb, mag, ang, sn, cs = [t64(n) for n in ("arT", "aiT", "dtb", "mag", "ang", "sn", "cs")]
            tf = t64("tf")
            ti = sb(k, tmp, "ti", [NP, 64], I32)
            abr, abi, den, nr, zr, zi, u1, u2 = [t64(n) for n in ("abr", "abi", "den", "nr", "zr", "zi", "u1", "u2")]
            t_dt = P.tok("dtb", dma=True)
            for i, dst in enumerate((arT, aiT)):
                P.op("pe", lambda e, i=i: e.transpose(pmisc[0:64, i * 64:(i + 1) * 64], a0[:, i, :], idf[0:64, 0:64]),
                     reads=[t_a0, k.t_ident], writes=[t_pmisc])
                P.op("dve", lambda e, i=i, dst=dst: e.tensor_copy(out=dst[:], in_=pmisc[0:64, i * 64:(i + 1) * 64]),
                     reads=[t_pmisc], writes=[t_tab])
            P.dma("sp", dtb[:], prm["log_dt"].partition_broadcast(NP), [], [t_dt], t_dt)
            P.op("act", lambda e: e.activation(out=dtb[:], in_=dtb[:], func=AF.Exp), reads=[t_dt], writes=[t_tab])
            rd = [t_tab]
            P.op("dve", lambda e: e.tensor_mul(out=mag[:], in0=dtb[:], in1=arT[:]), reads=rd, writes=[t_tab])
            P.op("act", lambda e: e.activation(out=mag[:], in_=mag[:], func=AF.Exp), reads=rd, writes=[t_tab])
            P.op("dve", lambda e: e.tensor_mul(out=ang[:], in0=dtb[:], in1=aiT[:]), reads=rd, writes=[t_tab])
            emit_sin(k, sn[:], ang[:], tf[:], ti[:], t_tab, 0.0)
            emit_sin(k, cs[:], ang[:], tf[:], ti[:], t_tab, 0.5 * math.pi)
            P.op("dve", lambda e: e.tensor_mul(out=abr[:], in0=mag[:], in1=cs[:]), reads=rd, writes=[t_tab])
            P.op("dve", lambda e: e.tensor_mul(out=abi[:], in0=mag[:], in1=sn[:]), reads=rd, writes=[t_tab])
            P.op("dve", lambda e: e.tensor_mul(out=den[:], in0=arT[:], in1=arT[:]), reads=rd, writes=[t_tab])
            P.op("dve", lambda e: e.tensor_mul(out=u1[:], in0=aiT[:], in1=aiT[:]), reads=rd, writes=[t_tab])
            P.op("dve", lambda e: e.tensor_add(out=den[:], in0=den[:], in1=u1[:]), reads=rd, writes=[t_tab])
            P.op("dve", lambda e: e.reciprocal(out=den[:], in_=den[:]), reads=rd, writes=[t_tab])
            P.op("dve", lambda e: e.tensor_scalar_add(out=nr[:], in0=abr[:], scalar1=-1.0), reads=rd, writes=[t_tab])
            P.op("dve", lambda e: e.tensor_mul(out=u1[:], in0=nr[:], in1=arT[:]), reads=rd, writes=[t_tab])
            P.op("dve", lambda e: e.tensor_mul(out=u2[:], in0=abi[:], in1=aiT[:]), reads=rd, writes=[t_tab])
            P.op("dve", lambda e: e.tensor_add(out=u1[:], in0=u1[:], in1=u2[:]), reads=rd, writes=[t_tab])
            P.op("dve", lambda e: e.tensor_mul(out=zr[:], in0=u1[:], in1=den[:]), reads=rd, writes=[t_tab])
            P.op("dve", lambda e: e.tensor_mul(out=u1[:], in0=abi[:], in1=arT[:]), reads=rd, writes=[t_tab])
            P.op("dve", lambda e: e.tensor_mul(out=u2[:], in0=nr[:], in1=aiT[:]), reads=rd, writes=[t_tab])
            P.op("dve", lambda e: e.tensor_sub(out=u1[:], in0=u1[:], in1=u2[:]), reads=rd, writes=[t_tab])
            P.op("dve", lambda e: e.tensor_mul(out=zi[:], in0=u1[:], in1=den[:]), reads=rd, writes=[t_tab])
            br = sb(k, tmp, "br", [NP, 64, 16], F32)
            bi = sb(k, tmp, "bi", [NP, 64, 16], F32)
            w1 = sb(k, tmp, "w1", [NP, 64, 16], F32)
            w2 = sb(k, tmp, "w2", [NP, 64, 16], F32)
            t_b = P.tok("brbi", dma=True)
            P.dma("sp", br[:], prm["b_re"].rearrange("g p c -> p g c"), [], [t_b], t_b)
            P.dma("sp", bi[:], prm["b_im"].rearrange("g p c -> p g c"), [], [t_b], t_b)
            P.op("dve", lambda e: e.tensor_copy(out=w1[:], in_=br[:]), reads=[t_b], writes=[t_tab])
            zrb = zr[:].unsqueeze(2).broadcast_to([NP, 64, 16])
            zib = zi[:].unsqueeze(2).broadcast_to([NP, 64, 16])
            cmul(k, bbr, bbi[:], br[:], bi[:], zrb, zib, w1[:], w2[:], t_tab)
            P.op("dve", lambda e: e.tensor_scalar_mul(out=nbbi, in0=bbi[:], scalar1=-1.0), reads=rd, writes=[t_tab])
            cst = sb(k, tmp, "cst", [128, 8, 64], F32)
            t_cst = P.tok("cst", dma=True)
            for ri, (src_c, dst) in enumerate(((prm["c_re"], Ctr), (prm["c_im"], Cti))):
                P.dma("sp", cst[:], src_c.rearrange("(a p) q -> p a q", p=128), [], [t_cst], t_cst)
                for half in range(2):
                    for a in range(4):
                        P.op("pe", lambda e, a=a, half=half: e.transpose(
                            pmisc[0:64, a * 128:(a + 1) * 128], cst[:, half * 4 + a, :], idf[:]),
                            reads=[t_cst, k.t_ident], writes=[t_pmisc], inc=(a == 3))
                    P.op("dve", lambda e, half=half, dst=dst: e.tensor_copy(
                        out=dst[:, half * 512:(half + 1) * 512], in_=pmisc[0:64, :]),
                        reads=[t_pmisc], writes=[t_tab])
            P.op("dve", lambda e: e.memset(APr[:, :, 0:1], 1.0), writes=[t_tab])
            P.op("dve", lambda e: e.memset(APi[:, :, 0:1], 0.0), writes=[t_tab])
            P.op("dve", lambda e: e.tensor_copy(out=APr[:, :, 1], in_=abr[:]), reads=rd, writes=[t_tab])
            P.op("dve", lambda e: e.tensor_copy(out=APi[:, :, 1], in_=abi[:]), reads=rd, writes=[t_tab])
            pw1 = sb(k, tmp, "pw1", [NP, 64, 16], F32)
            pw2 = sb(k, tmp, "pw2", [NP, 64, 16], F32)
            for t0 in (1, 2, 4, 8, 16):
                crb = APr[:, :, t0:t0 + 1].broadcast_to([NP, 64, t0])
                cib = APi[:, :, t0:t0 + 1].broadcast_to([NP, 64, t0])
                cmul(k, APr[:, :, t0 + 1:2 * t0 + 1], APi[:, :, t0 + 1:2 * t0 + 1],
                     APr[:, :, 1:t0 + 1], APi[:, :, 1:t0 + 1], crb, cib, pw1[:, :, 0:t0], pw2[:, :, 0:t0], t_tab)
            for s in range(32):
                P.op("pool", lambda e, s=s: e.tensor_copy(out=RPr[:, :, s], in_=APr[:, :, 31 - s]), reads=rd, writes=[t_tab])
                P.op("pool", lambda e, s=s: e.tensor_copy(out=RPi[:, :, s], in_=APi[:, :, 31 - s]), reads=rd, writes=[t_tab])
            P.op("dve", lambda e: e.tensor_copy(out=SQr[:, :, 0], in_=APr[:, :, 32]), reads=rd, writes=[t_tab])
            P.op("dve", lambda e: e.tensor_copy(out=SQi[:, :, 0], in_=APi[:, :, 32]), reads=rd, writes=[t_tab])
            for j in range(6):
                cmul(k, SQr[:, :, j + 1], SQi[:, :, j + 1], SQr[:, :, j], SQi[:, :, j], SQr[:, :, j], SQi[:, :, j],
                     u1[:], u2[:], t_tab)
            P.op("dve", lambda e: e.tensor_scalar_mul(out=nSQi[:], in0=SQi[:], scalar1=-1.0), reads=rd, writes=[t_tab])
            for i, off in enumerate((D, 0)):
                srcap = bass.AP(k.mod_s.tensor, off, [[0, 8], [1, 16], [16, 64]])
                for sl in range(8):
                    P.dma("sp", M10[sl * 16:(sl + 1) * 16, i, :], bass.AP(k.mod_s.tensor, off, [[1, 16], [16, 64]]),
                          [], [t_M], t_M, allow_slow_non_contiguous=True)
            P.op("dve", lambda e: e.tensor_scalar_add(out=M10[:, 0, :], in0=M10[:, 0, :], scalar1=1.0), reads=[t_M], writes=[t_M])
            P.dma("sp", dcol[:], bass.AP(prm["d_skip"].tensor, 0, [[1, 16], [16, 64]]), [], [t_dcol], t_dcol,
                  allow_slow_non_contiguous=True)
            P.flush()

        if getattr(k, "stop", None) == "tables":
            return
        CAr = [sb(k, ph, "CAr%d" % i, [NP, 33, 16], F32) for i in range(4)]
        CAi = [sb(k, ph, "CAi%d" % i, [NP, 33, 16], F32) for i in range(4)]
        ca1 = sb(k, ph, "ca1", [NP, 33, 16], F32)
        ca2 = sb(k, ph, "ca2", [NP, 33, 16], F32)
        Wc = [sb(k, ph, "Wc%d" % i, [NP, 2, 512], BF16) for i in range(4)]
        ab1 = sb(k, ph, "ab1", [NP, 32, 16], F32)
        ab2 = sb(k, ph, "ab2", [NP, 32, 16], F32)
        ABr = sb(k, ph, "ABr", [NP, 32, 16], F32)
        ABi = sb(k, ph, "ABi", [NP, 32, 16], F32)
        Wend = [sb(k, ph, "Wend%d" % i, [128, 2, 4, 64], BF16) for i in range(4)]
        Gsb = [sb(k, ph, "Gsb%d" % i, [16, 63 * 16], BF16) for i in range(4)]
        dtmp = sb(k, ph, "dtmp", [16, 16], F32)
        t_dtmp = P.tok("dtmp")
        Sel = sb(k, ph, "Sel", [16, 8, 128], BF16)
        t_Sel = P.tok("Sel")
        P.op("pool", lambda e: e.memset(Sel[:], 0.0), writes=[t_Sel])
        for s8 in range(8):
            P.op("pool", lambda e, s8=s8: e.tensor_copy(out=Sel[:, s8, 16 * s8:16 * s8 + 16], in_=idb[0:16, 0:16]),
                 reads=[k.t_ident], writes=[t_Sel])
        for i4 in range(4):
            P.op("pool", lambda e, i4=i4: e.memset(Gsb[i4][:, 0:31 * 16], 0.0), writes=[])
        pW = [ps(k, ph, "pW%d" % i, [128, 512]) for i in range(2)]
        t_pW = [P.tok("pW%d" % i) for i in range(2)]
        Wg = [sb(k, ph, "Wg%d" % i, [128, 4, 512], BF16) for i in range(4)]
        UT = [sb(k, ph, "UT%d" % i, [128, 4, 128], BF16) for i in range(2)]
        Ha = sb(k, ph, "Ha", [NP, 2, 128], F32)
        Hb = sb(k, ph, "Hb", [NP, 2, 128], F32)
        Hp = [sb(k, ph, "Hp%d" % i, [NP, 2, 128], BF16) for i in range(2)]
        xblk = [sb(k, ph, "xblk%d" % i, [128, 32, 128], F32) for i in range(1)] * 2
        xr = [sb(k, ph, "xr%d" % i, [128, 8, 32, 16], F32) for i in range(2)]
        t_xr = [P.tok("xr%d" % i) for i in range(2)]
        ybuf = sb(k, ph, "ybuf", [128, 32, 128], BF16)
        ygt = [sb(k, ph, "ygt%d" % i, [128, 512], BF16) for i in range(2)]
        t_ygt = [P.tok("ygt%d" % i) for i in range(2)]
        yTst = sb(k, ph, "yTst", [128, 32, 128], BF16)
        ptU = ps(k, ph, "ptU", [128, 512])
        pE = ps(k, ph, "pE", [128, 512])
        pY = [ps(k, ph, "pY%d" % i, [128, 512]) for i in range(1)] * 2
        pG = ps(k, ph, "pG", [128, 512])
        pWe = ps(k, ph, "pWe", [128, 512])
        ptY = ps(k, ph, "ptY", [128, 512], BF16)
        t_CA = [P.tok("CA%d" % i) for i in range(4)]
        t_Wc = [P.tok("Wc%d" % i) for i in range(4)]
        t_AB = P.tok("AB")
        t_Wend = [P.tok("Wend%d" % i) for i in range(4)]
        t_Gsb = [P.tok("Gsb%d" % i) for i in range(4)]
        t_Wg = [P.tok("Wg%d" % i) for i in range(4)]
        t_UT = [P.tok("UT%d" % i) for i in range(2)]
        t_H = P.tok("H")
        t_Hp = [P.tok("Hp%d" % i) for i in range(2)]
        t_xb = [P.tok("xblk%d" % i, dma=True) for i in range(1)] * 2
        t_ybuf = P.tok("ybuf")
        t_yT = P.tok("yTst", dma=True)
        t_ptU = P.tok("ptU"); t_pE = P.tok("pE"); t_pY = [P.tok("pY0")] * 2
        t_pG = P.tok("pG"); t_pWe = P.tok("pWe"); t_ptY = P.tok("ptY")
        t_yTs = P.tok("yT_s")
        ydbg = None
        if k.debug and "s5" in k.dbg:
            ydbg = sb(k, ph, "ydbg", [128, 32, 128], F32)
            t_ydbg = P.tok("ydbg", dma=True)
        x2v = k.x2_s.rearrange("(kk s) c -> kk s c", s=32)
        rd = [t_tab]
        NG = getattr(k, "ngroups", 64)

        def stageA(g):
            cb, gl = g // 8, g % 8
            b = g % 4
            if gl == 0:
                xb = cb % 2
                P.dma("sp", xblk[xb][:], x2v[:, :, cb * 128:(cb + 1) * 128], [], [t_xb[xb]], t_xb[xb])
                for g8 in range(8):
                    P.op("pool", lambda e, xb=xb, g8=g8: e.tensor_copy(out=xr[xb][:, g8, :, :],
                                                                     in_=xblk[xb][:, :, g8 * 16:(g8 + 1) * 16]),
                         reads=[t_xb[xb]], writes=[t_xr[xb]])
            ctr_b = Ctr[:, g * 16:(g + 1) * 16].unsqueeze(1).broadcast_to([NP, 33, 16])
            cti_b = Cti[:, g * 16:(g + 1) * 16].unsqueeze(1).broadcast_to([NP, 33, 16])
            apr_b = APr[:, g, :].unsqueeze(2).broadcast_to([NP, 33, 16])
            api_b = APi[:, g, :].unsqueeze(2).broadcast_to([NP, 33, 16])
            P.op("pool", lambda e: e.tensor_copy(out=ca1[:, 0, 0:1], in_=ca1[:, 0, 0:1]), reads=[t_tab, t_CA[b]], writes=[t_CA[b]])
            cmul(k, CAr[b][:], CAi[b][:], ctr_b, cti_b, apr_b, api_b, ca1[:], ca2[:], t_CA[b], eng="pool")
            P.op("pe", lambda e, b=b, g=g: e.matmul(pG[:], lhsT=bbr_f[:, g:g + 8, :].rearrange("p a b -> p (a b)"), rhs=CAr[b][:, 0:32, :].rearrange("p a b -> p (a b)"),
                                                   start=True, stop=False), reads=[t_tab, t_CA[b]], writes=[t_pG], inc=False)
            P.op("pe", lambda e, b=b, g=g: e.matmul(pG[:], lhsT=nbbi_f[:, g:g + 8, :].rearrange("p a b -> p (a b)"), rhs=CAi[b][:, 0:32, :].rearrange("p a b -> p (a b)"),
                                                   start=False, stop=True), reads=[t_tab, t_CA[b]], writes=[t_pG])
            P.op("act", lambda e, b=b: e.activation(out=Gsb[b][:, 496:1008], in_=pG[0:16, :], func=AF.Copy),
                 reads=[t_pG], writes=[t_Gsb[b]])
            P.op("pool", lambda e, g=g: e.tensor_scalar_mul(out=dtmp[:], in0=idf[0:16, 0:16], scalar1=dcol[:, g:g + 1]),
                 reads=[t_dcol, k.t_ident], writes=[t_dtmp])
            P.op("pool", lambda e, b=b: e.tensor_tensor(out=Gsb[b][:, 496:512], in0=Gsb[b][:, 496:512], in1=dtmp[:], op=ALU.add),
                 reads=[t_dtmp], writes=[t_Gsb[b]])
            for j in range(4):
                wb = (g * 4 + j) % 2
                c0 = 8 * j * 16
                for sl in range(8):
                    o = (31 - 8 * j - sl) * 16 + c0
                    P.op("pe", lambda e, b=b, wb=wb, sl=sl, o=o, c0=c0: e.matmul(
                        pW[wb][:, c0:512], lhsT=Sel[:, sl, :], rhs=Gsb[b][:, o:o + 512 - c0], start=(sl == 0), stop=(sl == 7)),
                        reads=[t_Sel, t_Gsb[b]], writes=[t_pW[wb]], inc=(sl == 7))
                P.op("act", lambda e, b=b, wb=wb, j=j, c0=c0: e.activation(out=Wg[b][:, j, c0:512], in_=pW[wb][:, c0:512], func=AF.Copy),
                     reads=[t_pW[wb]], writes=[t_Wg[b]])
            P.op("act", lambda e, b=b: e.activation(out=Wc[b][:, 0, :], in_=CAr[b][:, 1:33, :].rearrange("p a b -> p (a b)"), func=AF.Copy),
                 reads=[t_CA[b]], writes=[t_Wc[b]])
            P.op("act", lambda e, b=b: e.activation(out=Wc[b][:, 1, :], in_=CAi[b][:, 1:33, :].rearrange("p a b -> p (a b)"), func=AF.Copy, scale=-1.0),
                 reads=[t_CA[b]], writes=[t_Wc[b]])
            rpr_b = RPr[:, g, :].unsqueeze(2).broadcast_to([NP, 32, 16])
            rpi_b = RPi[:, g, :].unsqueeze(2).broadcast_to([NP, 32, 16])
            bbr_b = bbr[:, g, :].unsqueeze(1).broadcast_to([NP, 32, 16])
            bbi_b = bbi[:, g, :].unsqueeze(1).broadcast_to([NP, 32, 16])
            P.op("pool", lambda e: e.tensor_copy(out=ab1[:, 0, 0:1], in_=ab1[:, 0, 0:1]), reads=[t_tab, t_AB], writes=[t_AB])
            cmul(k, ABr[:], ABi[:], rpr_b, rpi_b, bbr_b, bbi_b, ab1[:], ab2[:], t_AB, eng="pool")
            for ri, AB_ in enumerate((ABr, ABi)):
                for j in range(4):
                    P.op("pe", lambda e, ri=ri, j=j, AB_=AB_: e.transpose(
                        pWe[:, (ri * 4 + j) * 64:(ri * 4 + j + 1) * 64], AB_[:, 8 * j:8 * j + 8, :].rearrange("p a b -> p (a b)"), idf[0:64, 0:64]),
                        reads=[t_AB, k.t_ident], writes=[t_pWe], inc=(ri == 1 and j == 3))
            P.op("act", lambda e, b=b: e.activation(out=Wend[b][:].rearrange("p a j q -> p (a j q)"), in_=pWe[:], func=AF.Copy),
                 reads=[t_pWe], writes=[t_Wend[b]])

        def stageB1(g):
            cb, gl = g // 8, g % 8
            b = g % 2
            b4 = g % 4
            xb = cb % 2
            for j in range(4):
                P.op("pe", lambda e, j=j, xb=xb, gl=gl: e.transpose(
                    ptU[:, j * 128:(j + 1) * 128], xr[xb][:, gl, 8 * j:8 * j + 8, :].rearrange("p a b -> p (a b)"), idf[:]),
                    reads=[t_xr[xb], k.t_ident], writes=[t_ptU], inc=(j == 3))
            P.op("act", lambda e, b=b, g=g: e.activation(
                out=UT[b][:].rearrange("p j q -> p (j q)"), in_=ptU[:], func=AF.Identity,
                scale=M10[:, 0, g:g + 1], bias=M10[:, 1, g:g + 1]), reads=[t_ptU, t_M], writes=[t_UT[b]])
            for ri in range(2):
                for j in range(4):
                    P.op("pe", lambda e, b=b, ri=ri, j=j: e.matmul(
                        pE[0:64, ri * 128:(ri + 1) * 128], lhsT=Wend[b4][:, ri, j, :], rhs=UT[b][:, j, :],
                        start=(j == 0), stop=(j == 3)), reads=[t_Wend[b4], t_UT[b]], writes=[t_pE],
                        inc=(ri == 1 and j == 3))
            P.op("dve", lambda e: e.tensor_copy(out=Ha[:].rearrange("p a q -> p (a q)"), in_=pE[0:64, 0:256]),
                 reads=[t_pE], writes=[t_H])
            src_, dst_ = Ha, Hb
            for js, d in enumerate((1, 2, 4, 8, 16, 32, 64)):
                rdh = [t_H, t_tab]
                P.op("dve", lambda e, s_=src_, d_=dst_, d=d: e.tensor_copy(out=d_[:, :, 0:d], in_=s_[:, :, 0:d]),
                     reads=rdh, writes=[t_H])
                ar_ = SQr[:, g, js:js + 1]
                ai_ = SQi[:, g, js:js + 1]
                nai_ = nSQi[:, g, js:js + 1]
                n = 128 - d
                P.op("dve", lambda e, s_=src_, d_=dst_, d=d, n=n, ar_=ar_: e.scalar_tensor_tensor(
                    out=d_[:, :, d:128], in0=s_[:, :, 0:n], scalar=ar_, in1=s_[:, :, d:128], op0=ALU.mult, op1=ALU.add),
                    reads=rdh, writes=[t_H])
                P.op("dve", lambda e, s_=src_, d_=dst_, d=d, n=n, nai_=nai_: e.scalar_tensor_tensor(
                    out=d_[:, 0, d:128], in0=s_[:, 1, 0:n], scalar=nai_, in1=d_[:, 0, d:128], op0=ALU.mult, op1=ALU.add),
                    reads=rdh, writes=[t_H])
                P.op("dve", lambda e, s_=src_, d_=dst_, d=d, n=n, ai_=ai_: e.scalar_tensor_tensor(
                    out=d_[:, 1, d:128], in0=s_[:, 0, 0:n], scalar=ai_, in1=d_[:, 1, d:128], op0=ALU.mult, op1=ALU.add),
                    reads=rdh, writes=[t_H])
                src_, dst_ = dst_, src_
            P.op("dve", lambda e, b=b: e.memset(Hp[b][:, :, 0:1], 0.0), writes=[t_Hp[b]])
            P.op("dve", lambda e, b=b, s_=src_: e.tensor_copy(out=Hp[b][:, :, 1:128], in_=s_[:, :, 0:127]),
                 reads=[t_H], writes=[t_Hp[b]])
            yb = g % 2
            for j in range(4):
                P.op("pe", lambda e, b=b, j=j, yb=yb: e.matmul(pY[yb][:, 128 * j:512], lhsT=UT[b][:, j, :], rhs=Wg[b4][:, j, 128 * j:512],
                                                             start=(j == 0), stop=False),
                     reads=[t_UT[b], t_Wg[b4]], writes=[t_pY[yb]], inc=False)

        def stageB2(g):
            cb, gl = g // 8, g % 8
            b = g % 2
            b4 = g % 4
            yb = g % 2
            for ri in range(2):
                P.op("pe", lambda e, b=b, ri=ri, yb=yb: e.matmul(pY[yb][:], lhsT=Hp[b][:, ri, :], rhs=Wc[b4][:, ri, :],
                                                               start=False, stop=(ri == 1)),
                     reads=[t_Hp[b], t_Wc[b4]], writes=[t_pY[yb]], inc=(ri == 1))
            P.op("act", lambda e, yb=yb: e.activation(out=ygt[yb][:], in_=pY[yb][:], func=AF.Gelu),
                 reads=[t_pY[yb]], writes=[t_ygt[yb]])
            P.op("dve", lambda e, yb=yb, gl=gl: e.tensor_copy(
                out=ybuf[:, :, gl * 16:(gl + 1) * 16], in_=ygt[yb][:].rearrange("p (t c) -> p t c", c=16)),
                reads=[t_ygt[yb]], writes=[t_ybuf])
            if ydbg is not None and not getattr(k, "no_ydbg", False):
                P.op("dve", lambda e, yb=yb, gl=gl: e.tensor_copy(
                    out=ydbg[:, :, gl * 16:(gl + 1) * 16], in_=pY[yb][:].rearrange("p (t c) -> p t c", c=16)),
                    reads=[t_pY[yb]], writes=[t_ydbg, t_pY[yb]])
            if gl == 7:
                for t4 in range(8):
                    for i in range(4):
                        t = t4 * 4 + i
                        P.op("pe", lambda e, i=i, t=t: e.transpose(ptY[:, i * 128:(i + 1) * 128], ybuf[:, t, :], idb[:]),
                             reads=[t_ybuf, k.t_ident], writes=[t_ptY], inc=(i == 3))
                    P.op("dve", lambda e, t4=t4: e.tensor_copy(
                        out=yTst[:, t4 * 4:(t4 + 1) * 4, :].rearrange("p a q -> p (a q)"), in_=ptY[:]),
                        reads=[t_ptY], writes=[t_yT])
                P.dma("sp", k.yT_s[cb], yTst[:], [t_yT], [t_yTs], t_yT)
                if ydbg is not None:
                    ev = P.dma("sp", k.dbg["s5"].rearrange("(kk s) c -> kk s c", s=32)[:, :, cb * 128:(cb + 1) * 128],
                               ydbg[:], [t_ydbg], [], t_ydbg)
                    P.out_events.append(ev)

        for g in range(min(3, NG)):
            stageA(g)
        for g in range(NG):
            stageB1(g)
            if g + 3 < NG:
                stageA(g + 3)
            stageB2(g)
        P.mute = False
        dma_barrier(P, "sp", [t_yT])
        P.flush()


def phaseD1b(k):
    nc, P = k.nc, k.P
    prm = k.l1
    modT = k.modT[1]
    gbc = k.gbc
    idf = k.ident_f
    with ExitStack() as ph:
        w_glu = sb(k, ph, "w_glu_sb", [128, KC, 2 * D], BF16)
        t_wglu = P.tok("w_glu", dma=True)
        load_cast_weight(k, w_glu, prm["w_glu"].rearrange("(kc p) n -> p kc n", p=128), t_wglu, 4)
        bglu = sb(k, ph, "bglu", [1, 2 * D], BF16)
        t_bglu = P.tok("bglu", dma=True)
        P.dma("pool", bglu[:], prm["b_glu"], [], [t_bglu], t_bglu)
        ones_b = sb(k, ph, "ones_b", [1, 128], BF16)
        ones_f = sb(k, ph, "ones_f", [1, 128], F32)
        t_ones = P.tok("onesD")
        P.op("dve", lambda e: e.memset(ones_b[:], 1.0), writes=[t_ones])
        P.op("dve", lambda e: e.memset(ones_f[:], 1.0), writes=[t_ones])
        rw = sb(k, ph, "rw", [128, KC, NE], F32)
        rb_ = sb(k, ph, "rb", [1, NE], F32)
        t_rw = P.tok("rw", dma=True)
        P.dma("sp", rw[:], prm["router_w"].rearrange("(kc p) e -> p kc e", p=128), [], [t_rw], t_rw)
        P.dma("sp", rb_[:], prm["router_b"], [], [t_rw], t_rw)
        lng = sb(k, ph, "l1ln1g", [128, D], F32)
        lnb = sb(k, ph, "l1ln1b", [128, D], F32)
        t_lng = P.tok("l1ln1g", dma=True)
        t_lnb = P.tok("l1ln1b", dma=True)
        load_bc(k, lng[:], prm["ln1_g"], t_lng)
        load_bc(k, lnb[:], prm["ln1_b"], t_lnb)
        gcol = sb(k, ph, "gcolD", [128, 2, KC], F32)
        t_gcol = P.tok("gcolD", dma=True)
        P.dma("sp", gcol[:, 0, :], prm["ln1_g"].rearrange("o (kc p) -> p (o kc)", p=128), [], [t_gcol], t_gcol,
              allow_slow_non_contiguous=True)
        P.dma("sp", gcol[:, 1, :], prm["ln1_b"].rearrange("o (kc p) -> p (o kc)", p=128), [], [t_gcol], t_gcol,
              allow_slow_non_contiguous=True)
        AB = sb(k, ph, "ABD", [128, 2, KC], F32)
        t_AB = P.tok("ABD")
        P.op("dve", lambda e: e.tensor_mul(out=AB[:, 0, :], in0=gcol[:, 0, :], in1=modT[:, 32:40]),
             reads=[t_gcol, k.t_modT[1]], writes=[t_AB])
        P.op("dve", lambda e: e.tensor_mul(out=AB[:, 1, :], in0=gcol[:, 1, :], in1=modT[:, 32:40]),
             reads=[t_gcol, k.t_modT[1]], writes=[t_AB])
        P.op("dve", lambda e: e.tensor_add(out=AB[:, 1, :], in0=AB[:, 1, :], in1=modT[:, 24:32]),
             reads=[k.t_modT[1]], writes=[t_AB])
        L = LNScratch(k, ph, "d1b")
        yT = [sb(k, ph, "yTt%d" % i, [128, 8, 128], BF16) for i in range(2)]
        x2t = [sb(k, ph, "x2t%d" % i, [128, D], F32) for i in range(2)]
        sig = sb(k, ph, "sig", [128, D], F32)
        r = [sb(k, ph, "rD%d" % i, [128, D], F32) for i in range(2)]
        x3o = [sb(k, ph, "x3o%d" % i, [128, D], F32) for i in range(2)]
        hmTf = sb(k, ph, "hmTf", [128, KC, 128], F32)
        hmTb = [sb(k, ph, "hmTb%d" % i, [128, KC, 128], BF16) for i in range(2)]
        lg = sb(k, ph, "lg", [128, NE], F32)
        mx8 = sb(k, ph, "mx8", [128, 8], F32)
        mk = sb(k, ph, "mk", [128, 2, NE], F32)
        gg = sb(k, ph, "gg", [128, 2], F32)
        cw = [sb(k, ph, "cw%d" % i, [128, NE], F32) for i in range(2)]
        pz = [ps(k, ph, "pz%d" % i, [128, 512]) for i in range(4)]
        ptf = [ps(k, ph, "ptfD%d" % i, [128, 512]) for i in range(2)]
        plg = ps(k, ph, "plg", [128, 512])
        t_yT = [P.tok("yTt%d" % i, dma=True) for i in range(2)]
        t_x2t = [P.tok("x2t%d" % i, dma=True) for i in range(2)]
        t_sig = P.tok("sig")
        t_r = [P.tok("rD%d" % i) for i in range(2)]
        t_x3o = [P.tok("x3o%d" % i, dma=True) for i in range(2)]
        t_hmTf = P.tok("hmTf")
        t_hmTb = [P.tok("hmTb%d" % i, dma=True) for i in range(2)]
        t_rt = P.tok("router")
        t_cw = [P.tok("cw%d" % i, dma=True) for i in range(2)]
        t_pz = [P.tok("pz%d" % i) for i in range(4)]
        t_ptf = [P.tok("ptfD%d" % i) for i in range(2)]
        t_plg = P.tok("plg")
        t_scr = P.tok("scrD1b")
        x2v = k.x2_s.rearrange("(kk s) c -> s kk c", s=32)
        hv = k.hmT_s.rearrange("(kc p) (t q) -> p kc t q", p=128, q=128)
        rx = sb(k, ph, "rD2", [128, D], F32)
        r = r + [rx]
        t_r = t_r + [P.tok("rD2")]
        x2x = sb(k, ph, "x2t2", [128, D], F32)
        x2t = x2t + [x2x]
        t_x2t = t_x2t + [P.tok("x2t2", dma=True)]
        dst = dict(itf=0)

        yTx = sb(k, ph, "yTt2", [128, 8, 128], BF16)
        yT = yT + [yTx]
        t_yT = t_yT + [P.tok("yTt2", dma=True)]

        def LD(t):
            b = t % 3
            P.dma("sp", yT[b][:], k.yT_s[:, :, t, :].rearrange("cb c q -> c cb q"), [], [t_yT[b]], t_yT[b])
            P.dma("sp", x2t[t % 3][:], x2v[t], [], [t_x2t[t % 3]], t_x2t[t % 3])

        def S1(t):
            b = t % 3
            for n in range(4):
                for cb in range(8):
                    P.op("pe", lambda e, n=n, cb=cb, b=b: e.matmul(
                        pz[n][:], lhsT=yT[b][:, cb, :], rhs=w_glu[:, cb, n * 512:(n + 1) * 512], start=(cb == 0), stop=False),
                        reads=[t_yT[b], t_wglu], writes=[t_pz[n]], inc=False)
                P.op("pe", lambda e, n=n: e.matmul(pz[n][:], lhsT=ones_b[0:1, :], rhs=bglu[0:1, n * 512:(n + 1) * 512],
                                                   start=False, stop=True),
                     reads=[t_ones, t_bglu], writes=[t_pz[n]])

        def S2(t):
            b = t % 2
            rb = t % 3
            for hf in range(2):
                P.op("act", lambda e, hf=hf: e.activation(out=sig[:, hf * 512:(hf + 1) * 512], in_=pz[2 + hf][:], func=AF.Sigmoid),
                     reads=[t_pz[2 + hf]], writes=[t_sig])
                P.op("dve", lambda e, hf=hf: e.tensor_tensor(out=sig[:, hf * 512:(hf + 1) * 512], in0=pz[hf][:],
                                                            in1=sig[:, hf * 512:(hf + 1) * 512], op=ALU.mult),
                     reads=[t_pz[hf]], writes=[t_sig])
            P.op("pool", lambda e: e.tensor_tensor(out=sig[:], in0=sig[:], in1=gbc[:, 0, :], op=ALU.mult),
                 reads=[k.t_gbc], writes=[t_sig])
            P.op("dve", lambda e, rb=rb: e.scalar_tensor_tensor(out=r[rb][:], in0=x2t[rb][:], scalar=DN_ALPHA, in1=sig[:],
                                                               op0=ALU.mult, op1=ALU.add),
                 reads=[t_x2t[rb], t_sig], writes=[t_r[rb]])
            emit_ln(k, r[rb][:], t_r[rb], L)
            P.op("pool", lambda e, b=b, rb=rb: e.tensor_tensor(out=x3o[b][:], in0=r[rb][:], in1=lng[:], op=ALU.mult),
                 reads=[t_r[rb], t_lng], writes=[t_x3o[b]])
            P.op("pool", lambda e, b=b: e.tensor_tensor(out=x3o[b][:], in0=x3o[b][:], in1=lnb[:], op=ALU.add),
                 reads=[t_lnb], writes=[t_x3o[b]])
            P.dma("sp", k.x3_s[t], x3o[b][:], [t_x3o[b]], [t_scr], t_x3o[b])

        def S3(t):
            b = t % 2
            rb = t % 3
            for k4 in range(2):
                tf = dst["itf"] % 2
                dst["itf"] += 1
                for q in range(4):
                    kc = k4 * 4 + q
                    P.op("pe", lambda e, tf=tf, kc=kc, q=q, rb=rb: e.transpose(
                        ptf[tf][:, q * 128:(q + 1) * 128], r[rb][:, kc * 128:(kc + 1) * 128], idf[:]),
                        reads=[t_r[rb], k.t_ident], writes=[t_ptf[tf]], inc=(q == 3))
                for q in range(4):
                    kc = k4 * 4 + q
                    P.op("act", lambda e, tf=tf, kc=kc, q=q: e.activation(
                        out=hmTf[:, kc, :], in_=ptf[tf][:, q * 128:(q + 1) * 128],
                        func=AF.Identity, scale=AB[:, 0, kc:kc + 1], bias=AB[:, 1, kc:kc + 1]),
                        reads=[t_ptf[tf], t_AB], writes=[t_hmTf])
            P.op("dve", lambda e, b=b: e.tensor_copy(out=hmTb[b][:], in_=hmTf[:]), reads=[t_hmTf], writes=[t_hmTb[b]])
            P.dma("sp", hv[:, :, t, :], hmTb[b][:], [t_hmTb[b]], [t_scr], t_hmTb[b])
            for kc in range(KC):
                P.op("pe", lambda e, kc=kc: e.matmul(plg[:, 0:NE], lhsT=hmTf[:, kc, :], rhs=rw[:, kc, :],
                                                     start=(kc == 0), stop=False),
                     reads=[t_hmTf, t_rw], writes=[t_plg], inc=False)
            P.op("pe", lambda e: e.matmul(plg[:, 0:NE], lhsT=ones_f[0:1, :], rhs=rb_[0:1, :], start=False, stop=True),
                 reads=[t_ones, t_rw], writes=[t_plg])
            P.op("dve", lambda e: e.tensor_copy(out=lg[:], in_=plg[:, 0:NE]), reads=[t_plg], writes=[t_rt])
            rdr = [t_rt]
            P.op("dve", lambda e: e.max(out=mx8[:], in_=lg[:]), reads=rdr, writes=[t_rt])
            P.op("dve", lambda e: e.tensor_scalar(out=mk[:, 0, :], in0=lg[:], scalar1=mx8[:, 0:1], scalar2=None, op0=ALU.is_equal),
                 reads=rdr, writes=[t_rt])
            P.op("dve", lambda e: e.tensor_scalar(out=mk[:, 1, :], in0=lg[:], scalar1=mx8[:, 1:2], scalar2=None, op0=ALU.is_equal),
                 reads=rdr, writes=[t_rt])
            P.op("dve", lambda e: e.tensor_sub(out=gg[:, 0:1], in0=mx8[:, 0:1], in1=mx8[:, 1:2]), reads=rdr, writes=[t_rt])
            P.op("act", lambda e: e.activation(out=gg[:, 0:1], in_=gg[:, 0:1], func=AF.Sigmoid), reads=rdr, writes=[t_rt])
            P.op("dve", lambda e: e.tensor_scalar(out=gg[:, 1:2], in0=gg[:, 0:1], scalar1=-1.0, scalar2=1.0,
                                                  op0=ALU.mult, op1=ALU.add), reads=rdr, writes=[t_rt])
            P.op("dve", lambda e, b=b: e.tensor_scalar_mul(out=cw[b][:], in0=mk[:, 0, :], scalar1=gg[:, 0:1]),
                 reads=rdr, writes=[t_cw[b]])
            P.op("dve", lambda e, b=b: e.scalar_tensor_tensor(out=cw[b][:], in0=mk[:, 1, :], scalar=gg[:, 1:2], in1=cw[b][:],
                                                             op0=ALU.mult, op1=ALU.add), reads=rdr, writes=[t_cw[b]])
            P.dma("sp", k.cw_s[t], cw[b][:], [t_cw[b]], [t_scr], t_cw[b])

        LD(0)
        LD(1)
        S1(0)
        for t in range(32):
            if t + 2 < 32:
                LD(t + 2)
            S2(t)
            if t + 1 < 32:
                S1(t + 1)
            if t >= 1:
                S3(t - 1)
        S3(31)
        dma_barrier(P, "sp", t_hmTb + t_x3o + t_cw)
        if k.debug and "x3" in k.dbg:
            big = sb(k, ph, "dbgbigD", [128, 2, 1024], F32)
            t_big = P.tok("dbgbigD", dma=True)
            for c8 in range(16):
                P.dma("sp", big[:], k.x3_s[c8 * 2:(c8 + 1) * 2].rearrange("t q d -> q t d"), [t_scr], [t_big], t_big)
                ev = P.dma("sp", k.dbg["x3"].rearrange("(kk s) d -> s kk d", s=32)[c8 * 2:(c8 + 1) * 2].rearrange("t q d -> q t d"),
                           big[:], [t_big], [], t_big)
            lgd = sb(k, ph, "dbgcw", [128, 32, 8], F32)
            t_lgd = P.tok("dbgcw", dma=True)
            P.dma("sp", lgd[:], k.cw_s.rearrange("t q e -> q t e"), [t_scr], [t_lgd], t_lgd)
            ev2 = P.dma("sp", k.dbg["cw"].rearrange("(kk s) e -> kk s e", s=32), lgd[:], [t_lgd], [], t_lgd)
            P.out_events.append(ev)
            P.out_events.append(ev2)
        P.flush()


def phaseD2(k):
    nc, P = k.nc, k.P
    prm = k.l1
    gbc = k.gbc
    with ExitStack() as ph:
        wg = [sb(k, ph, "ewg%d" % i, [128, KC, DFE], BF16) for i in range(2)]
        wu = [sb(k, ph, "ewu%d" % i, [128, KC, DFE], BF16) for i in range(2)]
        wd = [sb(k, ph, "ewd%d" % i, [128, FCE, D], BF16) for i in range(2)]
        t_wg = [P.tok("ewg%d" % i, dma=True) for i in range(2)]
        t_wu = [P.tok("ewu%d" % i, dma=True) for i in range(2)]
        t_wd = [P.tok("ewd%d" % i, dma=True) for i in range(2)]
        lng = sb(k, ph, "l1ln2g", [128, D], F32)
        lnb = sb(k, ph, "l1ln2b", [128, D], F32)
        t_lng = P.tok("l1ln2g", dma=True)
        t_lnb = P.tok("l1ln2b", dma=True)
        load_bc(k, lng[:], prm["ln2_g"], t_lng)
        load_bc(k, lnb[:], prm["ln2_b"], t_lnb)
        L = LNScratch(k, ph, "d2")
        hT = [sb(k, ph, "hTm%d" % i, [128, KC, 512], BF16) for i in range(2)]
        actT = sb(k, ph, "actTm", [128, FCE, 512], BF16)
        sg = [sb(k, ph, "sgm%d" % i, [128, 512], BF16) for i in range(2)]
        acc = [sb(k, ph, "accm%d" % i, [128, D], F32) for i in range(3)]
        x3t = [sb(k, ph, "x3t%d" % i, [128, D], F32) for i in range(2)]
        cwt = [sb(k, ph, "cwt%d" % i, [128, 4, NE], F32) for i in range(2)]
        pg = [ps(k, ph, "pgm%d" % i, [128, 512]) for i in range(2)]
        pu = [ps(k, ph, "pum%d" % i, [128, 512]) for i in range(2)]
        py = [ps(k, ph, "pym%d" % i, [128, 512]) for i in range(4)]
        t_hT = [P.tok("hTm%d" % i, dma=True) for i in range(2)]
        t_actT = P.tok("actTm")
        t_sg = [P.tok("sgm%d" % i) for i in range(2)]
        t_acc = [P.tok("accm%d" % i, dma=True) for i in range(3)]
        t_x3t = [P.tok("x3t%d" % i, dma=True) for i in range(2)]
        t_cwt = [P.tok("cwt%d" % i, dma=True) for i in range(2)]
        t_pg = [P.tok("pgm%d" % i) for i in range(2)]
        t_pu = [P.tok("pum%d" % i) for i in range(2)]
        t_py = [P.tok("pym%d" % i) for i in range(4)]
        t_yacc = [P.tok("yacc%d" % t) for t in range(32)]
        outv = k.out.rearrange("(kk s) d -> s kk d", s=32)
        hv = k.hmT_s.rearrange("(kc p) n -> p kc n", p=128)

        def load_expert(e):
            b = e % 2
            load_cast_weight(k, wg[b], prm["w_gate"][e].rearrange("(kc p) n -> p kc n", p=128), t_wg[b], 4)
            load_cast_weight(k, wu[b], prm["w_up"][e].rearrange("(kc p) n -> p kc n", p=128), t_wu[b], 4)
            load_cast_weight(k, wd[b], prm["w_down"][e].rearrange("(fc p) n -> p fc n", p=128), t_wd[b], 4)
            P.op("pool", lambda ee, b=b: ee.tensor_tensor(
                out=wd[b][:], in0=wd[b][:], in1=gbc[:, 1, :].unsqueeze(1).broadcast_to([128, FCE, D]), op=ALU.mult),
                reads=[k.t_gbc], writes=[t_wd[b]])
        load_expert(0)
        ipy = 0
        it = 0

        def ld_tile(i):
            e_ = i // 32
            t_ = i % 32
            if e_ > 0:
                P.dma("sp", acc[i % 3][:], k.yacc_s[t_], [t_yacc[t_]], [t_acc[i % 3]], t_acc[i % 3])
            if e_ == NE - 1:
                P.dma("sp", x3t[i % 2][:], k.x3_s[t_], [], [t_x3t[i % 2]], t_x3t[i % 2])

        def ld_group(idx):
            g_ = idx % 8
            hb_ = idx % 2
            P.dma("sp", hT[hb_][:], hv[:, :, g_ * 512:(g_ + 1) * 512], [], [t_hT[hb_]], t_hT[hb_])
            P.dma("sp", cwt[hb_][:], k.cw_s[g_ * 4:(g_ + 1) * 4].rearrange("t q e -> q t e"), [], [t_cwt[hb_]], t_cwt[hb_])
        for e in range(NE):
            eb = e % 2
            if e + 1 < NE:
                load_expert(e + 1)
            for g in range(8):
                hb = (e * 8 + g) % 2
                if e == 0 and g == 0:
                    ld_group(0)
                if e * 8 + g + 1 < NE * 8:
                    ld_group(e * 8 + g + 1)
                for fc in range(FCE):
                    b = fc % 2
                    for kc in range(KC):
                        P.op("pe", lambda ee, b=b, kc=kc, fc=fc, eb=eb, hb=hb: ee.matmul(
                            pg[b][:], lhsT=wg[eb][:, kc, fc * 128:(fc + 1) * 128], rhs=hT[hb][:, kc, :],
                            start=(kc == 0), stop=(kc == KC - 1)),
                            reads=[t_wg[eb], t_hT[hb]], writes=[t_pg[b]], inc=(kc == KC - 1))
                    for kc in range(KC):
                        P.op("pe", lambda ee, b=b, kc=kc, fc=fc, eb=eb, hb=hb: ee.matmul(
                            pu[b][:], lhsT=wu[eb][:, kc, fc * 128:(fc + 1) * 128], rhs=hT[hb][:, kc, :],
                            start=(kc == 0), stop=(kc == KC - 1)),
                            reads=[t_wu[eb], t_hT[hb]], writes=[t_pu[b]], inc=(kc == KC - 1))
                    P.op("act", lambda ee, b=b: ee.activation(out=sg[b][:], in_=pg[b][:], func=AF.Silu),
                         reads=[t_pg[b]], writes=[t_sg[b]])
                    P.op("dve", lambda ee, b=b, fc=fc: ee.tensor_tensor(out=actT[:, fc, :], in0=sg[b][:], in1=pu[b][:],
                                                                       op=ALU.mult),
                         reads=[t_sg[b], t_pu[b]], writes=[t_actT])
                for tt in range(4):
                    t = g * 4 + tt
                    ab = it % 3
                    xb3 = it % 2
                    if it == 0:
                        ld_tile(0)
                    if it + 1 < NE * 32:
                        ld_tile(it + 1)
                    it += 1
                    pys = []
                    for dh in range(2):
                        pb = ipy % 4
                        ipy += 1
                        pys.append(pb)
                        for fc in range(FCE):
                            P.op("pe", lambda ee, pb=pb, fc=fc, tt=tt, dh=dh, eb=eb: ee.matmul(
                                py[pb][:], lhsT=actT[:, fc, tt * 128:(tt + 1) * 128], rhs=wd[eb][:, fc, dh * 512:(dh + 1) * 512],
                                start=(fc == 0), stop=(fc == FCE - 1)),
                                reads=[t_actT, t_wd[eb]], writes=[t_py[pb]], inc=(fc == FCE - 1))
                    for dh in range(2):
                        pb = pys[dh]
                        if e == 0:
                            P.op("dve", lambda ee, pb=pb, dh=dh, ab=ab, hb=hb, tt=tt, e=e: ee.tensor_scalar_mul(
                                out=acc[ab][:, dh * 512:(dh + 1) * 512], in0=py[pb][:], scalar1=cwt[hb][:, tt, e:e + 1]),
                                reads=[t_py[pb], t_cwt[hb]], writes=[t_acc[ab]])
                        else:
                            P.op("dve", lambda ee, pb=pb, dh=dh, ab=ab, hb=hb, tt=tt, e=e: ee.scalar_tensor_tensor(
                                out=acc[ab][:, dh * 512:(dh + 1) * 512], in0=py[pb][:], scalar=cwt[hb][:, tt, e:e + 1],
                                in1=acc[ab][:, dh * 512:(dh + 1) * 512], op0=ALU.mult, op1=ALU.add),
                                reads=[t_py[pb], t_cwt[hb]], writes=[t_acc[ab]])
                    if e < NE - 1:
                        P.dma("sp", k.yacc_s[t], acc[ab][:], [t_acc[ab]], [t_yacc[t]], t_acc[ab])
                    else:
                        P.op("dve", lambda ee, ab=ab, xb3=xb3: ee.scalar_tensor_tensor(
                            out=acc[ab][:], in0=x3t[xb3][:], scalar=DN_ALPHA, in1=acc[ab][:], op0=ALU.mult, op1=ALU.add),
                            reads=[t_x3t[xb3]], writes=[t_acc[ab]])
                        emit_ln(k, acc[ab][:], t_acc[ab], L)
                        P.op("dve", lambda ee, ab=ab: ee.tensor_tensor(out=acc[ab][:], in0=acc[ab][:], in1=lng[:], op=ALU.mult),
                             reads=[t_lng], writes=[t_acc[ab]])
                        P.op("pool", lambda ee, ab=ab: ee.tensor_tensor(out=acc[ab][:], in0=acc[ab][:], in1=lnb[:], op=ALU.add),
                             reads=[t_lnb], writes=[t_acc[ab]])
                        ev = P.dma("sp", outv[t], acc[ab][:], [t_acc[ab]], [], t_acc[ab])
                        P.out_events.append(ev)
        P.flush()


_NC_CACHE = {}


def _core_inputs(inputs, b):
    f = lambda a: np.ascontiguousarray(np.asarray(a, dtype=np.float32))
    m = {"x": f(inputs["x"][b]), "c": f(inputs["c"][b:b + 1])}
    for n in ["ada_w", "w_in", "w_out", "ffn_w_gate", "ffn_w_up", "ffn_w_down"]:
        m["l0_" + n] = f(inputs["l0_" + n])
    for n in ["ada_b", "subln_w", "ln1_g", "ln1_b", "ln2_g", "ln2_b"]:
        m["l0_" + n] = f(inputs["l0_" + n])[None, :]
    m["l0_lam"] = f(np.concatenate([np.asarray(inputs["l0_lam_q1"]), np.asarray(inputs["l0_lam_k1"]),
                                    np.asarray(inputs["l0_lam_q2"]), np.asarray(inputs["l0_lam_k2"])]))[None, :]
    for n in ["ada_w", "a_re", "a_im", "b_re", "b_im", "w_glu", "router_w", "exp_w_gate", "exp_w_up", "exp_w_down"]:
        m["l1_" + n] = f(inputs["l1_" + n])
    for n in ["ada_b", "log_dt", "d_skip", "b_glu", "ln1_g", "ln1_b", "router_b", "ln2_g", "ln2_b"]:
        m["l1_" + n] = f(inputs["l1_" + n])[None, :]
    m["l1_c_re"] = f(inputs["l1_c_re"]).reshape(1024, 64)
    m["l1_c_im"] = f(inputs["l1_c_im"]).reshape(1024, 64)
    return m


def kernel(**inputs):
    if "nc" not in _NC_CACHE:
        _NC_CACHE["nc"] = build_nc()
    nc = _NC_CACHE["nc"]
    n = 8
    shared = _core_inputs(inputs, 0)
    in_maps = []
    for b in range(n):
        m = dict(shared)
        m["x"] = np.ascontiguousarray(np.asarray(inputs["x"][b], dtype=np.float32))
        m["c"] = np.ascontiguousarray(np.asarray(inputs["c"][b:b + 1], dtype=np.float32))
        in_maps.append(m)
    res = run_bass_kernel_spmd(nc, in_maps, core_ids=list(range(n)))
    return np.stack([np.asarray(r["out"], dtype=np.float32) for r in res.results], axis=0)
```

```python
import math
from contextlib import ExitStack
import numpy as np
import concourse.bass as bass
import concourse.mybir as mybir
from concourse.bass_utils import run_bass_kernel_spmd

F32 = mybir.dt.float32
BF16 = mybir.dt.bfloat16
I32 = mybir.dt.int32
AF = mybir.ActivationFunctionType
ALU = mybir.AluOpType
AX = mybir.AxisListType

D = 1024
S = 4096
NT = S // 128
KC = D // 128
DFF = 2816
FC = DFF // 128
NE = 8
DFE = 1408
FCE = DFE // 128
DN_ALPHA = (2.0 * 2) ** 0.25
EPS = 1e-5
LAM_INIT0 = 0.8 - 0.6 * math.exp(-0.3 * 0)
SLOPES = [2.0 ** (-8.0 * (i + 1) / 16.0) for i in range(16)]

ENGS = ("pe", "act", "dve", "pool", "sp")


class Tok:
    __slots__ = ("name", "w", "r", "sem", "cnt")

    def __init__(self, name, sem=None):
        self.name = name
        self.w = None
        self.r = {}
        self.sem = sem
        self.cnt = 0


class Prog:
    def __init__(self, nc, stack):
        self.nc = nc
        self.stack = stack
        self.sem = {e: stack.enter_context(nc.semaphore("eng_" + e)) for e in ENGS}
        self.cnt = {e: 0 for e in ENGS}
        self.waited = {e: {} for e in ENGS}
        self.q = {e: [] for e in ENGS}
        self.ndsem = 0
        self.out_events = []

    def tok(self, name, dma=False):
        t = Tok(name)
        if dma:
            t.sem = self.stack.enter_context(self.nc.semaphore("d%d_%s" % (self.ndsem, name)))
            self.ndsem += 1
        return t

    def _wait(self, eng, deps, skip_own):
        own = self.sem[eng]
        for s, v in deps.items():
            if skip_own and s is own:
                if eng == "pe" or self.cnt[eng] - v >= 4:
                    continue
            if self.waited[eng].get(s, 0) >= v:
                continue
            self.waited[eng][s] = v
            self.q[eng].append(("wait", s, v))

    @staticmethod
    def _collect(reads, writes):
        deps = {}

        def add(ev):
            if ev is None:
                return
            s, v = ev
            if deps.get(s, 0) < v:
                deps[s] = v
        for t in reads:
            add(t.w)
            if t.name[0] == "p" and t.name != "post":
                for s, v in t.r.items():
                    add((s, v))
        for t in writes:
            add(t.w)
            for s, v in t.r.items():
                add((s, v))
        return deps

    mute = False

    def op(self, eng, fn, reads=(), writes=(), inc=True):
        if self.mute:
            return
        deps = self._collect(reads, writes)
        self._wait(eng, deps, True)
        own = self.sem[eng]
        if inc:
            self.cnt[eng] += 1
            n = self.cnt[eng]
        else:
            n = self.cnt[eng] + 1
        self.q[eng].append(("op", fn, own if inc else None))
        for t in reads:
            if t.name[0] == "p" and t.name != "post":
                t.w = (own, n)
                t.r = {}
            elif t.r.get(own, 0) < n:
                t.r[own] = n
        for t in writes:
            t.w = (own, n)
            t.r = {}

    def dma(self, eng, out, in_, reads, writes, st, **kw):
        if self.mute:
            return (st.sem, st.cnt)
        deps = self._collect(reads, writes)
        self._wait(eng, deps, False)
        st.cnt += 16
        sem, v = st.sem, st.cnt
        self.q[eng].append(("dma", out, in_, sem, kw))
        for t in reads:
            if t.r.get(sem, 0) < v:
                t.r[sem] = v
        for t in writes:
            t.w = (sem, v)
            t.r = {}
        return (sem, v)

    def flush(self, final_waits=()):
        nc = self.nc
        q = self.q
        self.q = {e: [] for e in ENGS}

        def replay(e, lst, extra=()):
            for it in lst:
                k = it[0]
                if k == "wait":
                    e.wait_ge(it[1], it[2])
                elif k == "op":
                    ins = it[1](e)
                    if it[2] is not None:
                        ins.then_inc(it[2], 1)
                else:
                    e.dma_start(out=it[1], in_=it[2], **it[4]).then_inc(it[3], 16)
            for s, v in extra:
                e.wait_ge(s, v)

        with nc.Block() as block:
            @block.tensor
            def _(e):
                replay(e, q["pe"])

            @block.scalar
            def _(e):
                replay(e, q["act"])

            @block.vector
            def _(e):
                replay(e, q["dve"])

            @block.gpsimd
            def _(e):
                replay(e, q["pool"])

            @block.sync
            def _(e):
                replay(e, q["sp"], final_waits)


class K:
    pass


def build_nc(debug=None, phases=("0", "A", "B", "C", "D0", "D1", "D1b", "D2"), x2_input=False, opts=None):
    nc = bass.Bass("TRN2", target_bir_lowering=False)
    k = K()
    k.nc = nc
    k.debug = debug
    for kk_, vv_ in (opts or {}).items():
        setattr(k, kk_, vv_)

    def din(name, shape):
        return nc.dram_tensor(name, list(shape), F32, kind="ExternalInput").ap()

    k.x = din("x", [S, D])
    k.c = din("c", [1, D])
    k.l0 = dict(
        ada_w=din("l0_ada_w", [D, 6 * D]), ada_b=din("l0_ada_b", [1, 6 * D]),
        w_in=din("l0_w_in", [D, 3 * D]),
        lam=din("l0_lam", [1, 128]),
        subln_w=din("l0_subln_w", [1, 64]),
        w_out=din("l0_w_out", [D, D]),
        ln1_g=din("l0_ln1_g", [1, D]), ln1_b=din("l0_ln1_b", [1, D]),
        w_gate=din("l0_ffn_w_gate", [D, DFF]), w_up=din("l0_ffn_w_up", [D, DFF]),
        w_down=din("l0_ffn_w_down", [DFF, D]),
        ln2_g=din("l0_ln2_g", [1, D]), ln2_b=din("l0_ln2_b", [1, D]),
    )
    k.l1 = dict(
        ada_w=din("l1_ada_w", [D, 6 * D]), ada_b=din("l1_ada_b", [1, 6 * D]),
        a_re=din("l1_a_re", [64, 64]), a_im=din("l1_a_im", [64, 64]), log_dt=din("l1_log_dt", [1, 64]),
        b_re=din("l1_b_re", [64, 64, 16]), b_im=din("l1_b_im", [64, 64, 16]),
        c_re=din("l1_c_re", [1024, 64]), c_im=din("l1_c_im", [1024, 64]),
        d_skip=din("l1_d_skip", [1, D]),
        w_glu=din("l1_w_glu", [D, 2 * D]), b_glu=din("l1_b_glu", [1, 2 * D]),
        ln1_g=din("l1_ln1_g", [1, D]), ln1_b=din("l1_ln1_b", [1, D]),
        router_w=din("l1_router_w", [D, NE]), router_b=din("l1_router_b", [1, NE]),
        w_gate=din("l1_exp_w_gate", [NE, D, DFE]), w_up=din("l1_exp_w_up", [NE, D, DFE]),
        w_down=din("l1_exp_w_down", [NE, DFE, D]),
        ln2_g=din("l1_ln2_g", [1, D]), ln2_b=din("l1_ln2_b", [1, D]),
    )
    k.out = nc.dram_tensor("out", [S, D], F32, kind="ExternalOutput").ap()

    k.qT_s = nc.dram_tensor("qT_s", [4 * 512, S], BF16).ap()
    k.v_s = nc.dram_tensor("v_s", [S, D], BF16).ap()
    k.o_s = nc.dram_tensor("o_s", [S, D], BF16).ap()
    if x2_input:
        k.x2_s = din("x2_in", [S, D])
    else:
        k.x2_s = nc.dram_tensor("x2_s", [S, D], F32).ap()
    k.mod_s = nc.dram_tensor("mod_s", [1, 6 * D], F32).ap()
    k.gpad_s = nc.dram_tensor("gpad_s", [64, 16, 63 * 16], BF16).ap()
    k.yT_s = nc.dram_tensor("yT_s", [8, 128, 32, 128], BF16).ap()
    k.x3_s = nc.dram_tensor("x3_s", [32, 128, D], F32).ap()
    k.hmT_s = nc.dram_tensor("hmT_s", [D, S], BF16).ap()
    k.cw_s = nc.dram_tensor("cw_s", [32, 128, NE], F32).ap()
    k.yacc_s = nc.dram_tensor("yacc_s", [32, 128, D], F32).ap()
    k.aug_s = nc.dram_tensor("aug_s", [3, 16, 512], BF16).ap()
    k.x1_s = nc.dram_tensor("x1_s", [S, D], F32).ap()
    k.h2T_s = nc.dram_tensor("h2T_s", [D, S], BF16).ap()

    if debug:
        k.dbg = {}
        for name, shape, dt in debug:
            k.dbg[name] = nc.dram_tensor("dbg_" + name, list(shape), dt, kind="ExternalOutput").ap()

    with ExitStack() as gs:
        P = Prog(nc, gs)
        k.P = P
        k.gs = gs
        setup_consts(k)
        with ExitStack() as cB:
            if "B" in phases:
                attn_consts_alloc(k, cB)
            if "0" in phases:
                phase0_mod(k, 0)
            if "A" in phases:
                phaseA(k)
            if "B" in phases:
                phaseB(k)
        if "C" in phases:
            phaseC(k)
        if "D0" in phases:
            phase0_mod(k, 1)
        if "D1" in phases:
            phaseD1(k)
        if "D1b" in phases:
            phaseD1b(k)
        if "D2" in phases:
            phaseD2(k)
        P.flush(final_waits=P.out_events)
    return nc


_UNIQ = [0]


def sb(k, stack, name, shape, dt):
    _UNIQ[0] += 1
    return stack.enter_context(k.nc.sbuf_tensor("%s_%d" % (name, _UNIQ[0]), list(shape), dt))


def ps(k, stack, name, shape, dt=F32):
    _UNIQ[0] += 1
    return stack.enter_context(k.nc.psum_tensor("%s_%d" % (name, _UNIQ[0]), list(shape), dt))


def setup_consts(k):
    nc, P, gs = k.nc, k.P, k.gs
    k.ident_f = sb(k, gs, "ident_f", [128, 128], F32)
    k.ident_b = sb(k, gs, "ident_b", [128, 128], BF16)
    k.t_ident = P.tok("ident")
    idf, idb = k.ident_f, k.ident_b

    P.op("pool", lambda e: e.memset(idf[:], 1.0), writes=[k.t_ident])
    P.op("pool", lambda e: e.affine_select(out=idf[:], in_=idf[:], pattern=[[-1, 128]], compare_op=ALU.is_equal,
                                           fill=0.0, base=0, channel_multiplier=1),
         reads=[k.t_ident], writes=[k.t_ident])
    P.op("pool", lambda e: e.tensor_copy(out=idb[:], in_=idf[:]), reads=[k.t_ident], writes=[k.t_ident])
    k.modT = [sb(k, gs, "modT%d" % l, [128, 48], F32) for l in range(2)]
    k.t_modT = [P.tok("modT%d" % l) for l in range(2)]
    k.gbc = sb(k, gs, "gbc", [128, 2, D], F32)
    k.t_gbc = P.tok("gbc")


def phase0_mod(k, l):
    nc, P = k.nc, k.P
    prm = k.l0 if l == 0 else k.l1
    with ExitStack() as ph:
        c_col = sb(k, ph, "c_col", [128, KC], F32)
        sc_bc = sb(k, ph, "sc_bc", [128, KC, 128], F32)
        ab = sb(k, ph, "ada_b_sb", [1, 6 * D], F32)
        ones1 = sb(k, ph, "ones1", [1, 128], F32)
        wst = [sb(k, ph, "ada_st%d" % i, [128, KC, 512], F32) for i in range(2)]
        modbc = sb(k, ph, "modbc", [128, 6 * D], F32)
        pmm = [ps(k, ph, "pmod%d" % i, [128, 512]) for i in range(2)]
        ptr = ps(k, ph, "ptr_mod", [128, 512])
        t_c = P.tok("c_col", dma=True)
        t_ab = P.tok("ada_b", dma=True)
        t_w = [P.tok("ada_st%d" % i, dma=True) for i in range(2)]
        t_scbc = P.tok("sc_bc")
        t_ones = P.tok("ones1")
        t_pmm = [P.tok("pmod%d" % i) for i in range(2)]
        t_ptr = P.tok("ptr")
        t_modbc = P.tok("modbc")

        P.dma("sp", c_col[:], k.c.rearrange("o (kc p) -> p (o kc)", p=128), [], [t_c], t_c,
              allow_slow_non_contiguous=True)
        P.dma("sp", ab[:], prm["ada_b"], [], [t_ab], t_ab)
        P.op("act", lambda e: e.activation(out=c_col[:], in_=c_col[:], func=AF.Silu), reads=[t_c], writes=[t_c])
        P.op("dve", lambda e: e.memset(ones1[:], 1.0), writes=[t_ones])
        for kc in range(KC):
            P.op("dve", lambda e, kc=kc: e.tensor_copy(out=sc_bc[:, kc, :],
                                                      in_=c_col[:, kc:kc + 1].broadcast_to([128, 128])),
                 reads=[t_c], writes=[t_scbc])
        wv = prm["ada_w"].rearrange("(kc p) n -> p kc n", p=128)
        for n in range(12):
            b = n % 2
            P.dma("sp", wst[b][:], wv[:, :, n * 512:(n + 1) * 512], [], [t_w[b]], t_w[b])
            for kc in range(KC):
                P.op("pe", lambda e, b=b, kc=kc: e.matmul(pmm[b][:], lhsT=sc_bc[:, kc, :], rhs=wst[b][:, kc, :],
                                                         start=(kc == 0), stop=False),
                     reads=[t_scbc, t_w[b]], writes=[t_pmm[b]], inc=False)
            P.op("pe", lambda e, b=b, n=n: e.matmul(pmm[b][:], lhsT=ones1[0:1, :], rhs=ab[0:1, n * 512:(n + 1) * 512],
                                                   start=False, stop=True),
                 reads=[t_ones, t_ab], writes=[t_pmm[b]])
            P.op("dve", lambda e, b=b, n=n: e.tensor_copy(out=modbc[:, n * 512:(n + 1) * 512], in_=pmm[b][:]),
                 reads=[t_pmm[b]], writes=[t_modbc])
        gbc = k.gbc
        P.op("dve", lambda e: e.tensor_scalar_add(out=gbc[:, 0, :], in0=modbc[:, 2 * D:3 * D], scalar1=1.0),
             reads=[t_modbc], writes=[k.t_gbc])
        P.op("dve", lambda e: e.tensor_scalar_add(out=gbc[:, 1, :], in0=modbc[:, 5 * D:6 * D], scalar1=1.0),
             reads=[t_modbc], writes=[k.t_gbc])
        if l == 1:
            t_ms = P.tok("modrow", dma=True)
            P.dma("sp", k.mod_s, modbc[0:1, :], [t_modbc], [], t_ms)
            dma_barrier(P, "sp", [t_ms])
        modT = k.modT[l]
        idf = k.ident_f
        for g4 in range(12):
            for i in range(4):
                j = g4 * 4 + i
                P.op("pe", lambda e, i=i, j=j: e.transpose(ptr[:, i * 128:(i + 1) * 128],
                                                          modbc[:, j * 128:(j + 1) * 128], idf[:]),
                     reads=[t_modbc, k.t_ident], writes=[t_ptr], inc=(i == 3))
            P.op("dve", lambda e, g4=g4: e.tensor_copy(
                out=modT[:, g4 * 4:(g4 + 1) * 4],
                in_=ptr[:].rearrange("p (i c) -> p i c", c=128)[:, :, 0]),
                reads=[t_ptr], writes=[k.t_modT[l]])
        P.op("dve", lambda e: e.tensor_scalar_add(out=modT[:, 8:16], in0=modT[:, 8:16], scalar1=1.0),
             reads=[k.t_modT[l]], writes=[k.t_modT[l]])
        P.op("dve", lambda e: e.tensor_scalar_add(out=modT[:, 32:40], in0=modT[:, 32:40], scalar1=1.0),
             reads=[k.t_modT[l]], writes=[k.t_modT[l]])
        if k.debug and ("modT%d" % l) in k.dbg:
            t_d = P.tok("dbgmod", dma=True)
            ev = P.dma("sp", k.dbg["modT%d" % l], modT[:], [k.t_modT[l]], [], t_d)
            P.out_events.append(ev)
        if l == 0 and getattr(k, "BC", None) is not None:
            with ExitStack() as tmpc:
                attn_consts_gen(k, tmpc)
                P.flush()
        else:
            P.flush()


def dma_barrier(P, eng, toks):
    for t in toks:
        if t.cnt > 0 and P.waited[eng].get(t.sem, 0) < t.cnt:
            P.waited[eng][t.sem] = t.cnt
            P.q[eng].append(("wait", t.sem, t.cnt))


def load_cast_weight(k, dst, src_view, tok, nchunks, eng="pool"):
    P = k.P
    A = dst.shape[1]
    step = (A + nchunks - 1) // nchunks
    for a0 in range(0, A, step):
        a1 = min(A, a0 + step)
        P.dma(eng, dst[:, a0:a1, :], src_view[:, a0:a1, :], [], [tok], tok)


def phaseA(k):
    nc, P = k.nc, k.P
    modT = k.modT[0]
    with ExitStack() as ph:
        w_in = sb(k, ph, "w_in_sb", [128, KC, 3 * D], BF16)
        xs = [sb(k, ph, "xA%d" % i, [128, 4, D], F32) for i in range(2)]
        hT = [sb(k, ph, "hT%d" % i, [128, KC, 512], BF16) for i in range(2)]
        qt = [sb(k, ph, "qtA%d" % i, [128, 512], BF16) for i in range(4)]
        vsb = [sb(k, ph, "vA%d" % i, [128, D], BF16) for i in range(2)]
        ptr = [ps(k, ph, "ptrA%d" % i, [128, 512]) for i in range(2)]
        pm = [ps(k, ph, "pmA%d" % i, [128, 512]) for i in range(3)]
        t_win = P.tok("w_in", dma=True)
        t_x = [P.tok("xA%d" % i, dma=True) for i in range(2)]
        t_hT = [P.tok("hT%d" % i) for i in range(2)]
        t_qt = [P.tok("qt%d" % i, dma=True) for i in range(4)]
        t_v = [P.tok("vsb%d" % i, dma=True) for i in range(2)]
        t_ptr = [P.tok("ptrA%d" % i) for i in range(2)]
        t_pm = [P.tok("pmA%d" % i) for i in range(3)]
        t_scr = P.tok("scrA")

        load_cast_weight(k, w_in, k.l0["w_in"].rearrange("(kc p) n -> p kc n", p=128), t_win, 8)
        xv = k.x.rearrange("(g t p) d -> g p t d", p=128, t=4)
        idf = k.ident_f
        sec_col = [0, 512, 1536, 2048]
        sec_scale = [32 ** -0.5, 1.0, 64 ** -0.5, 1.0]
        v_col = [1024, 2560]
        ipm = 0
        iqt = 0
        itr = 0
        P.dma("sp", xs[0][:], xv[0], [], [t_x[0]], t_x[0])
        for g in range(8):
            b = g % 2
            if g + 1 < 8:
                P.dma("sp", xs[1 - b][:], xv[g + 1], [], [t_x[1 - b]], t_x[1 - b])
            for kc in range(KC):
                tb = itr % 2
                itr += 1
                for tt in range(4):
                    P.op("pe", lambda e, tb=tb, tt=tt, kc=kc, b=b: e.transpose(
                        ptr[tb][:, tt * 128:(tt + 1) * 128], xs[b][:, tt, kc * 128:(kc + 1) * 128], idf[:]),
                        reads=[t_x[b], k.t_ident], writes=[t_ptr[tb]], inc=(tt == 3))
                P.op("act", lambda e, tb=tb, kc=kc, b=b: e.activation(
                    out=hT[b][:, kc, :], in_=ptr[tb][:], func=AF.Identity,
                    scale=modT[:, 8 + kc:9 + kc], bias=modT[:, kc:kc + 1]),
                    reads=[t_ptr[tb], k.t_modT[0]], writes=[t_hT[b]])
            for sec in range(4):
                for f in range(4):
                    pb = ipm % 3
                    ipm += 1
                    c0 = sec_col[sec] + f * 128
                    for kc in range(KC):
                        P.op("pe", lambda e, pb=pb, kc=kc, c0=c0, b=b: e.matmul(
                            pm[pb][:], lhsT=w_in[:, kc, c0:c0 + 128], rhs=hT[b][:, kc, :],
                            start=(kc == 0), stop=(kc == KC - 1)),
                            reads=[t_win, t_hT[b]], writes=[t_pm[pb]], inc=(kc == KC - 1))
                    qb = iqt % 4
                    iqt += 1
                    sc = sec_scale[sec]
                    if (sec * 4 + f) % 2 == 0:
                        P.op("act", lambda e, qb=qb, pb=pb, sc=sc: e.activation(
                            out=qt[qb][:], in_=pm[pb][:], func=AF.Copy, scale=sc),
                            reads=[t_pm[pb]], writes=[t_qt[qb]])
                    else:
                        P.op("dve", lambda e, qb=qb, pb=pb, sc=sc: e.tensor_scalar_mul(
                            out=qt[qb][:], in0=pm[pb][:], scalar1=sc),
                            reads=[t_pm[pb]], writes=[t_qt[qb]])
                    r0 = sec * 512 + f * 128
                    P.dma("sp", k.qT_s[r0:r0 + 128, g * 512:(g + 1) * 512], qt[qb][:], [t_qt[qb]], [t_scr], t_qt[qb])
            for tt in range(4):
                vb = (g * 4 + tt) % 2
                for vs in range(2):
                    pb = ipm % 3
                    ipm += 1
                    c0 = v_col[vs]
                    for kc in range(KC):
                        P.op("pe", lambda e, pb=pb, kc=kc, c0=c0, b=b, tt=tt: e.matmul(
                            pm[pb][:], lhsT=hT[b][:, kc, tt * 128:(tt + 1) * 128], rhs=w_in[:, kc, c0:c0 + 512],
                            start=(kc == 0), stop=(kc == KC - 1)),
                            reads=[t_win, t_hT[b]], writes=[t_pm[pb]], inc=(kc == KC - 1))
                    if vs == 0:
                        P.op("act", lambda e, vb=vb, pb=pb: e.activation(
                            out=vsb[vb][:, 0:512], in_=pm[pb][:], func=AF.Copy),
                            reads=[t_pm[pb]], writes=[t_v[vb]])
                    else:
                        P.op("dve", lambda e, vb=vb, pb=pb: e.tensor_copy(
                            out=vsb[vb][:, 512:1024], in_=pm[pb][:]),
                            reads=[t_pm[pb]], writes=[t_v[vb]])
                t0 = g * 512 + tt * 128
                P.dma("sp", k.v_s[t0:t0 + 128, :], vsb[vb][:], [t_v[vb]], [t_scr], t_v[vb])
        dma_barrier(P, "sp", t_qt + t_v)
        if k.debug and "qT" in k.dbg:
            big = sb(k, ph, "dbgbig", [128, 16, 1024], BF16)
            t_big = P.tok("dbgbig", dma=True)
            for c4 in range(4):
                P.dma("sp", big[:], k.qT_s.rearrange("(a p) t -> p a t", p=128)[:, :, c4 * 1024:(c4 + 1) * 1024], [t_scr], [t_big], t_big)
                ev = P.dma("sp", k.dbg["qT"].rearrange("(a p) t -> p a t", p=128)[:, :, c4 * 1024:(c4 + 1) * 1024], big[:], [t_big], [], t_big)
            for c2 in range(2):
                P.dma("sp", big[:], k.v_s.rearrange("(a p) t -> p a t", p=128)[:, c2 * 16:(c2 + 1) * 16, :], [t_scr], [t_big], t_big)
                ev = P.dma("sp", k.dbg["v"].rearrange("(a p) t -> p a t", p=128)[:, c2 * 16:(c2 + 1) * 16, :], big[:], [t_big], [], t_big)
            P.out_events.append(ev)
        P.flush()


def attn_consts_alloc(k, ph):
    nc, P = k.nc, k.P
    aug = sb(k, ph, "aug", [128, 16, 512], BF16)
    t_aug = P.tok("aug", dma=True)
    bt = sb(k, ph, "bt", [128, 16, 32], F32)
    t_bt = P.tok("bt")
    negmask = sb(k, ph, "negmask", [128, 128], F32)
    t_negmask = P.tok("negmask")
    tm = sb(k, ph, "tm", [128, 2944], BF16)
    t_tm = P.tok("tm")
    zer = sb(k, ph, "zer", [1, 512], BF16)
    t_zer = P.tok("zer")
    neglam = sb(k, ph, "neglam", [128, 1], F32)
    t_neglam = P.tok("neglam")
    wsub = sb(k, ph, "wsub", [128, 64], F32)
    t_wsub = P.tok("wsub", dma=True)
    epsb = sb(k, ph, "epsb", [128, 1], F32)
    t_eps = P.tok("epsb")
    k.BC = dict(aug=aug, t_aug=t_aug, bt=bt, t_bt=t_bt, negmask=negmask, t_negmask=t_negmask, tm=tm, t_tm=t_tm, zer=zer, t_zer=t_zer, neglam=neglam, t_neglam=t_neglam, wsub=wsub, t_wsub=t_wsub, epsb=epsb, t_eps=t_eps)


def attn_consts_gen(k, tmp):
    nc, P = k.nc, k.P
    aug = k.BC['aug']
    t_aug = k.BC['t_aug']
    bt = k.BC['bt']
    t_bt = k.BC['t_bt']
    negmask = k.BC['negmask']
    t_negmask = k.BC['t_negmask']
    tm = k.BC['tm']
    t_tm = k.BC['t_tm']
    zer = k.BC['zer']
    t_zer = k.BC['t_zer']
    neglam = k.BC['neglam']
    t_neglam = k.BC['t_neglam']
    wsub = k.BC['wsub']
    t_wsub = k.BC['t_wsub']
    epsb = k.BC['epsb']
    t_eps = k.BC['t_eps']
    io = sb(k, tmp, "io", [16, 512], F32)
    slc = sb(k, tmp, "slc", [16, 1], F32)
    val = sb(k, tmp, "val", [16, 512], F32)
    hi = sb(k, tmp, "hi", [16, 512], BF16)
    mid = sb(k, tmp, "mid", [16, 512], BF16)
    lo = sb(k, tmp, "lo", [16, 512], BF16)
    hif = sb(k, tmp, "hif", [16, 512], F32)
    t_t = P.tok("augtmp")
    t_hi = P.tok("hi"); t_mid = P.tok("mid"); t_lo = P.tok("lo")
    P.op("pool", lambda e: e.iota(io[:], [[1, 512]], base=0, channel_multiplier=0,
                                  allow_small_or_imprecise_dtypes=True), writes=[t_t])
    P.op("pool", lambda e: e.iota(slc[:], [[1, 1]], base=1, channel_multiplier=1,
                                  allow_small_or_imprecise_dtypes=True), writes=[t_t])
    P.op("act", lambda e: e.activation(out=slc[:], in_=slc[:], func=AF.Exp, scale=-0.5 * math.log(2.0)),
         reads=[t_t], writes=[t_t])
    P.op("dve", lambda e: e.tensor_scalar(out=val[:], in0=io[:], scalar1=slc[:, 0:1], scalar2=-1.0,
                                          op0=ALU.mult, op1=ALU.mult), reads=[t_t], writes=[t_t])
    P.op("dve", lambda e: e.tensor_copy(out=hi[:], in_=val[:]), reads=[t_t], writes=[t_hi])
    P.op("dve", lambda e: e.tensor_copy(out=hif[:], in_=hi[:]), reads=[t_hi], writes=[t_t])
    P.op("dve", lambda e: e.tensor_sub(out=val[:], in0=val[:], in1=hif[:]), reads=[t_t], writes=[t_t])
    P.op("dve", lambda e: e.tensor_copy(out=mid[:], in_=val[:]), reads=[t_t], writes=[t_mid])
    P.op("dve", lambda e: e.tensor_copy(out=hif[:], in_=mid[:]), reads=[t_mid], writes=[t_t])
    P.op("dve", lambda e: e.tensor_sub(out=val[:], in0=val[:], in1=hif[:]), reads=[t_t], writes=[t_t])
    P.op("dve", lambda e: e.tensor_copy(out=lo[:], in_=val[:]), reads=[t_t], writes=[t_lo])
    t_augs = P.tok("aug_s")
    t_hi.sem = t_mid.sem = t_lo.sem = None
    t_hml = P.tok("hml", dma=True)
    P.dma("sp", k.aug_s[0], hi[:], [t_hi], [t_augs], t_hml)
    P.dma("sp", k.aug_s[1], mid[:], [t_mid], [t_augs], t_hml)
    P.dma("sp", k.aug_s[2], lo[:], [t_lo], [t_augs], t_hml)
    dma_barrier(P, "sp", [t_hml])
    for base in (32, 96, 64):
        for r in range(3):
            P.dma("sp", aug[base + r:base + r + 1], k.aug_s[r:r + 1], [t_augs], [t_aug], t_aug)
    bti = sb(k, tmp, "bti", [128, 32], F32)
    P.op("pool", lambda e: e.iota(bti[:], [[128, 32]], base=-28 * 128, channel_multiplier=1,
                                  allow_small_or_imprecise_dtypes=True), writes=[t_bt])
    for h in range(16):
        P.op("dve", lambda e, h=h: e.tensor_scalar_mul(out=bt[:, h, :], in0=bti[:], scalar1=SLOPES[h]),
             reads=[t_bt], writes=[t_bt])
    P.op("pool", lambda e: e.memset(negmask[:], 0.0), writes=[t_negmask])
    P.op("pool", lambda e: e.affine_select(out=negmask[:], in_=negmask[:], pattern=[[1, 128]],
                                           compare_op=ALU.is_ge, fill=-30000.0, base=0,
                                           channel_multiplier=-1),
         reads=[t_negmask], writes=[t_negmask])
    dl = sb(k, tmp, "dl", [128, 2944], I32)
    dlf = sb(k, tmp, "dlf", [128, 2944], F32)
    a_i = sb(k, tmp, "a_i", [128, 2944], I32)
    m1 = sb(k, tmp, "m1", [128, 2944], F32)
    m2 = sb(k, tmp, "m2", [128, 2944], F32)
    acc = sb(k, tmp, "acc", [128, 2944], F32)
    t_dl = P.tok("dl")
    P.op("pool", lambda e: e.iota(dl[:], [[1, 2944]], base=-384, channel_multiplier=-1), writes=[t_dl])
    P.op("dve", lambda e: e.tensor_copy(out=dlf[:], in_=dl[:]), reads=[t_dl], writes=[t_dl])
    P.op("dve", lambda e: e.tensor_scalar(out=m1[:], in0=dlf[:], scalar1=0.0, scalar2=None, op0=ALU.is_ge),
         reads=[t_dl], writes=[t_dl])
    P.op("dve", lambda e: e.tensor_scalar(out=m2[:], in0=dlf[:], scalar1=128.0, scalar2=None, op0=ALU.is_le),
         reads=[t_dl], writes=[t_dl])
    P.op("dve", lambda e: e.tensor_mul(out=acc[:], in0=m1[:], in1=m2[:]), reads=[t_dl], writes=[t_dl])
    for dd, win in ((4, 512.0), (16, 2048.0)):
        P.op("dve", lambda e, dd=dd: e.tensor_single_scalar(out=a_i[:], in_=dl[:], scalar=dd - 1,
                                                            op=ALU.bitwise_and),
             reads=[t_dl], writes=[t_dl])
        P.op("dve", lambda e: e.tensor_copy(out=m2[:], in_=a_i[:]), reads=[t_dl], writes=[t_dl])
        P.op("dve", lambda e: e.tensor_scalar(out=m2[:], in0=m2[:], scalar1=0.0, scalar2=None,
                                              op0=ALU.is_equal), reads=[t_dl], writes=[t_dl])
        P.op("dve", lambda e: e.tensor_mul(out=m2[:], in0=m2[:], in1=m1[:]), reads=[t_dl], writes=[t_dl])
        P.op("dve", lambda e, win=win: e.scalar_tensor_tensor(out=m2[:], in0=dlf[:], scalar=win, in1=m2[:],
                                                              op0=ALU.is_le, op1=ALU.mult),
             reads=[t_dl], writes=[t_dl])
        P.op("dve", lambda e: e.tensor_add(out=acc[:], in0=acc[:], in1=m2[:]), reads=[t_dl], writes=[t_dl])
    P.op("dve", lambda e: e.tensor_copy(out=tm[:], in_=acc[:]), reads=[t_dl], writes=[t_tm])
    P.op("dve", lambda e: e.memset(zer[:], 0.0), writes=[t_zer])
    P.op("dve", lambda e: e.memset(epsb[:], EPS), writes=[t_eps])
    lam4 = sb(k, tmp, "lam4", [128, 128], F32)
    t_lam = P.tok("lam4", dma=True)
    P.dma("sp", lam4[:], k.l0["lam"].partition_broadcast(128), [], [t_lam], t_lam)
    pr = sb(k, tmp, "lampr", [128, 2, 32], F32)
    s2 = sb(k, tmp, "lams2", [128, 2], F32)
    P.op("dve", lambda e: e.tensor_mul(out=pr[:, 0, :], in0=lam4[:, 0:32], in1=lam4[:, 32:64]),
         reads=[t_lam], writes=[t_neglam])
    P.op("dve", lambda e: e.tensor_mul(out=pr[:, 1, :], in0=lam4[:, 64:96], in1=lam4[:, 96:128]),
         reads=[t_lam], writes=[t_neglam])
    P.op("dve", lambda e: e.reduce_sum(out=s2[:], in_=pr[:], axis=AX.X), reads=[t_neglam], writes=[t_neglam])
    P.op("act", lambda e: e.activation(out=s2[:], in_=s2[:], func=AF.Exp), reads=[t_neglam], writes=[t_neglam])
    P.op("dve", lambda e: e.tensor_sub(out=neglam[:], in0=s2[:, 1:2], in1=s2[:, 0:1]),
         reads=[t_neglam], writes=[t_neglam])
    P.op("dve", lambda e: e.tensor_scalar_add(out=neglam[:], in0=neglam[:], scalar1=-LAM_INIT0),
         reads=[t_neglam], writes=[t_neglam])
    P.dma("sp", wsub[:], k.l0["subln_w"].partition_broadcast(128), [], [t_wsub], t_wsub)
    P.op("dve", lambda e: e.tensor_scalar_mul(out=wsub[:], in0=wsub[:], scalar1=1.0 - LAM_INIT0),
         reads=[t_wsub], writes=[t_wsub])


def phaseB(k):
    nc, P = k.nc, k.P
    aug = k.BC['aug']
    t_aug = k.BC['t_aug']
    bt = k.BC['bt']
    t_bt = k.BC['t_bt']
    negmask = k.BC['negmask']
    t_negmask = k.BC['t_negmask']
    tm = k.BC['tm']
    t_tm = k.BC['t_tm']
    zer = k.BC['zer']
    t_zer = k.BC['t_zer']
    neglam = k.BC['neglam']
    t_neglam = k.BC['t_neglam']
    wsub = k.BC['wsub']
    t_wsub = k.BC['t_wsub']
    epsb = k.BC['epsb']
    t_eps = k.BC['t_eps']
    with ExitStack() as ph:
        QT = [[sb(k, ph, "QT%d_%d" % (i, m), [128, S], BF16) for m in range(2)] for i in range(2)]
        KT = [sb(k, ph, "KT%d" % i, [128, S], BF16) for i in range(2)]
        VT = [sb(k, ph, "VT%d" % i, [128, NT, 65], BF16) for i in range(2)]
        PT = [sb(k, ph, "PT%d" % i, [128, 512], BF16) for i in range(6)]
        pst = [ps(k, ph, "pst%d" % i, [128, 512]) for i in range(3)]
        t_post = P.tok("post")
        pO = [[ps(k, ph, "pO%d_%d" % (i, m), [128, 512]) for m in range(2)] for i in range(2)]
        pTr = [ps(k, ph, "pTr%d" % m, [128, 512]) for m in range(1)] * 2
        t_pTr = [P.tok("pTr%d" % m) for m in range(1)] * 2
        mhalfB = sb(k, ph, "mhalfB", [128, 4], F32)
        P.op("pool", lambda e: e.memset(mhalfB[:], -0.5), writes=[t_post])
        OTs = [sb(k, ph, "OTs%d" % m, [65, 512], F32) for m in range(2)]
        t_OTs = [P.tok("OTs%d" % m) for m in range(2)]
        idf65 = k.ident_f[0:65, 0:65]
        odf = sb(k, ph, "odf", [128, 4, 64], F32)
        otmp = sb(k, ph, "otmp", [128, 4, 64], F32)
        rr = sb(k, ph, "rr", [128, 8], F32)
        ss = sb(k, ph, "ss", [128, 4], F32)
        ob = [sb(k, ph, "ob%d" % i, [128, 4, 64], BF16) for i in range(2)]
        t_Q = [P.tok("QT%d" % i, dma=True) for i in range(2)]
        t_K = [P.tok("KT%d" % i, dma=True) for i in range(2)]
        t_V = [P.tok("VT%d" % i, dma=True) for i in range(2)]
        t_PT = [P.tok("PT%d" % i) for i in range(6)]
        t_pst = [P.tok("pst%d" % i) for i in range(3)]
        t_pO = [[P.tok("pO%d_%d" % (i, m)) for m in range(2)] for i in range(2)]
        t_ob = [P.tok("ob%d" % i, dma=True) for i in range(2)]
        t_os = P.tok("o_s")
        for i in range(2):
            P.op("pool", lambda e, i=i: e.memset(VT[i][:, :, 64:65], 1.0), writes=[t_V[i]])
            P.op("pool", lambda e, i=i: e.memset(KT[i][:], 1.0), writes=[t_K[i]])
            for m in range(2):
                P.op("pool", lambda e, i=i, m=m: e.memset(QT[i][m][:], 0.0), writes=[t_Q[i]])

        NPT = len(PT)
        LA = NPT - 2

        def emit_loads(hh):
            diff = hh < 8
            h = hh if diff else hh - 8
            b = hh % 2
            if diff:
                for m in range(2):
                    r0 = h * 64 + m * 32
                    P.dma("sp", QT[b][m][64 * m:64 * m + 32, :], k.qT_s[r0:r0 + 32, :], [], [t_Q[b]], t_Q[b])
                    P.dma("sp", KT[b][64 * m:64 * m + 32, :], k.qT_s[512 + r0:512 + r0 + 32, :], [], [t_K[b]], t_K[b])
                for base in (32, 96):
                    P.op("pool", lambda e, b=b, hh=hh, base=base: e.tensor_copy(
                        out=QT[b][base // 64][base:base + 3, :].rearrange("p (c j) -> p c j", j=512),
                        in_=aug[base:base + 3, hh, :].unsqueeze(1).broadcast_to([3, 8, 512])),
                        reads=[t_aug], writes=[t_Q[b]])
                    P.op("pool", lambda e, b=b, base=base: e.memset(KT[b][base:base + 3, :], 1.0), writes=[t_K[b]])
                vcol = h * 64
            else:
                r0 = 1024 + h * 64
                P.dma("sp", QT[b][0][0:64, :], k.qT_s[r0:r0 + 64, :], [], [t_Q[b]], t_Q[b])
                P.dma("sp", KT[b][0:64, :], k.qT_s[512 + r0:512 + r0 + 64, :], [], [t_K[b]], t_K[b])
                P.op("pool", lambda e, b=b, hh=hh: e.tensor_copy(
                    out=QT[b][0][64:67, :].rearrange("p (c j) -> p c j", j=512),
                    in_=aug[64:67, hh, :].unsqueeze(1).broadcast_to([3, 8, 512])),
                    reads=[t_aug], writes=[t_Q[b]])
                P.op("pool", lambda e, b=b: e.memset(KT[b][64:67, :], 1.0), writes=[t_K[b]])
                vcol = 512 + h * 64
            P.dma("sp", VT[b][:, :, 0:64], k.v_s[:, vcol:vcol + 64].rearrange("(n p) d -> p n d", p=128),
                  [], [t_V[b]], t_V[b])

        tiles = []
        for hh in range(16):
            diff = hh < 8
            nm = 2 if diff else 1
            for qc in range(8):
                kb_lo = 0 if diff else max(0, 4 * qc - 16)
                lst = [(kb, m) for kb in range(kb_lo, 4 * qc + 4) for m in range(nm)]
                for idx, (kb, m) in enumerate(lst):
                    tiles.append(dict(hh=hh, qc=qc, kb=kb, m=m, first=(idx == 0), last=(idx == len(lst) - 1),
                                      head_first=(qc == 0 and idx == 0)))

        st = dict(ist=0, ipt=0, iob=0, cpar=0)

        def stage1(T):
            hh, qc, kb, m = T["hh"], T["qc"], T["kb"], T["m"]
            diff = hh < 8
            b = hh % 2
            kr = 35 if diff else 67
            j = kb - 4 * qc
            n0 = max(0, j)
            pb = 64 * m
            si = st["ist"] % len(pst)
            st["ist"] += 1
            P.op("pe", lambda e, si=si, b=b, m=m, kb=kb, qc=qc, n0=n0: e.matmul(
                pst[si][:, n0 * 128:512], lhsT=KT[b][:, kb * 128:(kb + 1) * 128],
                rhs=QT[b][m][:, qc * 512 + n0 * 128:(qc + 1) * 512], start=True, stop=True),
                reads=[t_K[b], t_Q[b]], writes=[t_pst[si]])
            if j >= 0 and diff:
                P.op("dve", lambda e, si=si, j=j: e.tensor_tensor(
                    out=pst[si][:, j * 128:(j + 1) * 128], in0=pst[si][:, j * 128:(j + 1) * 128],
                    in1=negmask[:], op=ALU.add),
                    reads=[t_negmask], writes=[t_pst[si]])
            pi = st["ipt"] % NPT
            st["ipt"] += 1
            T["pi"] = pi
            P.op("act", lambda e, si=si, pi=pi, n0=n0, hh=hh, j=j: e.activation(
                out=PT[pi][:, n0 * 128:512], in_=pst[si][:, n0 * 128:512], func=AF.Exp,
                bias=bt[:, hh, j + 28:j + 29]),
                reads=[t_pst[si], t_bt], writes=[t_PT[pi]])
            if not diff:
                x0 = (4 * qc - kb + 3) * 128 + n0 * 128
                eng = "pool" if (st["ipt"] % 2 == 0) else "dve"
                P.op(eng, lambda e, pi=pi, n0=n0, x0=x0: e.tensor_tensor(
                    out=PT[pi][:, n0 * 128:512], in0=PT[pi][:, n0 * 128:512],
                    in1=tm[:, x0:x0 + 512 - n0 * 128], op=ALU.mult),
                    reads=[t_tm], writes=[t_PT[pi]])

        def stage2(T):
            hh, qc, kb, m, pi = T["hh"], T["qc"], T["kb"], T["m"], T["pi"]
            diff = hh < 8
            b = hh % 2
            if T["first"]:
                st["cpar"] ^= 1
            ob_i = st["cpar"]
            n0 = max(0, kb - 4 * qc)
            kb_lo = 0 if diff else max(0, 4 * qc - 16)
            P.op("pe", lambda e, pi=pi, n0=n0, ob_i=ob_i, m=m, b=b, kb=kb, first=(kb == kb_lo): e.matmul(
                pO[ob_i][m][0:65, n0 * 128:512], lhsT=VT[b][:, kb, :], rhs=PT[pi][:, n0 * 128:512],
                start=first, stop=True, skip_group_check=True),
                reads=[t_PT[pi], t_V[b]], writes=[t_pO[ob_i][m]])
            if T["last"]:
                post(hh, qc, ob_i)

        def post_a(hh, qc, ob_i):
            nm = 2 if hh < 8 else 1
            for m in range(nm):
                P.op("dve", lambda e, ob_i=ob_i, m=m: e.tensor_copy(out=OTs[m][:], in_=pO[ob_i][m][0:65, :]),
                     reads=[t_pO[ob_i][m]], writes=[t_OTs[m]])

        def tr_map(m):
            for sub in range(4):
                P.op("pe", lambda e, m=m, sub=sub: e.transpose(
                    pTr[0][:, sub * 65:(sub + 1) * 65], OTs[m][:, sub * 128:(sub + 1) * 128], idf65),
                    reads=[t_OTs[m], k.t_ident], writes=[t_pTr[0]], inc=(sub == 3))

        def post_b(hh, qc):
            diff = hh < 8
            O0 = pTr[0][:, 0:260].rearrange("p (s c) -> p s c", c=65)
            obi = st["iob"] % 2
            st["iob"] += 1
            if diff:
                rd = [t_pTr[0], t_neglam, t_wsub, t_eps]
                tr_map(1)
                P.op("dve", lambda e: e.reciprocal(out=rr[:, 4:8], in_=O0[:, :, 64]), reads=rd, writes=[t_post])
                P.op("dve", lambda e: e.tensor_scalar_mul(out=rr[:, 4:8], in0=rr[:, 4:8], scalar1=neglam[:, 0:1]),
                     reads=rd, writes=[t_post])
                for sub in range(4):
                    P.op("dve", lambda e, sub=sub: e.tensor_scalar_mul(
                        out=otmp[:, sub, :], in0=O0[:, sub, 0:64], scalar1=rr[:, 4 + sub:5 + sub]),
                        reads=rd, writes=[t_post])
                tr_map(0)
                P.op("dve", lambda e: e.reciprocal(out=rr[:, 0:4], in_=O0[:, :, 64]), reads=rd, writes=[t_post])
                for sub in range(4):
                    P.op("dve", lambda e, sub=sub: e.scalar_tensor_tensor(
                        out=odf[:, sub, :], in0=O0[:, sub, 0:64], scalar=rr[:, sub:sub + 1], in1=otmp[:, sub, :],
                        op0=ALU.mult, op1=ALU.add), reads=rd, writes=[t_post])
                P.op("dve", lambda e: e.tensor_mul(out=otmp[:], in0=odf[:], in1=odf[:]), reads=rd, writes=[t_post])
                P.op("dve", lambda e: e.reduce_sum(out=ss[:], in_=otmp[:], axis=AX.X), reads=rd, writes=[t_post])
                P.op("dve", lambda e: e.tensor_scalar(out=ss[:], in0=ss[:], scalar1=1.0 / 64.0, scalar2=EPS,
                                                      op0=ALU.mult, op1=ALU.add), reads=rd, writes=[t_post])
                P.op("pool", lambda e: e.tensor_tensor(out=ss[:], in0=ss[:], in1=mhalfB[:], op=ALU.pow),
                     reads=[t_post], writes=[t_post])
                for sub in range(4):
                    P.op("dve", lambda e, sub=sub, obi=obi: e.scalar_tensor_tensor(
                        out=ob[obi][:, sub, :], in0=odf[:, sub, :], scalar=ss[:, sub:sub + 1], in1=wsub[:],
                        op0=ALU.mult, op1=ALU.mult), reads=[t_post, t_wsub], writes=[t_ob[obi]])
            else:
                tr_map(0)
                rd = [t_pTr[0]]
                P.op("dve", lambda e: e.reciprocal(out=rr[:, 0:4], in_=O0[:, :, 64]), reads=rd, writes=[t_post])
                for sub in range(4):
                    P.op("dve", lambda e, sub=sub, obi=obi: e.tensor_scalar_mul(
                        out=ob[obi][:, sub, :], in0=O0[:, sub, 0:64], scalar1=rr[:, sub:sub + 1]),
                        reads=rd + [t_post], writes=[t_ob[obi]])
            ocol = hh * 64
            P.dma("sp", k.o_s[qc * 512:(qc + 1) * 512, ocol:ocol + 64].rearrange("(s p) d -> p s d", p=128),
                  ob[obi][:], [t_ob[obi]], [t_os], t_ob[obi])

        pending = []

        def post(hh, qc, ob_i):
            while pending:
                _, h2, q2 = pending.pop(0)
                post_b(h2, q2)
            post_a(hh, qc, ob_i)
            pending.append([3, hh, qc])

        def tick():
            if pending:
                pending[0][0] -= 1
                if pending[0][0] <= 0:
                    _, h2, q2 = pending.pop(0)
                    post_b(h2, q2)

        emit_loads(0)
        emit_loads(1)
        for step in range(len(tiles) + LA):
            if step < len(tiles):
                stage1(tiles[step])
            if step >= LA:
                U = tiles[step - LA]
                if U["head_first"] and U["hh"] >= 1 and U["hh"] + 1 < 16:
                    emit_loads(U["hh"] + 1)
                tick()
                stage2(U)
        while pending:
            _, h2, q2 = pending.pop(0)
            post_b(h2, q2)
        dma_barrier(P, "sp", t_ob)
        if k.debug and "o" in k.dbg:
            big = sb(k, ph, "dbgbigB", [128, 16, 1024], BF16)
            t_big = P.tok("dbgbigB", dma=True)
            for c2 in range(2):
                P.dma("sp", big[:], k.o_s.rearrange("(a p) t -> p a t", p=128)[:, c2 * 16:(c2 + 1) * 16, :], [t_os], [t_big], t_big)
                ev = P.dma("sp", k.dbg["o"].rearrange("(a p) t -> p a t", p=128)[:, c2 * 16:(c2 + 1) * 16, :], big[:], [t_big], [], t_big)
            P.out_events.append(ev)
        P.flush()


class LNScratch:
    def __init__(self, k, stack, name):
        P = k.P
        self.stats = sb(k, stack, name + "_st", [128, 2, 6], F32)
        self.mv = sb(k, stack, name + "_mv", [128, 2], F32)
        self.rstd = sb(k, stack, name + "_rs", [128, 1], F32)
        self.nmr = sb(k, stack, name + "_nm", [128, 1], F32)
        self.mhalf = sb(k, stack, name + "_mh", [128, 1], F32)
        self.t = P.tok(name + "_ln")
        mh = self.mhalf
        P.op("pool", lambda e: e.memset(mh[:], -0.5), writes=[self.t])


def emit_ln(k, r, t_r, L):
    P = k.P
    for hf in range(2):
        P.op("dve", lambda e, hf=hf: e.bn_stats(out=L.stats[:, hf, :], in_=r[:, hf * 512:(hf + 1) * 512]),
             reads=[t_r], writes=[L.t])
    P.op("dve", lambda e: e.bn_aggr(out=L.mv[:], in_=L.stats[:].rearrange("p a b -> p (a b)")), reads=[L.t], writes=[L.t])
    P.op("dve", lambda e: e.tensor_scalar_add(out=L.rstd[:], in0=L.mv[:, 1:2], scalar1=EPS), reads=[L.t], writes=[L.t])
    P.op("pool", lambda e: e.tensor_tensor(out=L.rstd[:], in0=L.rstd[:], in1=L.mhalf[:], op=ALU.pow),
         reads=[L.t], writes=[L.t])
    P.op("dve", lambda e: e.scalar_tensor_tensor(out=L.nmr[:], in0=L.mv[:, 0:1], scalar=-1.0, in1=L.rstd[:],
                                                 op0=ALU.mult, op1=ALU.mult), reads=[L.t], writes=[L.t])
    P.op("act", lambda e: e.activation(out=r, in_=r, func=AF.Identity, scale=L.rstd[:, 0:1], bias=L.nmr[:, 0:1]),
         reads=[t_r, L.t], writes=[t_r])


def load_bc(k, dst, src_row, tok):
    k.P.dma("sp", dst, src_row.partition_broadcast(128), [], [tok], tok)


def phaseC(k):
    nc, P = k.nc, k.P
    prm = k.l0
    modT = k.modT[0]
    gbc = k.gbc
    with ExitStack() as wsc:
        w_gate = sb(k, wsc, "w_gate_sb", [128, KC, DFF], BF16)
        w_up = sb(k, wsc, "w_up_sb", [128, KC, DFF], BF16)
        t_wg = P.tok("w_gate", dma=True)
        t_wu = P.tok("w_up", dma=True)
        t_wd = P.tok("w_down", dma=True)
        load_cast_weight(k, w_gate, prm["w_gate"].rearrange("(kc p) n -> p kc n", p=128), t_wg, 4)
        load_cast_weight(k, w_up, prm["w_up"].rearrange("(kc p) n -> p kc n", p=128), t_wu, 4)
        with ExitStack() as ph:
            w_out = sb(k, ph, "w_out_sb", [128, KC, D], BF16)
            wst = sb(k, ph, "w_out_st", [128, 2, D], F32)
            t_wo = P.tok("w_out")
            t_wst = P.tok("w_out_st", dma=True)
            wov = prm["w_out"].rearrange("(kc p) n -> p kc n", p=128)
            for c in range(4):
                P.dma("sp", wst[:], wov[:, 2 * c:2 * c + 2, :], [], [t_wst], t_wst)
                for i in range(2):
                    P.op("dve", lambda e, c=c, i=i: e.tensor_mul(out=w_out[:, 2 * c + i, :], in0=wst[:, i, :],
                                                                in1=gbc[:, 0, :]),
                         reads=[t_wst, k.t_gbc], writes=[t_wo])
            lng = sb(k, ph, "ln1g", [128, D], F32)
            lnb = sb(k, ph, "ln1b", [128, D], F32)
            t_lng = P.tok("ln1g", dma=True)
            t_lnb = P.tok("ln1b", dma=True)
            load_bc(k, lng[:], prm["ln1_g"], t_lng)
            load_bc(k, lnb[:], prm["ln1_b"], t_lnb)
            gcol = sb(k, ph, "gcol", [128, 2, KC], F32)
            t_gcol = P.tok("gcol", dma=True)
            P.dma("sp", gcol[:, 0, :], prm["ln1_g"].rearrange("o (kc p) -> p (o kc)", p=128), [], [t_gcol], t_gcol,
                  allow_slow_non_contiguous=True)
            P.dma("sp", gcol[:, 1, :], prm["ln1_b"].rearrange("o (kc p) -> p (o kc)", p=128), [], [t_gcol], t_gcol,
                  allow_slow_non_contiguous=True)
            AB = sb(k, ph, "AB2", [128, 2, KC], F32)
            t_AB = P.tok("AB2")
            P.op("dve", lambda e: e.tensor_mul(out=AB[:, 0, :], in0=gcol[:, 0, :], in1=modT[:, 32:40]),
                 reads=[t_gcol, k.t_modT[0]], writes=[t_AB])
            P.op("dve", lambda e: e.tensor_mul(out=AB[:, 1, :], in0=gcol[:, 1, :], in1=modT[:, 32:40]),
                 reads=[t_gcol, k.t_modT[0]], writes=[t_AB])
            P.op("dve", lambda e: e.tensor_add(out=AB[:, 1, :], in0=AB[:, 1, :], in1=modT[:, 24:32]),
                 reads=[k.t_modT[0]], writes=[t_AB])
            L = LNScratch(k, ph, "c1")
            xs = None
            og = [sb(k, ph, "ogC%d" % i, [128, 4, D], BF16) for i in range(1)] * 2
            oT = sb(k, ph, "oT", [128, KC, 512], BF16)
            r1 = [sb(k, ph, "r1_%d" % i, [128, D], F32) for i in range(2)]
            x1o = [sb(k, ph, "x1o_%d" % i, [128, D], F32) for i in range(2)]
            h2T = [sb(k, ph, "h2TC%d" % i, [128, KC, 512], BF16) for i in range(1)] * 2
            ptb = [ps(k, ph, "ptbC%d" % i, [128, 512], BF16) for i in range(2)]
            ptf = [ps(k, ph, "ptfC%d" % i, [128, 512]) for i in range(2)]
            py = [ps(k, ph, "pyC%d" % i, [128, 512]) for i in range(4)]
            t_x = None
            t_og = [P.tok("ogC%d" % i, dma=True) for i in range(1)] * 2
            t_oT = P.tok("oT")
            t_r1 = [P.tok("r1_%d" % i) for i in range(2)]
            t_x1o = [P.tok("x1o_%d" % i, dma=True) for i in range(2)]
            t_h2T = [P.tok("h2TC%d" % i, dma=True) for i in range(1)] * 2
            t_ptb = [P.tok("ptbC%d" % i) for i in range(2)]
            t_ptf = [P.tok("ptfC%d" % i) for i in range(2)]
            t_py = [P.tok("pyC%d" % i) for i in range(4)]
            t_scr = P.tok("scrC1")
            xv = k.x.rearrange("(g t p) d -> g p t d", p=128, t=4)
            ov = k.o_s.rearrange("(g t p) d -> g p t d", p=128, t=4)
            idf, idb = k.ident_f, k.ident_b
            xt3 = [sb(k, ph, "xtC%d" % i, [128, D], F32) for i in range(3)]
            t_xt3 = [P.tok("xtC%d" % i, dma=True) for i in range(3)]
            r1x = sb(k, ph, "r1_2", [128, D], F32)
            r1 = r1 + [r1x]
            t_r1 = t_r1 + [P.tok("r1_2")]
            cst = dict(itb=0, itf=0, ipy=0)
            pys_of = {}

            def G1(g):
                P.dma("sp", og[0][:], ov[g], [], [t_og[0]], t_og[0])
                for kc in range(KC):
                    tb = cst["itb"] % 2
                    cst["itb"] += 1
                    for tt in range(4):
                        P.op("pe", lambda e, tb=tb, tt=tt, kc=kc: e.transpose(
                            ptb[tb][:, tt * 128:(tt + 1) * 128], og[0][:, tt, kc * 128:(kc + 1) * 128], idb[:]),
                            reads=[t_og[0], k.t_ident], writes=[t_ptb[tb]], inc=(tt == 3))
                    if kc % 2 == 0:
                        P.op("act", lambda e, tb=tb, kc=kc: e.activation(out=oT[:, kc, :], in_=ptb[tb][:], func=AF.Copy),
                             reads=[t_ptb[tb]], writes=[t_oT])
                    else:
                        P.op("dve", lambda e, tb=tb, kc=kc: e.tensor_copy(out=oT[:, kc, :], in_=ptb[tb][:]),
                             reads=[t_ptb[tb]], writes=[t_oT])

            def S1(i):
                g, tt = i // 4, i % 4
                xb = i % 3
                t0 = i * 128
                P.dma("sp", xt3[xb][:], k.x[t0:t0 + 128, :], [], [t_xt3[xb]], t_xt3[xb])
                pys = []
                for dh in range(2):
                    pb = cst["ipy"] % 4
                    cst["ipy"] += 1
                    pys.append(pb)
                    for kc in range(KC):
                        P.op("pe", lambda e, pb=pb, kc=kc, tt=tt, dh=dh: e.matmul(
                            py[pb][:], lhsT=oT[:, kc, tt * 128:(tt + 1) * 128], rhs=w_out[:, kc, dh * 512:(dh + 1) * 512],
                            start=(kc == 0), stop=(kc == KC - 1)),
                            reads=[t_oT, t_wo], writes=[t_py[pb]], inc=(kc == KC - 1))
                pys_of[i] = pys

            def S2(i):
                rb = i % 3
                xb = i % 3
                ob_ = i % 2
                pys = pys_of[i]
                for dh in range(2):
                    pb = pys[dh]
                    P.op("dve", lambda e, pb=pb, dh=dh, rb=rb, xb=xb: e.scalar_tensor_tensor(
                        out=r1[rb][:, dh * 512:(dh + 1) * 512], in0=xt3[xb][:, dh * 512:(dh + 1) * 512],
                        scalar=DN_ALPHA, in1=py[pb][:], op0=ALU.mult, op1=ALU.add),
                        reads=[t_xt3[xb], t_py[pb]], writes=[t_r1[rb]])
                emit_ln(k, r1[rb][:], t_r1[rb], L)
                P.op("pool", lambda e, rb=rb, ob_=ob_: e.tensor_tensor(out=x1o[ob_][:], in0=r1[rb][:], in1=lng[:], op=ALU.mult),
                     reads=[t_r1[rb], t_lng], writes=[t_x1o[ob_]])
                P.op("pool", lambda e, ob_=ob_: e.tensor_tensor(out=x1o[ob_][:], in0=x1o[ob_][:], in1=lnb[:], op=ALU.add),
                     reads=[t_lnb], writes=[t_x1o[ob_]])
                t0 = i * 128
                P.dma("sp", k.x1_s[t0:t0 + 128, :], x1o[ob_][:], [t_x1o[ob_]], [t_scr], t_x1o[ob_])

            def S3(i):
                g, tt = i // 4, i % 4
                rb = i % 3
                for k4 in range(2):
                    tf = cst["itf"] % 2
                    cst["itf"] += 1
                    for q in range(4):
                        kc = k4 * 4 + q
                        P.op("pe", lambda e, tf=tf, kc=kc, q=q, rb=rb: e.transpose(
                            ptf[tf][:, q * 128:(q + 1) * 128], r1[rb][:, kc * 128:(kc + 1) * 128], idf[:]),
                            reads=[t_r1[rb], k.t_ident], writes=[t_ptf[tf]], inc=(q == 3))
                    for q in range(4):
                        kc = k4 * 4 + q
                        P.op("act", lambda e, tf=tf, kc=kc, q=q, tt=tt: e.activation(
                            out=h2T[0][:, kc, tt * 128:(tt + 1) * 128], in_=ptf[tf][:, q * 128:(q + 1) * 128],
                            func=AF.Identity, scale=AB[:, 0, kc:kc + 1], bias=AB[:, 1, kc:kc + 1]),
                            reads=[t_ptf[tf], t_AB], writes=[t_h2T[0]])
                if tt == 3:
                    P.dma("sp", k.h2T_s.rearrange("(kc p) t -> p kc t", p=128)[:, :, g * 512:(g + 1) * 512], h2T[0][:],
                          [t_h2T[0]], [t_scr], t_h2T[0])

            G1(0)
            S1(0)
            for i in range(32):
                if i + 1 < 32:
                    if (i + 1) % 4 == 0:
                        G1((i + 1) // 4)
                    S1(i + 1)
                S2(i)
                if i >= 1:
                    S3(i - 1)
            S3(31)
            dma_barrier(P, "sp", t_x1o + t_h2T[:1])
            if k.debug and "x1" in k.dbg:
                big = sb(k, ph, "dbgbigC", [128, 4, 1024], F32)
                t_big = P.tok("dbgbigC", dma=True)
                for c8 in range(8):
                    P.dma("sp", big[:], k.x1_s.rearrange("(a p) t -> p a t", p=128)[:, c8 * 4:(c8 + 1) * 4, :], [t_scr], [t_big], t_big)
                    ev = P.dma("sp", k.dbg["x1"].rearrange("(a p) t -> p a t", p=128)[:, c8 * 4:(c8 + 1) * 4, :], big[:], [t_big], [], t_big)
                P.out_events.append(ev)
            P.flush()
        with ExitStack() as ph:
            w_down = sb(k, ph, "w_down_sb", [128, FC, D], BF16)
            load_cast_weight(k, w_down, prm["w_down"].rearrange("(fc p) n -> p fc n", p=128), t_wd, 11)
            lng = sb(k, ph, "ln2g", [128, D], F32)
            lnb = sb(k, ph, "ln2b", [128, D], F32)
            t_lng = P.tok("ln2g", dma=True)
            t_lnb = P.tok("ln2b", dma=True)
            load_bc(k, lng[:], prm["ln2_g"], t_lng)
            load_bc(k, lnb[:], prm["ln2_b"], t_lnb)
            L = LNScratch(k, ph, "c2")
            h2T = sb(k, ph, "h2T2", [128, KC, 512], BF16)
            actT = sb(k, ph, "actT", [128, FC, 512], BF16)
            sg = [sb(k, ph, "sg%d" % i, [128, 512], BF16) for i in range(2)]
            x1t = [sb(k, ph, "x1t%d" % i, [128, D], F32) for i in range(2)]
            r2 = [sb(k, ph, "r2_%d" % i, [128, D], F32) for i in range(2)]
            pg = [ps(k, ph, "pgC%d" % i, [128, 512]) for i in range(2)]
            pu = [ps(k, ph, "puC%d" % i, [128, 512]) for i in range(2)]
            py = [ps(k, ph, "py2C%d" % i, [128, 512]) for i in range(4)]
            t_h2T = P.tok("h2T2", dma=True)
            t_actT = P.tok("actT")
            t_sg = [P.tok("sg%d" % i) for i in range(2)]
            t_x1t = [P.tok("x1t%d" % i, dma=True) for i in range(2)]
            t_r2 = [P.tok("r2_%d" % i, dma=True) for i in range(2)]
            t_pg = [P.tok("pg%d" % i) for i in range(2)]
            t_pu = [P.tok("pu%d" % i) for i in range(2)]
            t_py = [P.tok("py2_%d" % i) for i in range(4)]
            t_scr2 = P.tok("scrC2")
            ipy = 0
            P.dma("sp", h2T[:], k.h2T_s.rearrange("(kc p) t -> p kc t", p=128)[:, :, 0:512], [], [t_h2T], t_h2T)
            for g in range(8):
                for fc in range(FC):
                    b = fc % 2
                    for kc in range(KC):
                        P.op("pe", lambda e, b=b, kc=kc, fc=fc: e.matmul(
                            pg[b][:], lhsT=w_gate[:, kc, fc * 128:(fc + 1) * 128], rhs=h2T[:, kc, :],
                            start=(kc == 0), stop=(kc == KC - 1)),
                            reads=[t_wg, t_h2T], writes=[t_pg[b]], inc=(kc == KC - 1))
                    for kc in range(KC):
                        P.op("pe", lambda e, b=b, kc=kc, fc=fc: e.matmul(
                            pu[b][:], lhsT=w_up[:, kc, fc * 128:(fc + 1) * 128], rhs=h2T[:, kc, :],
                            start=(kc == 0), stop=(kc == KC - 1)),
                            reads=[t_wu, t_h2T], writes=[t_pu[b]], inc=(kc == KC - 1))
                    P.op("act", lambda e, b=b: e.activation(out=sg[b][:], in_=pg[b][:], func=AF.Silu),
                         reads=[t_pg[b]], writes=[t_sg[b]])
                    P.op("dve", lambda e, b=b, fc=fc: e.tensor_tensor(out=actT[:, fc, :], in0=sg[b][:], in1=pu[b][:],
                                                                     op=ALU.mult),
                         reads=[t_sg[b], t_pu[b]], writes=[t_actT])
                if g + 1 < 8:
                    P.dma("sp", h2T[:], k.h2T_s.rearrange("(kc p) t -> p kc t", p=128)[:, :, (g + 1) * 512:(g + 2) * 512],
                          [], [t_h2T], t_h2T)
                for tt in range(4):
                    rb = (g * 4 + tt) % 2
                    t0 = g * 512 + tt * 128
                    P.dma("sp", x1t[rb][:], k.x1_s[t0:t0 + 128, :], [], [t_x1t[rb]], t_x1t[rb])
                    pys = []
                    for dh in range(2):
                        pb = ipy % 4
                        ipy += 1
                        pys.append(pb)
                        for fc in range(FC):
                            P.op("pe", lambda e, pb=pb, fc=fc, tt=tt, dh=dh: e.matmul(
                                py[pb][:], lhsT=actT[:, fc, tt * 128:(tt + 1) * 128], rhs=w_down[:, fc, dh * 512:(dh + 1) * 512],
                                start=(fc == 0), stop=(fc == FC - 1)),
                                reads=[t_actT, t_wd], writes=[t_py[pb]], inc=(fc == FC - 1))
                    for dh in range(2):
                        pb = pys[dh]
                        P.op("dve", lambda e, pb=pb, dh=dh, rb=rb: e.tensor_tensor(
                            out=r2[rb][:, dh * 512:(dh + 1) * 512], in0=py[pb][:], in1=gbc[:, 1, dh * 512:(dh + 1) * 512],
                            op=ALU.mult), reads=[t_py[pb], k.t_gbc], writes=[t_r2[rb]])
                        P.op("dve", lambda e, dh=dh, rb=rb: e.scalar_tensor_tensor(
                            out=r2[rb][:, dh * 512:(dh + 1) * 512], in0=x1t[rb][:, dh * 512:(dh + 1) * 512],
                            scalar=DN_ALPHA, in1=r2[rb][:, dh * 512:(dh + 1) * 512], op0=ALU.mult, op1=ALU.add),
                            reads=[t_x1t[rb]], writes=[t_r2[rb]])
                    emit_ln(k, r2[rb][:], t_r2[rb], L)
                    P.op("pool", lambda e, rb=rb: e.tensor_tensor(out=r2[rb][:], in0=r2[rb][:], in1=lng[:], op=ALU.mult),
                         reads=[t_lng], writes=[t_r2[rb]])
                    P.op("pool", lambda e, rb=rb: e.tensor_tensor(out=r2[rb][:], in0=r2[rb][:], in1=lnb[:], op=ALU.add),
                         reads=[t_lnb], writes=[t_r2[rb]])
                    P.dma("sp", k.x2_s[t0:t0 + 128, :], r2[rb][:], [t_r2[rb]], [t_scr2], t_r2[rb])
            dma_barrier(P, "sp", t_r2)
            if k.debug and "x2" in k.dbg:
                big = sb(k, ph, "dbgbigC2", [128, 2, 1024], F32)
                t_big = P.tok("dbgbigC2", dma=True)
                for c8 in range(16):
                    P.dma("sp", big[:], k.x2_s.rearrange("(a p) t -> p a t", p=128)[:, c8 * 2:(c8 + 1) * 2, :], [t_scr2], [t_big], t_big)
                    ev = P.dma("sp", k.dbg["x2"].rearrange("(a p) t -> p a t", p=128)[:, c8 * 2:(c8 + 1) * 2, :], big[:], [t_big], [], t_big)
                P.out_events.append(ev)
            P.flush()


TWO_PI = 2.0 * math.pi


def emit_sin(k, out, x, tmpf, tmpi, tok, shift=0.0):
    P = k.P
    rd = [tok]
    P.op("dve", lambda e: e.tensor_scalar(out=out, in0=x, scalar1=shift, scalar2=1.0 / TWO_PI, op0=ALU.add, op1=ALU.mult),
         reads=rd, writes=[tok])
    P.op("dve", lambda e: e.tensor_copy(out=tmpi, in_=out), reads=rd, writes=[tok])
    P.op("dve", lambda e: e.tensor_copy(out=tmpf, in_=tmpi), reads=rd, writes=[tok])
    P.op("dve", lambda e: e.scalar_tensor_tensor(out=out, in0=tmpf, scalar=-TWO_PI, in1=x, op0=ALU.mult, op1=ALU.add),
         reads=rd, writes=[tok])
    if shift != 0.0:
        P.op("dve", lambda e: e.tensor_scalar_add(out=out, in0=out, scalar1=shift), reads=rd, writes=[tok])
    P.op("dve", lambda e: e.tensor_single_scalar(out=tmpf, in_=out, scalar=math.pi, op=ALU.is_gt), reads=rd, writes=[tok])
    P.op("dve", lambda e: e.scalar_tensor_tensor(out=out, in0=tmpf, scalar=-TWO_PI, in1=out, op0=ALU.mult, op1=ALU.add),
         reads=rd, writes=[tok])
    P.op("dve", lambda e: e.tensor_single_scalar(out=tmpf, in_=out, scalar=-math.pi, op=ALU.is_lt), reads=rd, writes=[tok])
    P.op("dve", lambda e: e.scalar_tensor_tensor(out=out, in0=tmpf, scalar=TWO_PI, in1=out, op0=ALU.mult, op1=ALU.add),
         reads=rd, writes=[tok])
    P.op("act", lambda e: e.activation(out=out, in_=out, func=AF.Sin), reads=rd, writes=[tok])


def cmul(k, out_r, out_i, xr, xi, cr, ci, t1, t2, tok, eng="dve"):
    P = k.P
    rd = [tok]
    P.op(eng, lambda e: e.tensor_tensor(out=t1, in0=xr, in1=cr, op=ALU.mult), reads=rd, writes=[tok])
    P.op(eng, lambda e: e.tensor_tensor(out=t2, in0=xi, in1=ci, op=ALU.mult), reads=rd, writes=[tok])
    P.op(eng, lambda e: e.tensor_tensor(out=out_r, in0=t1, in1=t2, op=ALU.subtract), reads=rd, writes=[tok])
    P.op(eng, lambda e: e.tensor_tensor(out=t1, in0=xr, in1=ci, op=ALU.mult), reads=rd, writes=[tok])
    P.op(eng, lambda e: e.tensor_tensor(out=t2, in0=xi, in1=cr, op=ALU.mult), reads=rd, writes=[tok])
    P.op(eng, lambda e: e.tensor_tensor(out=out_i, in0=t1, in1=t2, op=ALU.add), reads=rd, writes=[tok])


def phaseD1(k):
    nc, P = k.nc, k.P
    prm = k.l1
    idf, idb = k.ident_f, k.ident_b
    with ExitStack() as ph:
        NP = 64
        APr = sb(k, ph, "APr", [NP, 64, 33], F32)
        APi = sb(k, ph, "APi", [NP, 64, 33], F32)
        RPr = sb(k, ph, "RPr", [NP, 64, 32], F32)
        RPi = sb(k, ph, "RPi", [NP, 64, 32], F32)
        Ctr = sb(k, ph, "Ctr", [NP, 1024], F32)
        Cti = sb(k, ph, "Cti", [NP, 1024], F32)
        bbr_f = sb(k, ph, "bbr", [NP, 72, 16], F32)
        bbi = sb(k, ph, "bbi", [NP, 64, 16], F32)
        nbbi_f = sb(k, ph, "nbbi", [NP, 72, 16], F32)
        bbr = bbr_f[:, 0:64, :]
        nbbi = nbbi_f[:, 0:64, :]
        SQr = sb(k, ph, "SQr", [NP, 64, 7], F32)
        SQi = sb(k, ph, "SQi", [NP, 64, 7], F32)
        nSQi = sb(k, ph, "nSQi", [NP, 64, 7], F32)
        M10 = sb(k, ph, "M10", [128, 2, 64], F32)
        dcol = sb(k, ph, "dcol", [16, 64], F32)
        t_tab = P.tok("s5tab")
        P.op("pool", lambda e: e.memset(bbr_f[:, 64:72, :], 0.0), writes=[t_tab])
        P.op("pool", lambda e: e.memset(nbbi_f[:, 64:72, :], 0.0), writes=[t_tab])
        t_M = P.tok("M10", dma=True)
        t_dcol = P.tok("dcol", dma=True)
        with ExitStack() as tmp:
            pmisc = ps(k, tmp, "pmiscD", [128, 512])
            t_pmisc = P.tok("pmiscD")
            def t64(name):
                return sb(k, tmp, name, [NP, 64], F32)
            a0 = sb(k, tmp, "a0", [64, 2, 64], F32)
            t_a0 = P.tok("a0", dma=True)
            P.dma("sp", a0[:, 0, :], prm["a_re"], [], [t_a0], t_a0)
            P.dma("sp", a0[:, 1, :], prm["a_im"], [], [t_a0], t_a0)
            arT, aiT, dtb, mag, ang, sn, cs = [t64(n) for n in ("arT", "aiT", "dtb", "mag", "ang", "sn", "cs")]
            tf = t64("tf")
            ti = sb(k, tmp, "ti", [NP, 64], I32)
            abr, abi, den, nr, zr, zi, u1, u2 = [t64(n) for n in ("abr", "abi", "den", "nr", "zr", "zi", "u1", "u2")]
            t_dt = P.tok("dtb", dma=True)
            for i, dst in enumerate((arT, aiT)):
                P.op("pe", lambda e, i=i: e.transpose(pmisc[0:64, i * 64:(i + 1) * 64], a0[:, i, :], idf[0:64, 0:64]),
                     reads=[t_a0, k.t_ident], writes=[t_pmisc])
                P.op("dve", lambda e, i=i, dst=dst: e.tensor_copy(out=dst[:], in_=pmisc[0:64, i * 64:(i + 1) * 64]),
                     reads=[t_pmisc], writes=[t_tab])
            P.dma("sp", dtb[:], prm["log_dt"].partition_broadcast(NP), [], [t_dt], t_dt)
            P.op("act", lambda e: e.activation(out=dtb[:], in_=dtb[:], func=AF.Exp), reads=[t_dt], writes=[t_tab])
            rd = [t_tab]
            P.op("dve", lambda e: e.tensor_mul(out=mag[:], in0=dtb[:], in1=arT[:]), reads=rd, writes=[t_tab])
            P.op("act", lambda e: e.activation(out=mag[:], in_=mag[:], func=AF.Exp), reads=rd, writes=[t_tab])
            P.op("dve", lambda e: e.tensor_mul(out=ang[:], in0=dtb[:], in1=aiT[:]), reads=rd, writes=[t_tab])
            emit_sin(k, sn[:], ang[:], tf[:], ti[:], t_tab, 0.0)
            emit_sin(k, cs[:], ang[:], tf[:], ti[:], t_tab, 0.5 * math.pi)
            P.op("dve", lambda e: e.tensor_mul(out=abr[:], in0=mag[:], in1=cs[:]), reads=rd, writes=[t_tab])
            P.op("dve", lambda e: e.tensor_mul(out=abi[:], in0=mag[:], in1=sn[:]), reads=rd, writes=[t_tab])
            P.op("dve", lambda e: e.tensor_mul(out=den[:], in0=arT[:], in1=arT[:]), reads=rd, writes=[t_tab])
            P.op("dve", lambda e: e.tensor_mul(out=u1[:], in0=aiT[:], in1=aiT[:]), reads=rd, writes=[t_tab])
            P.op("dve", lambda e: e.tensor_add(out=den[:], in0=den[:], in1=u1[:]), reads=rd, writes=[t_tab])
            P.op("dve", lambda e: e.reciprocal(out=den[:], in_=den[:]), reads=rd, writes=[t_tab])
            P.op("dve", lambda e: e.tensor_scalar_add(out=nr[:], in0=abr[:], scalar1=-1.0), reads=rd, writes=[t_tab])
            P.op("dve", lambda e: e.tensor_mul(out=u1[:], in0=nr[:], in1=arT[:]), reads=rd, writes=[t_tab])
            P.op("dve", lambda e: e.tensor_mul(out=u2[:], in0=abi[:], in1=aiT[:]), reads=rd, writes=[t_tab])
            P.op("dve", lambda e: e.tensor_add(out=u1[:], in0=u1[:], in1=u2[:]), reads=rd, writes=[t_tab])
            P.op("dve", lambda e: e.tensor_mul(out=zr[:], in0=u1[:], in1=den[:]), reads=rd, writes=[t_tab])
            P.op("dve", lambda e: e.tensor_mul(out=u1[:], in0=abi[:], in1=arT[:]), reads=rd, writes=[t_tab])
            P.op("dve", lambda e: e.tensor_mul(out=u2[:], in0=nr[:], in1=aiT[:]), reads=rd, writes=[t_tab])
            P.op("dve", lambda e: e.tensor_sub(out=u1[:], in0=u1[:], in1=u2[:]), reads=rd, writes=[t_tab])
            P.op("dve", lambda e: e.tensor_mul(out=zi[:], in0=u1[:], in1=den[:]), reads=rd, writes=[t_tab])
            br = sb(k, tmp, "br", [NP, 64, 16], F32)
            bi = sb(k, tmp, "bi", [NP, 64, 16], F32)
            w1 = sb(k, tmp, "w1", [NP, 64, 16], F32)
            w2 = sb(k, tmp, "w2", [NP, 64, 16], F32)
            t_b = P.tok("brbi", dma=True)
            P.dma("sp", br[:], prm["b_re"].rearrange("g p c -> p g c"), [], [t_b], t_b)
            P.dma("sp", bi[:], prm["b_im"].rearrange("g p c -> p g c"), [], [t_b], t_b)
            P.op("dve", lambda e: e.tensor_copy(out=w1[:], in_=br[:]), reads=[t_b], writes=[t_tab])
            zrb = zr[:].unsqueeze(2).broadcast_to([NP, 64, 16])
            zib = zi[:].unsqueeze(2).broadcast_to([NP, 64, 16])
            cmul(k, bbr, bbi[:], br[:], bi[:], zrb, zib, w1[:], w2[:], t_tab)
            P.op("dve", lambda e: e.tensor_scalar_mul(out=nbbi, in0=bbi[:], scalar1=-1.0), reads=rd, writes=[t_tab])
            cst = sb(k, tmp, "cst", [128, 8, 64], F32)
            t_cst = P.tok("cst", dma=True)
            for ri, (src_c, dst) in enumerate(((prm["c_re"], Ctr), (prm["c_im"], Cti))):
                P.dma("sp", cst[:], src_c.rearrange("(a p) q -> p a q", p=128), [], [t_cst], t_cst)
                for half in range(2):
                    for a in range(4):
                        P.op("pe", lambda e, a=a, half=half: e.transpose(
                            pmisc[0:64, a * 128:(a + 1) * 128], cst[:, half * 4 + a, :], idf[:]),
                            reads=[t_cst, k.t_ident], writes=[t_pmisc], inc=(a == 3))
                    P.op("dve", lambda e, half=half, dst=dst: e.tensor_copy(
                        out=dst[:, half * 512:(half + 1) * 512], in_=pmisc[0:64, :]),
                        reads=[t_pmisc], writes=[t_tab])
            P.op("dve", lambda e: e.memset(APr[:, :, 0:1], 1.0), writes=[t_tab])
            P.op("dve", lambda e: e.memset(APi[:, :, 0:1], 0.0), writes=[t_tab])
            P.op("dve", lambda e: e.tensor_copy(out=APr[:, :, 1], in_=abr[:]), reads=rd, writes=[t_tab])
            P.op("dve", lambda e: e.tensor_copy(out=APi[:, :, 1], in_=abi[:]), reads=rd, writes=[t_tab])
            pw1 = sb(k, tmp, "pw1", [NP, 64, 16], F32)
            pw2 = sb(k, tmp, "pw2", [NP, 64, 16], F32)
            for t0 in (1, 2, 4, 8, 16):
                crb = APr[:, :, t0:t0 + 1].broadcast_to([NP, 64, t0])
                cib = APi[:, :, t0:t0 + 1].broadcast_to([NP, 64, t0])
                cmul(k, APr[:, :, t0 + 1:2 * t0 + 1], APi[:, :, t0 + 1:2 * t0 + 1],
                     APr[:, :, 1:t0 + 1], APi[:, :, 1:t0 + 1], crb, cib, pw1[:, :, 0:t0], pw2[:, :, 0:t0], t_tab)
            for s in range(32):
                P.op("pool", lambda e, s=s: e.tensor_copy(out=RPr[:, :, s], in_=APr[:, :, 31 - s]), reads=rd, writes=[t_tab])
                P.op("pool", lambda e, s=s: e.tensor_copy(out=RPi[:, :, s], in_=APi[:, :, 31 - s]), reads=rd, writes=[t_tab])
            P.op("dve", lambda e: e.tensor_copy(out=SQr[:, :, 0], in_=APr[:, :, 32]), reads=rd, writes=[t_tab])
            P.op("dve", lambda e: e.tensor_copy(out=SQi[:, :, 0], in_=APi[:, :, 32]), reads=rd, writes=[t_tab])
            for j in range(6):
                cmul(k, SQr[:, :, j + 1], SQi[:, :, j + 1], SQr[:, :, j], SQi[:, :, j], SQr[:, :, j], SQi[:, :, j],
                     u1[:], u2[:], t_tab)
            P.op("dve", lambda e: e.tensor_scalar_mul(out=nSQi[:], in0=SQi[:], scalar1=-1.0), reads=rd, writes=[t_tab])
            for i, off in enumerate((D, 0)):
                srcap = bass.AP(k.mod_s.tensor, off, [[0, 8], [1, 16], [16, 64]])
                for sl in range(8):
                    P.dma("sp", M10[sl * 16:(sl + 1) * 16, i, :], bass.AP(k.mod_s.tensor, off, [[1, 16], [16, 64]]),
                          [], [t_M], t_M, allow_slow_non_contiguous=True)
            P.op("dve", lambda e: e.tensor_scalar_add(out=M10[:, 0, :], in0=M10[:, 0, :], scalar1=1.0), reads=[t_M], writes=[t_M])
            P.dma("sp", dcol[:], bass.AP(prm["d_skip"].tensor, 0, [[1, 16], [16, 64]]), [], [t_dcol], t_dcol,
                  allow_slow_non_contiguous=True)
            P.flush()

        if getattr(k, "stop", None) == "tables":
            return
        CAr = [sb(k, ph, "CAr%d" % i, [NP, 33, 16], F32) for i in range(4)]
        CAi = [sb(k, ph, "CAi%d" % i, [NP, 33, 16], F32) for i in range(4)]
        ca1 = sb(k, ph, "ca1", [NP, 33, 16], F32)
        ca2 = sb(k, ph, "ca2", [NP, 33, 16], F32)
        Wc = [sb(k, ph, "Wc%d" % i, [NP, 2, 512], BF16) for i in range(4)]
        ab1 = sb(k, ph, "ab1", [NP, 32, 16], F32)
        ab2 = sb(k, ph, "ab2", [NP, 32, 16], F32)
        ABr = sb(k, ph, "ABr", [NP, 32, 16], F32)
        ABi = sb(k, ph, "ABi", [NP, 32, 16], F32)
        Wend = [sb(k, ph, "Wend%d" % i, [128, 2, 4, 64], BF16) for i in range(4)]
        Gsb = [sb(k, ph, "Gsb%d" % i, [16, 63 * 16], BF16) for i in range(4)]
        dtmp = sb(k, ph, "dtmp", [16, 16], F32)
        t_dtmp = P.tok("dtmp")
        Sel = sb(k, ph, "Sel", [16, 8, 128], BF16)
        t_Sel = P.tok("Sel")
        P.op("pool", lambda e: e.memset(Sel[:], 0.0), writes=[t_Sel])
        for s8 in range(8):
            P.op("pool", lambda e, s8=s8: e.tensor_copy(out=Sel[:, s8, 16 * s8:16 * s8 + 16], in_=idb[0:16, 0:16]),
                 reads=[k.t_ident], writes=[t_Sel])
        pW = [ps(k, ph, "pW%d" % i, [128, 512]) for i in range(2)]
        t_pW = [P.tok("pW%d" % i) for i in range(2)]
        Wg = [sb(k, ph, "Wg%d" % i, [128, 4, 512], BF16) for i in range(4)]
        UT = [sb(k, ph, "UT%d" % i, [128, 4, 128], BF16) for i in range(2)]
        Ha = sb(k, ph, "Ha", [NP, 2, 128], F32)
        Hb = sb(k, ph, "Hb", [NP, 2, 128], F32)
        Hp = [sb(k, ph, "Hp%d" % i, [NP, 2, 128], BF16) for i in range(2)]
        xblk = [sb(k, ph, "xblk%d" % i, [128, 32, 128], F32) for i in range(1)] * 2
        xr = [sb(k, ph, "xr%d" % i, [128, 8, 32, 16], F32) for i in range(2)]
        t_xr = [P.tok("xr%d" % i) for i in range(2)]
        ybuf = sb(k, ph, "ybuf", [128, 32, 128], BF16)
        ygt = [sb(k, ph, "ygt%d" % i, [128, 512], BF16) for i in range(2)]
        t_ygt = [P.tok("ygt%d" % i) for i in range(2)]
        yTst = sb(k, ph, "yTst", [128, 32, 128], BF16)
        ptU = ps(k, ph, "ptU", [128, 512])
        pE = ps(k, ph, "pE", [128, 512])
        pY = [ps(k, ph, "pY%d" % i, [128, 512]) for i in range(1)] * 2
        pG = ps(k, ph, "pG", [128, 512])
        pWe = ps(k, ph, "pWe", [128, 512])
        ptY = ps(k, ph, "ptY", [128, 512], BF16)
        t_CA = [P.tok("CA%d" % i) for i in range(4)]
        t_Wc = [P.tok("Wc%d" % i) for i in range(4)]
        t_AB = P.tok("AB")
        t_Wend = [P.tok("Wend%d" % i) for i in range(4)]
        t_Gsb = [P.tok("Gsb%d" % i) for i in range(4)]
        for i4 in range(4):
            P.op("pool", lambda e, i4=i4: e.memset(Gsb[i4][:, 0:31 * 16], 0.0), writes=[t_Gsb[i4]])
        t_Wg = [P.tok("Wg%d" % i) for i in range(4)]
        t_UT = [P.tok("UT%d" % i) for i in range(2)]
        t_H = P.tok("H")
        t_Hp = [P.tok("Hp%d" % i) for i in range(2)]
        t_xb = [P.tok("xblk%d" % i, dma=True) for i in range(1)] * 2
        t_ybuf = P.tok("ybuf")
        t_yT = P.tok("yTst", dma=True)
        t_ptU = P.tok("ptU"); t_pE = P.tok("pE"); t_pY = [P.tok("pY0")] * 2
        t_pG = P.tok("pG"); t_pWe = P.tok("pWe"); t_ptY = P.tok("ptY")
        t_yTs = P.tok("yT_s")
        ydbg = None
        if k.debug and "s5" in k.dbg:
            ydbg = sb(k, ph, "ydbg", [128, 32, 128], F32)
            t_ydbg = P.tok("ydbg", dma=True)
        x2v = k.x2_s.rearrange("(kk s) c -> kk s c", s=32)
        rd = [t_tab]
        NG = getattr(k, "ngroups", 64)

        def stageA(g):
            cb, gl = g // 8, g % 8
            b = g % 4
            if gl == 0:
                xb = cb % 2
                P.dma("sp", xblk[xb][:], x2v[:, :, cb * 128:(cb + 1) * 128], [], [t_xb[xb]], t_xb[xb])
                for g8 in range(8):
                    P.op("pool", lambda e, xb=xb, g8=g8: e.tensor_copy(out=xr[xb][:, g8, :, :],
                                                                     in_=xblk[xb][:, :, g8 * 16:(g8 + 1) * 16]),
                         reads=[t_xb[xb]], writes=[t_xr[xb]])
            ctr_b = Ctr[:, g * 16:(g + 1) * 16].unsqueeze(1).broadcast_to([NP, 33, 16])
            cti_b = Cti[:, g * 16:(g + 1) * 16].unsqueeze(1).broadcast_to([NP, 33, 16])
            apr_b = APr[:, g, :].unsqueeze(2).broadcast_to([NP, 33, 16])
            api_b = APi[:, g, :].unsqueeze(2).broadcast_to([NP, 33, 16])
            P.op("pool", lambda e: e.tensor_copy(out=ca1[:, 0, 0:1], in_=ca1[:, 0, 0:1]), reads=[t_tab, t_CA[b]], writes=[t_CA[b]])
            cmul(k, CAr[b][:], CAi[b][:], ctr_b, cti_b, apr_b, api_b, ca1[:], ca2[:], t_CA[b], eng="pool")
            P.op("pe", lambda e, b=b, g=g: e.matmul(pG[:], lhsT=bbr_f[:, g:g + 8, :].rearrange("p a b -> p (a b)"), rhs=CAr[b][:, 0:32, :].rearrange("p a b -> p (a b)"),
                                                   start=True, stop=False), reads=[t_tab, t_CA[b]], writes=[t_pG], inc=False)
            P.op("pe", lambda e, b=b, g=g: e.matmul(pG[:], lhsT=nbbi_f[:, g:g + 8, :].rearrange("p a b -> p (a b)"), rhs=CAi[b][:, 0:32, :].rearrange("p a b -> p (a b)"),
                                                   start=False, stop=True), reads=[t_tab, t_CA[b]], writes=[t_pG])
            P.op("act", lambda e, b=b: e.activation(out=Gsb[b][:, 496:1008], in_=pG[0:16, :], func=AF.Copy),
                 reads=[t_pG], writes=[t_Gsb[b]])
            P.op("pool", lambda e, g=g: e.tensor_scalar_mul(out=dtmp[:], in0=idf[0:16, 0:16], scalar1=dcol[:, g:g + 1]),
                 reads=[t_dcol, k.t_ident], writes=[t_dtmp])
            P.op("pool", lambda e, b=b: e.tensor_tensor(out=Gsb[b][:, 496:512], in0=Gsb[b][:, 496:512], in1=dtmp[:], op=ALU.add),
                 reads=[t_dtmp], writes=[t_Gsb[b]])
            for j in range(4):
                wb = (g * 4 + j) % 2
                c0 = 8 * j * 16
                for sl in range(8):
                    o = (31 - 8 * j - sl) * 16 + c0
                    P.op("pe", lambda e, b=b, wb=wb, sl=sl, o=o, c0=c0: e.matmul(
                        pW[wb][:, c0:512], lhsT=Sel[:, sl, :], rhs=Gsb[b][:, o:o + 512 - c0], start=(sl == 0), stop=(sl == 7)),
                        reads=[t_Sel, t_Gsb[b]], writes=[t_pW[wb]], inc=(sl == 7))
                P.op("act", lambda e, b=b, wb=wb, j=j, c0=c0: e.activation(out=Wg[b][:, j, c0:512], in_=pW[wb][:, c0:512], func=AF.Copy),
                     reads=[t_pW[wb]], writes=[t_Wg[b]])
            P.op("act", lambda e, b=b: e.activation(out=Wc[b][:, 0, :], in_=CAr[b][:, 1:33, :].rearrange("p a b -> p (a b)"), func=AF.Copy),
                 reads=[t_CA[b]], writes=[t_Wc[b]])
            P.op("act", lambda e, b=b: e.activation(out=Wc[b][:, 1, :], in_=CAi[b][:, 1:33, :].rearrange("p a b -> p (a b)"), func=AF.Copy, scale=-1.0),
                 reads=[t_CA[b]], writes=[t_Wc[b]])
            rpr_b = RPr[:, g, :].unsqueeze(2).broadcast_to([NP, 32, 16])
            rpi_b = RPi[:, g, :].unsqueeze(2).broadcast_to([NP, 32, 16])
            bbr_b = bbr[:, g, :].unsqueeze(1).broadcast_to([NP, 32, 16])
            bbi_b = bbi[:, g, :].unsqueeze(1).broadcast_to([NP, 32, 16])
            P.op("pool", lambda e: e.tensor_copy(out=ab1[:, 0, 0:1], in_=ab1[:, 0, 0:1]), reads=[t_tab, t_AB], writes=[t_AB])
            cmul(k, ABr[:], ABi[:], rpr_b, rpi_b, bbr_b, bbi_b, ab1[:], ab2[:], t_AB, eng="pool")
            for ri, AB_ in enumerate((ABr, ABi)):
                for j in range(4):
                    P.op("pe", lambda e, ri=ri, j=j, AB_=AB_: e.transpose(
                        pWe[:, (ri * 4 + j) * 64:(ri * 4 + j + 1) * 64], AB_[:, 8 * j:8 * j + 8, :].rearrange("p a b -> p (a b)"), idf[0:64, 0:64]),
                        reads=[t_AB, k.t_ident], writes=[t_pWe], inc=(ri == 1 and j == 3))
            P.op("act", lambda e, b=b: e.activation(out=Wend[b][:].rearrange("p a j q -> p (a j q)"), in_=pWe[:], func=AF.Copy),
                 reads=[t_pWe], writes=[t_Wend[b]])

        def stageB1(g):
            cb, gl = g // 8, g % 8
            b = g % 2
            b4 = g % 4
            xb = cb % 2
            for j in range(4):
                P.op("pe", lambda e, j=j, xb=xb, gl=gl: e.transpose(
                    ptU[:, j * 128:(j + 1) * 128], xr[xb][:, gl, 8 * j:8 * j + 8, :].rearrange("p a b -> p (a b)"), idf[:]),
                    reads=[t_xr[xb], k.t_ident], writes=[t_ptU], inc=(j == 3))
            P.op("act", lambda e, b=b, g=g: e.activation(
                out=UT[b][:].rearrange("p j q -> p (j q)"), in_=ptU[:], func=AF.Identity,
                scale=M10[:, 0, g:g + 1], bias=M10[:, 1, g:g + 1]), reads=[t_ptU, t_M], writes=[t_UT[b]])
            for ri in range(2):
                for j in range(4):
                    P.op("pe", lambda e, b=b, ri=ri, j=j: e.matmul(
                        pE[0:64, ri * 128:(ri + 1) * 128], lhsT=Wend[b4][:, ri, j, :], rhs=UT[b][:, j, :],
                        start=(j == 0), stop=(j == 3)), reads=[t_Wend[b4], t_UT[b]], writes=[t_pE],
                        inc=(ri == 1 and j == 3))
            P.op("dve", lambda e: e.tensor_copy(out=Ha[:].rearrange("p a q -> p (a q)"), in_=pE[0:64, 0:256]),
                 reads=[t_pE], writes=[t_H])
            src_, dst_ = Ha, Hb
            for js, d in enumerate((1, 2, 4, 8, 16, 32, 64)):
                rdh = [t_H, t_tab]
                P.op("dve", lambda e, s_=src_, d_=dst_, d=d: e.tensor_copy(out=d_[:, :, 0:d], in_=s_[:, :, 0:d]),
                     reads=rdh, writes=[t_H])
                ar_ = SQr[:, g, js:js + 1]
                ai_ = SQi[:, g, js:js + 1]
                nai_ = nSQi[:, g, js:js + 1]
                n = 128 - d
                P.op("dve", lambda e, s_=src_, d_=dst_, d=d, n=n, ar_=ar_: e.scalar_tensor_tensor(
                    out=d_[:, :, d:128], in0=s_[:, :, 0:n], scalar=ar_, in1=s_[:, :, d:128], op0=ALU.mult, op1=ALU.add),
                    reads=rdh, writes=[t_H])
                P.op("dve", lambda e, s_=src_, d_=dst_, d=d, n=n, nai_=nai_: e.scalar_tensor_tensor(
                    out=d_[:, 0, d:128], in0=s_[:, 1, 0:n], scalar=nai_, in1=d_[:, 0, d:128], op0=ALU.mult, op1=ALU.add),
                    reads=rdh, writes=[t_H])
                P.op("dve", lambda e, s_=src_, d_=dst_, d=d, n=n, ai_=ai_: e.scalar_tensor_tensor(
                    out=d_[:, 1, d:128], in0=s_[:, 0, 0:n], scalar=ai_, in1=d_[:, 1, d:128], op0=ALU.mult, op1=ALU.add),
                    reads=rdh, writes=[t_H])
                src_, dst_ = dst_, src_
            P.op("dve", lambda e, b=b: e.memset(Hp[b][:, :, 0:1], 0.0), writes=[t_Hp[b]])
            P.op("dve", lambda e, b=b, s_=src_: e.tensor_copy(out=Hp[b][:, :, 1:128], in_=s_[:, :, 0:127]),
                 reads=[t_H], writes=[t_Hp[b]])
            yb = g % 2
            for j in range(4):
                P.op("pe", lambda e, b=b, j=j, yb=yb: e.matmul(pY[yb][:, 128 * j:512], lhsT=UT[b][:, j, :], rhs=Wg[b4][:, j, 128 * j:512],
                                                             start=(j == 0), stop=False),
                     reads=[t_UT[b], t_Wg[b4]], writes=[t_pY[yb]], inc=False)

        def stageB2(g):
            cb, gl = g // 8, g % 8
            b = g % 2
            b4 = g % 4
            yb = g % 2
            for ri in range(2):
                P.op("pe", lambda e, b=b, ri=ri, yb=yb: e.matmul(pY[yb][:], lhsT=Hp[b][:, ri, :], rhs=Wc[b4][:, ri, :],
                                                               start=False, stop=(ri == 1)),
                     reads=[t_Hp[b], t_Wc[b4]], writes=[t_pY[yb]], inc=(ri == 1))
            P.op("act", lambda e, yb=yb: e.activation(out=ygt[yb][:], in_=pY[yb][:], func=AF.Gelu),
                 reads=[t_pY[yb]], writes=[t_ygt[yb]])
            P.op("dve", lambda e, yb=yb, gl=gl: e.tensor_copy(
                out=ybuf[:, :, gl * 16:(gl + 1) * 16], in_=ygt[yb][:].rearrange("p (t c) -> p t c", c=16)),
                reads=[t_ygt[yb]], writes=[t_ybuf])
            if ydbg is not None and not getattr(k, "no_ydbg", False):
                P.op("dve", lambda e, yb=yb, gl=gl: e.tensor_copy(
                    out=ydbg[:, :, gl * 16:(gl + 1) * 16], in_=pY[yb][:].rearrange("p (t c) -> p t c", c=16)),
                    reads=[t_pY[yb]], writes=[t_ydbg, t_pY[yb]])
            if gl == 7:
                for t4 in range(8):
                    for i in range(4):
                        t = t4 * 4 + i
                        P.op("pe", lambda e, i=i, t=t: e.transpose(ptY[:, i * 128:(i + 1) * 128], ybuf[:, t, :], idb[:]),
                             reads=[t_ybuf, k.t_ident], writes=[t_ptY], inc=(i == 3))
                    P.op("dve", lambda e, t4=t4: e.tensor_copy(
                        out=yTst[:, t4 * 4:(t4 + 1) * 4, :].rearrange("p a q -> p (a q)"), in_=ptY[:]),
                        reads=[t_ptY], writes=[t_yT])
                P.dma("sp", k.yT_s[cb], yTst[:], [t_yT], [t_yTs], t_yT)
                if ydbg is not None:
                    ev = P.dma("sp", k.dbg["s5"].rearrange("(kk s) c -> kk s c", s=32)[:, :, cb * 128:(cb + 1) * 128],
                               ydbg[:], [t_ydbg], [], t_ydbg)
                    P.out_events.append(ev)

        for g in range(min(3, NG)):
            stageA(g)
        for g in range(NG):
            stageB1(g)
            if g + 3 < NG:
                stageA(g + 3)
            stageB2(g)
        P.mute = False
        dma_barrier(P, "sp", [t_yT])
        P.flush()


def phaseD1b(k):
    nc, P = k.nc, k.P
    prm = k.l1
    modT = k.modT[1]
    gbc = k.gbc
    idf = k.ident_f
    with ExitStack() as ph:
        w_glu = sb(k, ph, "w_glu_sb", [128, KC, 2 * D], BF16)
        t_wglu = P.tok("w_glu", dma=True)
        load_cast_weight(k, w_glu, prm["w_glu"].rearrange("(kc p) n -> p kc n", p=128), t_wglu, 4)
        bglu = sb(k, ph, "bglu", [1, 2 * D], BF16)
        t_bglu = P.tok("bglu", dma=True)
        P.dma("pool", bglu[:], prm["b_glu"], [], [t_bglu], t_bglu)
        ones_b = sb(k, ph, "ones_b", [1, 128], BF16)
        ones_f = sb(k, ph, "ones_f", [1, 128], F32)
        t_ones = P.tok("onesD")
        P.op("dve", lambda e: e.memset(ones_b[:], 1.0), writes=[t_ones])
        P.op("dve", lambda e: e.memset(ones_f[:], 1.0), writes=[t_ones])
        rw = sb(k, ph, "rw", [128, KC, NE], F32)
        rb_ = sb(k, ph, "rb", [1, NE], F32)
        t_rw = P.tok("rw", dma=True)
        P.dma("sp", rw[:], prm["router_w"].rearrange("(kc p) e -> p kc e", p=128), [], [t_rw], t_rw)
        P.dma("sp", rb_[:], prm["router_b"], [], [t_rw], t_rw)
        lng = sb(k, ph, "l1ln1g", [128, D], F32)
        lnb = sb(k, ph, "l1ln1b", [128, D], F32)
        t_lng = P.tok("l1ln1g", dma=True)
        t_lnb = P.tok("l1ln1b", dma=True)
        load_bc(k, lng[:], prm["ln1_g"], t_lng)
        load_bc(k, lnb[:], prm["ln1_b"], t_lnb)
        gcol = sb(k, ph, "gcolD", [128, 2, KC], F32)
        t_gcol = P.tok("gcolD", dma=True)
        P.dma("sp", gcol[:, 0, :], prm["ln1_g"].rearrange("o (kc p) -> p (o kc)", p=128), [], [t_gcol], t_gcol,
              allow_slow_non_contiguous=True)
        P.dma("sp", gcol[:, 1, :], prm["ln1_b"].rearrange("o (kc p) -> p (o kc)", p=128), [], [t_gcol], t_gcol,
              allow_slow_non_contiguous=True)
        AB = sb(k, ph, "ABD", [128, 2, KC], F32)
        t_AB = P.tok("ABD")
        P.op("dve", lambda e: e.tensor_mul(out=AB[:, 0, :], in0=gcol[:, 0, :], in1=modT[:, 32:40]),
             reads=[t_gcol, k.t_modT[1]], writes=[t_AB])
        P.op("dve", lambda e: e.tensor_mul(out=AB[:, 1, :], in0=gcol[:, 1, :], in1=modT[:, 32:40]),
             reads=[t_gcol, k.t_modT[1]], writes=[t_AB])
        P.op("dve", lambda e: e.tensor_add(out=AB[:, 1, :], in0=AB[:, 1, :], in1=modT[:, 24:32]),
             reads=[k.t_modT[1]], writes=[t_AB])
        L = LNScratch(k, ph, "d1b")
        yT = [sb(k, ph, "yTt%d" % i, [128, 8, 128], BF16) for i in range(2)]
        x2t = [sb(k, ph, "x2t%d" % i, [128, D], F32) for i in range(2)]
        sig = sb(k, ph, "sig", [128, D], F32)
        r = [sb(k, ph, "rD%d" % i, [128, D], F32) for i in range(2)]
        x3o = [sb(k, ph, "x3o%d" % i, [128, D], F32) for i in range(2)]
        hmTf = sb(k, ph, "hmTf", [128, KC, 128], F32)
        hmTb = [sb(k, ph, "hmTb%d" % i, [128, KC, 128], BF16) for i in range(2)]
        lg = sb(k, ph, "lg", [128, NE], F32)
        mx8 = sb(k, ph, "mx8", [128, 8], F32)
        mk = sb(k, ph, "mk", [128, 2, NE], F32)
        gg = sb(k, ph, "gg", [128, 2], F32)
        cw = [sb(k, ph, "cw%d" % i, [128, NE], F32) for i in range(2)]
        pz = [ps(k, ph, "pz%d" % i, [128, 512]) for i in range(4)]
        ptf = [ps(k, ph, "ptfD%d" % i, [128, 512]) for i in range(2)]
        plg = ps(k, ph, "plg", [128, 512])
        t_yT = [P.tok("yTt%d" % i, dma=True) for i in range(2)]
        t_x2t = [P.tok("x2t%d" % i, dma=True) for i in range(2)]
        t_sig = P.tok("sig")
        t_r = [P.tok("rD%d" % i) for i in range(2)]
        t_x3o = [P.tok("x3o%d" % i, dma=True) for i in range(2)]
        t_hmTf = P.tok("hmTf")
        t_hmTb = [P.tok("hmTb%d" % i, dma=True) for i in range(2)]
        t_rt = P.tok("router")
        t_cw = [P.tok("cw%d" % i, dma=True) for i in range(2)]
        t_pz = [P.tok("pz%d" % i) for i in range(4)]
        t_ptf = [P.tok("ptfD%d" % i) for i in range(2)]
        t_plg = P.tok("plg")
        t_scr = P.tok("scrD1b")
        x2v = k.x2_s.rearrange("(kk s) c -> s kk c", s=32)
        hv = k.hmT_s.rearrange("(kc p) (t q) -> p kc t q", p=128, q=128)
        rx = sb(k, ph, "rD2", [128, D], F32)
        r = r + [rx]
        t_r = t_r + [P.tok("rD2")]
        x2x = sb(k, ph, "x2t2", [128, D], F32)
        x2t = x2t + [x2x]
        t_x2t = t_x2t + [P.tok("x2t2", dma=True)]
        dst = dict(itf=0)

        yTx = sb(k, ph, "yTt2", [128, 8, 128], BF16)
        yT = yT + [yTx]
        t_yT = t_yT + [P.tok("yTt2", dma=True)]

        def LD(t):
            b = t % 3
            P.dma("sp", yT[b][:], k.yT_s[:, :, t, :].rearrange("cb c q -> c cb q"), [], [t_yT[b]], t_yT[b])
            P.dma("sp", x2t[t % 3][:], x2v[t], [], [t_x2t[t % 3]], t_x2t[t % 3])

        def S1(t):
            b = t % 3
            for n in range(4):
                for cb in range(8):
                    P.op("pe", lambda e, n=n, cb=cb, b=b: e.matmul(
                        pz[n][:], lhsT=yT[b][:, cb, :], rhs=w_glu[:, cb, n * 512:(n + 1) * 512], start=(cb == 0), stop=False),
                        reads=[t_yT[b], t_wglu], writes=[t_pz[n]], inc=False)
                P.op("pe", lambda e, n=n: e.matmul(pz[n][:], lhsT=ones_b[0:1, :], rhs=bglu[0:1, n * 512:(n + 1) * 512],
                                                   start=False, stop=True),
                     reads=[t_ones, t_bglu], writes=[t_pz[n]])

        def S2(t):
            b = t % 2
            rb = t % 3
            for hf in range(2):
                P.op("act", lambda e, hf=hf: e.activation(out=sig[:, hf * 512:(hf + 1) * 512], in_=pz[2 + hf][:], func=AF.Sigmoid),
                     reads=[t_pz[2 + hf]], writes=[t_sig])
                P.op("dve", lambda e, hf=hf: e.tensor_tensor(out=sig[:, hf * 512:(hf + 1) * 512], in0=pz[hf][:],
                                                            in1=sig[:, hf * 512:(hf + 1) * 512], op=ALU.mult),
                     reads=[t_pz[hf]], writes=[t_sig])
            P.op("pool", lambda e: e.tensor_tensor(out=sig[:], in0=sig[:], in1=gbc[:, 0, :], op=ALU.mult),
                 reads=[k.t_gbc], writes=[t_sig])
            P.op("dve", lambda e, rb=rb: e.scalar_tensor_tensor(out=r[rb][:], in0=x2t[rb][:], scalar=DN_ALPHA, in1=sig[:],
                                                               op0=ALU.mult, op1=ALU.add),
                 reads=[t_x2t[rb], t_sig], writes=[t_r[rb]])
            emit_ln(k, r[rb][:], t_r[rb], L)
            P.op("pool", lambda e, b=b, rb=rb: e.tensor_tensor(out=x3o[b][:], in0=r[rb][:], in1=lng[:], op=ALU.mult),
                 reads=[t_r[rb], t_lng], writes=[t_x3o[b]])
            P.op("pool", lambda e, b=b: e.tensor_tensor(out=x3o[b][:], in0=x3o[b][:], in1=lnb[:], op=ALU.add),
                 reads=[t_lnb], writes=[t_x3o[b]])
            P.dma("sp", k.x3_s[t], x3o[b][:], [t_x3o[b]], [t_scr], t_x3o[b])

        def S3(t):
            b = t % 2
            rb = t % 3
            for k4 in range(2):
                tf = dst["itf"] % 2
                dst["itf"] += 1
                for q in range(4):
                    kc = k4 * 4 + q
                    P.op("pe", lambda e, tf=tf, kc=kc, q=q, rb=rb: e.transpose(
                        ptf[tf][:, q * 128:(q + 1) * 128], r[rb][:, kc * 128:(kc + 1) * 128], idf[:]),
                        reads=[t_r[rb], k.t_ident], writes=[t_ptf[tf]], inc=(q == 3))
                for q in range(4):
                    kc = k4 * 4 + q
                    P.op("act", lambda e, tf=tf, kc=kc, q=q: e.activation(
                        out=hmTf[:, kc, :], in_=ptf[tf][:, q * 128:(q + 1) * 128],
                        func=AF.Identity, scale=AB[:, 0, kc:kc + 1], bias=AB[:, 1, kc:kc + 1]),
                        reads=[t_ptf[tf], t_AB], writes=[t_hmTf])
            P.op("dve", lambda e, b=b: e.tensor_copy(out=hmTb[b][:], in_=hmTf[:]), reads=[t_hmTf], writes=[t_hmTb[b]])
            P.dma("sp", hv[:, :, t, :], hmTb[b][:], [t_hmTb[b]], [t_scr], t_hmTb[b])
            for kc in range(KC):
                P.op("pe", lambda e, kc=kc: e.matmul(plg[:, 0:NE], lhsT=hmTf[:, kc, :], rhs=rw[:, kc, :],
                                                     start=(kc == 0), stop=False),
                     reads=[t_hmTf, t_rw], writes=[t_plg], inc=False)
            P.op("pe", lambda e: e.matmul(plg[:, 0:NE], lhsT=ones_f[0:1, :], rhs=rb_[0:1, :], start=False, stop=True),
                 reads=[t_ones, t_rw], writes=[t_plg])
            P.op("dve", lambda e: e.tensor_copy(out=lg[:], in_=plg[:, 0:NE]), reads=[t_plg], writes=[t_rt])
            rdr = [t_rt]
            P.op("dve", lambda e: e.max(out=mx8[:], in_=lg[:]), reads=rdr, writes=[t_rt])
            P.op("dve", lambda e: e.tensor_scalar(out=mk[:, 0, :], in0=lg[:], scalar1=mx8[:, 0:1], scalar2=None, op0=ALU.is_equal),
                 reads=rdr, writes=[t_rt])
            P.op("dve", lambda e: e.tensor_scalar(out=mk[:, 1, :], in0=lg[:], scalar1=mx8[:, 1:2], scalar2=None, op0=ALU.is_equal),
                 reads=rdr, writes=[t_rt])
            P.op("dve", lambda e: e.tensor_sub(out=gg[:, 0:1], in0=mx8[:, 0:1], in1=mx8[:, 1:2]), reads=rdr, writes=[t_rt])
            P.op("act", lambda e: e.activation(out=gg[:, 0:1], in_=gg[:, 0:1], func=AF.Sigmoid), reads=rdr, writes=[t_rt])
            P.op("dve", lambda e: e.tensor_scalar(out=gg[:, 1:2], in0=gg[:, 0:1], scalar1=-1.0, scalar2=1.0,
                                                  op0=ALU.mult, op1=ALU.add), reads=rdr, writes=[t_rt])
            P.op("dve", lambda e, b=b: e.tensor_scalar_mul(out=cw[b][:], in0=mk[:, 0, :], scalar1=gg[:, 0:1]),
                 reads=rdr, writes=[t_cw[b]])
            P.op("dve", lambda e, b=b: e.scalar_tensor_tensor(out=cw[b][:], in0=mk[:, 1, :], scalar=gg[:, 1:2], in1=cw[b][:],
                                                             op0=ALU.mult, op1=ALU.add), reads=rdr, writes=[t_cw[b]])
            P.dma("sp", k.cw_s[t], cw[b][:], [t_cw[b]], [t_scr], t_cw[b])

        LD(0)
        LD(1)
        S1(0)
        for t in range(32):
            if t + 2 < 32:
                LD(t + 2)
            S2(t)
            if t + 1 < 32:
                S1(t + 1)
            if t >= 1:
                S3(t - 1)
        S3(31)
        dma_barrier(P, "sp", t_hmTb + t_x3o + t_cw)
        if k.debug and "x3" in k.dbg:
            big = sb(k, ph, "dbgbigD", [128, 2, 1024], F32)
            t_big = P.tok("dbgbigD", dma=True)
            for c8 in range(16):
                P.dma("sp", big[:], k.x3_s[c8 * 2:(c8 + 1) * 2].rearrange("t q d -> q t d"), [t_scr], [t_big], t_big)
                ev = P.dma("sp", k.dbg["x3"].rearrange("(kk s) d -> s kk d", s=32)[c8 * 2:(c8 + 1) * 2].rearrange("t q d -> q t d"),
                           big[:], [t_big], [], t_big)
            lgd = sb(k, ph, "dbgcw", [128, 32, 8], F32)
            t_lgd = P.tok("dbgcw", dma=True)
            P.dma("sp", lgd[:], k.cw_s.rearrange("t q e -> q t e"), [t_scr], [t_lgd], t_lgd)
            ev2 = P.dma("sp", k.dbg["cw"].rearrange("(kk s) e -> kk s e", s=32), lgd[:], [t_lgd], [], t_lgd)
            P.out_events.append(ev)
            P.out_events.append(ev2)
        P.flush()


def phaseD2(k):
    nc, P = k.nc, k.P
    prm = k.l1
    gbc = k.gbc
    with ExitStack() as ph:
        wg = [sb(k, ph, "ewg%d" % i, [128, KC, DFE], BF16) for i in range(2)]
        wu = [sb(k, ph, "ewu%d" % i, [128, KC, DFE], BF16) for i in range(2)]
        wd = [sb(k, ph, "ewd%d" % i, [128, FCE, D], BF16) for i in range(2)]
        t_wg = [P.tok("ewg%d" % i, dma=True) for i in range(2)]
        t_wu = [P.tok("ewu%d" % i, dma=True) for i in range(2)]
        t_wd = [P.tok("ewd%d" % i, dma=True) for i in range(2)]
        lng = sb(k, ph, "l1ln2g", [128, D], F32)
        lnb = sb(k, ph, "l1ln2b", [128, D], F32)
        t_lng = P.tok("l1ln2g", dma=True)
        t_lnb = P.tok("l1ln2b", dma=True)
        load_bc(k, lng[:], prm["ln2_g"], t_lng)
        load_bc(k, lnb[:], prm["ln2_b"], t_lnb)
        L = LNScratch(k, ph, "d2")
        hT = [sb(k, ph, "hTm%d" % i, [128, KC, 512], BF16) for i in range(2)]
        actT = sb(k, ph, "actTm", [128, FCE, 512], BF16)
        sg = [sb(k, ph, "sgm%d" % i, [128, 512], BF16) for i in range(2)]
        acc = [sb(k, ph, "accm%d" % i, [128, D], F32) for i in range(3)]
        x3t = [sb(k, ph, "x3t%d" % i, [128, D], F32) for i in range(2)]
        cwt = [sb(k, ph, "cwt%d" % i, [128, 4, NE], F32) for i in range(2)]
        pg = [ps(k, ph, "pgm%d" % i, [128, 512]) for i in range(2)]
        pu = [ps(k, ph, "pum%d" % i, [128, 512]) for i in range(2)]
        py = [ps(k, ph, "pym%d" % i, [128, 512]) for i in range(4)]
        t_hT = [P.tok("hTm%d" % i, dma=True) for i in range(2)]
        t_actT = P.tok("actTm")
        t_sg = [P.tok("sgm%d" % i) for i in range(2)]
        t_acc = [P.tok("accm%d" % i, dma=True) for i in range(3)]
        t_x3t = [P.tok("x3t%d" % i, dma=True) for i in range(2)]
        t_cwt = [P.tok("cwt%d" % i, dma=True) for i in range(2)]
        t_pg = [P.tok("pgm%d" % i) for i in range(2)]
        t_pu = [P.tok("pum%d" % i) for i in range(2)]
        t_py = [P.tok("pym%d" % i) for i in range(4)]
        t_yacc = [P.tok("yacc%d" % t) for t in range(32)]
        outv = k.out.rearrange("(kk s) d -> s kk d", s=32)
        hv = k.hmT_s.rearrange("(kc p) n -> p kc n", p=128)

        def load_expert(e):
            b = e % 2
            load_cast_weight(k, wg[b], prm["w_gate"][e].rearrange("(kc p) n -> p kc n", p=128), t_wg[b], 4)
            load_cast_weight(k, wu[b], prm["w_up"][e].rearrange("(kc p) n -> p kc n", p=128), t_wu[b], 4)
            load_cast_weight(k, wd[b], prm["w_down"][e].rearrange("(fc p) n -> p fc n", p=128), t_wd[b], 4)
            P.op("pool", lambda ee, b=b: ee.tensor_tensor(
                out=wd[b][:], in0=wd[b][:], in1=gbc[:, 1, :].unsqueeze(1).broadcast_to([128, FCE, D]), op=ALU.mult),
                reads=[k.t_gbc], writes=[t_wd[b]])
        load_expert(0)
        ipy = 0
        it = 0

        def ld_tile(i):
            e_ = i // 32
            t_ = i % 32
            if e_ > 0:
                P.dma("sp", acc[i % 3][:], k.yacc_s[t_], [t_yacc[t_]], [t_acc[i % 3]], t_acc[i % 3])
            if e_ == NE - 1:
                P.dma("sp", x3t[i % 2][:], k.x3_s[t_], [], [t_x3t[i % 2]], t_x3t[i % 2])

        def ld_group(idx):
            g_ = idx % 8
            hb_ = idx % 2
            P.dma("sp", hT[hb_][:], hv[:, :, g_ * 512:(g_ + 1) * 512], [], [t_hT[hb_]], t_hT[hb_])
            P.dma("sp", cwt[hb_][:], k.cw_s[g_ * 4:(g_ + 1) * 4].rearrange("t q e -> q t e"), [], [t_cwt[hb_]], t_cwt[hb_])
        for e in range(NE):
            eb = e % 2
            if e + 1 < NE:
                load_expert(e + 1)
            for g in range(8):
                hb = (e * 8 + g) % 2
                if e == 0 and g == 0:
                    ld_group(0)
                if e * 8 + g + 1 < NE * 8:
                    ld_group(e * 8 + g + 1)
                for fc in range(FCE):
                    b = fc % 2
                    for kc in range(KC):
                        P.op("pe", lambda ee, b=b, kc=kc, fc=fc, eb=eb, hb=hb: ee.matmul(
                            pg[b][:], lhsT=wg[eb][:, kc, fc * 128:(fc + 1) * 128], rhs=hT[hb][:, kc, :],
                            start=(kc == 0), stop=(kc == KC - 1)),
                            reads=[t_wg[eb], t_hT[hb]], writes=[t_pg[b]], inc=(kc == KC - 1))
                    for kc in range(KC):
                        P.op("pe", lambda ee, b=b, kc=kc, fc=fc, eb=eb, hb=hb: ee.matmul(
                            pu[b][:], lhsT=wu[eb][:, kc, fc * 128:(fc + 1) * 128], rhs=hT[hb][:, kc, :],
                            start=(kc == 0), stop=(kc == KC - 1)),
                            reads=[t_wu[eb], t_hT[hb]], writes=[t_pu[b]], inc=(kc == KC - 1))
                    P.op("act", lambda ee, b=b: ee.activation(out=sg[b][:], in_=pg[b][:], func=AF.Silu),
                         reads=[t_pg[b]], writes=[t_sg[b]])
                    P.op("dve", lambda ee, b=b, fc=fc: ee.tensor_tensor(out=actT[:, fc, :], in0=sg[b][:], in1=pu[b][:],
                                                                       op=ALU.mult),
                         reads=[t_sg[b], t_pu[b]], writes=[t_actT])
                for tt in range(4):
                    t = g * 4 + tt
                    ab = it % 3
                    xb3 = it % 2
                    if it == 0:
                        ld_tile(0)
                    if it + 1 < NE * 32:
                        ld_tile(it + 1)
                    it += 1
                    pys = []
                    for dh in range(2):
                        pb = ipy % 4
                        ipy += 1
                        pys.append(pb)
                        for fc in range(FCE):
                            P.op("pe", lambda ee, pb=pb, fc=fc, tt=tt, dh=dh, eb=eb: ee.matmul(
                                py[pb][:], lhsT=actT[:, fc, tt * 128:(tt + 1) * 128], rhs=wd[eb][:, fc, dh * 512:(dh + 1) * 512],
                                start=(fc == 0), stop=(fc == FCE - 1)),
                                reads=[t_actT, t_wd[eb]], writes=[t_py[pb]], inc=(fc == FCE - 1))
                    for dh in range(2):
                        pb = pys[dh]
                        if e == 0:
                            P.op("dve", lambda ee, pb=pb, dh=dh, ab=ab, hb=hb, tt=tt, e=e: ee.tensor_scalar_mul(
                                out=acc[ab][:, dh * 512:(dh + 1) * 512], in0=py[pb][:], scalar1=cwt[hb][:, tt, e:e + 1]),
                                reads=[t_py[pb], t_cwt[hb]], writes=[t_acc[ab]])
                        else:
                            P.op("dve", lambda ee, pb=pb, dh=dh, ab=ab, hb=hb, tt=tt, e=e: ee.scalar_tensor_tensor(
                                out=acc[ab][:, dh * 512:(dh + 1) * 512], in0=py[pb][:], scalar=cwt[hb][:, tt, e:e + 1],
                                in1=acc[ab][:, dh * 512:(dh + 1) * 512], op0=ALU.mult, op1=ALU.add),
                                reads=[t_py[pb], t_cwt[hb]], writes=[t_acc[ab]])
                    if e < NE - 1:
                        P.dma("sp", k.yacc_s[t], acc[ab][:], [t_acc[ab]], [t_yacc[t]], t_acc[ab])
                    else:
                        P.op("dve", lambda ee, ab=ab, xb3=xb3: ee.scalar_tensor_tensor(
                            out=acc[ab][:], in0=x3t[xb3][:], scalar=DN_ALPHA, in1=acc[ab][:], op0=ALU.mult, op1=ALU.add),
                            reads=[t_x3t[xb3]], writes=[t_acc[ab]])
                        emit_ln(k, acc[ab][:], t_acc[ab], L)
                        P.op("dve", lambda ee, ab=ab: ee.tensor_tensor(out=acc[ab][:], in0=acc[ab][:], in1=lng[:], op=ALU.mult),
                             reads=[t_lng], writes=[t_acc[ab]])
                        P.op("pool", lambda ee, ab=ab: ee.tensor_tensor(out=acc[ab][:], in0=acc[ab][:], in1=lnb[:], op=ALU.add),
                             reads=[t_lnb], writes=[t_acc[ab]])
                        ev = P.dma("sp", outv[t], acc[ab][:], [t_acc[ab]], [], t_acc[ab])
                        P.out_events.append(ev)
        P.flush()


_NC_CACHE = {}


def _core_inputs(inputs, b):
    f = lambda a: np.ascontiguousarray(np.asarray(a, dtype=np.float32))
    m = {"x": f(inputs["x"][b]), "c": f(inputs["c"][b:b + 1])}
    for n in ["ada_w", "w_in", "w_out", "ffn_w_gate", "ffn_w_up", "ffn_w_down"]:
        m["l0_" + n] = f(inputs["l0_" + n])
    for n in ["ada_b", "subln_w", "ln1_g", "ln1_b", "ln2_g", "ln2_b"]:
        m["l0_" + n] = f(inputs["l0_" + n])[None, :]
    m["l0_lam"] = f(np.concatenate([np.asarray(inputs["l0_lam_q1"]), np.asarray(inputs["l0_lam_k1"]),
                                    np.asarray(inputs["l0_lam_q2"]), np.asarray(inputs["l0_lam_k2"])]))[None, :]
    for n in ["ada_w", "a_re", "a_im", "b_re", "b_im", "w_glu", "router_w", "exp_w_gate", "exp_w_up", "exp_w_down"]:
        m["l1_" + n] = f(inputs["l1_" + n])
    for n in ["ada_b", "log_dt", "d_skip", "b_glu", "ln1_g", "ln1_b", "router_b", "ln2_g", "ln2_b"]:
        m["l1_" + n] = f(inputs["l1_" + n])[None, :]
    m["l1_c_re"] = f(inputs["l1_c_re"]).reshape(1024, 64)
    m["l1_c_im"] = f(inputs["l1_c_im"]).reshape(1024, 64)
    return m


def kernel(**inputs):
    if "nc" not in _NC_CACHE:
        _NC_CACHE["nc"] = build_nc()
    nc = _NC_CACHE["nc"]
    n = 8
    shared = _core_inputs(inputs, 0)
    in_maps = []
    for b in range(n):
        m = dict(shared)
        m["x"] = np.ascontiguousarray(np.asarray(inputs["x"][b], dtype=np.float32))
        m["c"] = np.ascontiguousarray(np.asarray(inputs["c"][b:b + 1], dtype=np.float32))
        in_maps.append(m)
    res = run_bass_kernel_spmd(nc, in_maps, core_ids=list(range(n)))
    return np.stack([np.asarray(r["out"], dtype=np.float32) for r in res.results], axis=0)
```
